# Optimizing a Trainium2 kernel written in Bass

```python
import math
import jax, jax.numpy as jnp
from jax import lax
import numpy as np

D_MODEL = 1024
BATCH = 8
SEQ = 8192
DEPTH = 2

MEM_LEN = 256
N_MIXERS = 2
N_LAYERS_A = (DEPTH + 1) // 2
N_LAYERS_B = DEPTH // 2

MEM_HEADS = 4
MEM_HEAD_DIM = 128
MEM_W = MEM_HEADS * MEM_HEAD_DIM

CHUNK = 128
A_WIDTH = D_MODEL
A_GROUPS = 8
A_GROUP_DIM = A_WIDTH // A_GROUPS

MLA_HEADS = 8
Q_LORA = 512
KV_LORA = 256
NOPE_DIM = 128
ROPE_DIM = 64
V_DIM = 128
QK_DIM = NOPE_DIM + ROPE_DIM
ROPE_BASE = 10000.0
Q_BLOCK = 128

N_GROUPS = 4
EXPERTS_PER_GROUP = 8
N_EXPERTS = N_GROUPS * EXPERTS_PER_GROUP
TOP_K = 2
EXPERT_FF = 256
TOKEN_BLOCK = 2048

EPS = 1e-6

kernel_name = "hybrid_gmlp_mla_memory_hmoe"


def rms_norm(x, g):
    xf = x.astype(jnp.float32)
    y = xf * lax.rsqrt(jnp.mean(xf * xf, axis=-1, keepdims=True) + EPS)
    return (y * g.astype(jnp.float32)).astype(x.dtype)


def layer_norm(x, g, b):
    xf = x.astype(jnp.float32)
    mu = jnp.mean(xf, axis=-1, keepdims=True)
    var = jnp.mean(jnp.square(xf - mu), axis=-1, keepdims=True)
    y = (xf - mu) * lax.rsqrt(var + EPS)
    return (y * g.astype(jnp.float32) + b.astype(jnp.float32)).astype(x.dtype)


def rope(x, pos):
    r = x.shape[-1]
    half = r // 2
    inv = ROPE_BASE ** (-(jnp.arange(half, dtype=jnp.float32) * 2.0 / r))
    ang = pos.astype(jnp.float32)[:, :, None, None] * inv
    cos, sin = jnp.cos(ang), jnp.sin(ang)
    xf = x.astype(jnp.float32)
    x1, x2 = xf[..., :half], xf[..., half:]
    return jnp.concatenate([x1 * cos - x2 * sin, x1 * sin + x2 * cos], axis=-1).astype(x.dtype)


def causal_block_attention(q, k, v):
    b, s, h, dk = q.shape
    nb = s // Q_BLOCK
    scale = dk ** -0.5
    qb = q.reshape(b, nb, Q_BLOCK, h, dk).transpose(1, 0, 2, 3, 4)
    kpos = jnp.arange(s)
    neg = jnp.finfo(jnp.float32).min

    def one_block(args):
        i, qi = args
        sc = jnp.einsum('bqhd,bkhd->bhqk', qi, k, preferred_element_type=jnp.float32) * scale
        qpos = i * Q_BLOCK + jnp.arange(Q_BLOCK)
        sc = jnp.where(kpos[None, :] <= qpos[:, None], sc, neg)
        p = jax.nn.softmax(sc, axis=-1)
        return jnp.einsum('bhqk,bkhd->bqhd', p.astype(v.dtype), v)

    out = lax.map(one_block, (jnp.arange(nb), qb))
    return out.transpose(1, 0, 2, 3, 4).reshape(b, s, h, v.shape[-1])


def memory_attention(q, mem_k, mem_v, qn_g, kn_g):
    q = rms_norm(q, qn_g)
    k = rms_norm(mem_k, kn_g)
    sc = jnp.einsum('bshd,bmhd->bhsm', q, k, preferred_element_type=jnp.float32) * (MEM_HEAD_DIM ** -0.5)
    p = jax.nn.softmax(sc, axis=-1)
    o = jnp.einsum('bhsm,bmhd->bshd', p.astype(mem_v.dtype), mem_v)
    return o.reshape(q.shape[0], q.shape[1], MEM_W)


def gmlp_mixer(h, w_in, ln_g, ln_b, w_s, b_s):
    b, s, _ = h.shape
    z = h @ w_in
    uv = jax.nn.gelu(z[..., :2 * A_WIDTH], approximate=False)
    u, v = uv[..., :A_WIDTH], uv[..., A_WIDTH:]
    q_mem = z[..., 2 * A_WIDTH:].reshape(b, s, MEM_HEADS, MEM_HEAD_DIM)
    v = layer_norm(v, ln_g, ln_b)
    vc = v.reshape(b, s // CHUNK, CHUNK, A_GROUPS, A_GROUP_DIM)
    causal = jnp.tril(jnp.ones((CHUNK, CHUNK), dtype=bool))
    w = jnp.where(causal[None], w_s, jnp.zeros((), w_s.dtype))
    mixed = jnp.einsum('gts,bnsgc->bntgc', w, vc) + b_s.T[None, None, :, :, None]
    return u * mixed.reshape(b, s, A_WIDTH), q_mem


def mla_mixer(h, positions, w_in, q_norm_g, kv_norm_g, w_q_up, w_kv_up, qn_g, kn_g):
    b, s, _ = h.shape
    z = h @ w_in
    o1 = Q_LORA
    o2 = o1 + KV_LORA
    o3 = o2 + ROPE_DIM
    cq = rms_norm(z[..., :o1], q_norm_g)
    ckv = rms_norm(z[..., o1:o2], kv_norm_g)
    k_rope = z[..., o2:o3]
    q_mem = z[..., o3:].reshape(b, s, MEM_HEADS, MEM_HEAD_DIM)
    q = (cq @ w_q_up).reshape(b, s, MLA_HEADS, QK_DIM)
    kv = (ckv @ w_kv_up).reshape(b, s, MLA_HEADS, NOPE_DIM + V_DIM)
    k_nope, v = kv[..., :NOPE_DIM], kv[..., NOPE_DIM:]
    k_rope = jnp.broadcast_to(k_rope[:, :, None, :], (b, s, MLA_HEADS, ROPE_DIM))
    k = jnp.concatenate([k_nope, k_rope], axis=-1)
    q = rms_norm(q, qn_g)
    k = rms_norm(k, kn_g)
    q = jnp.concatenate([q[..., :NOPE_DIM], rope(q[..., NOPE_DIM:], positions)], axis=-1)
    k = jnp.concatenate([k[..., :NOPE_DIM], rope(k[..., NOPE_DIM:], positions)], axis=-1)
    o = causal_block_attention(q, k, v)
    return o.reshape(b, s, MLA_HEADS * V_DIM), q_mem


def hierarchical_moe(h, w_group, b_group, w_expert, b_expert, w_gate, w_up, w_down):
    b, s, d = h.shape
    t = h.reshape(-1, d)
    n = t.shape[0]
    g_prob = jax.nn.softmax((t @ w_group).astype(jnp.float32) + b_group.astype(jnp.float32), axis=-1)
    g_idx = jnp.argmax(g_prob, axis=-1)
    g_w = jnp.take_along_axis(g_prob, g_idx[:, None], axis=-1)
    e_logits = ((t @ w_expert).astype(jnp.float32) + b_expert.astype(jnp.float32))
    e_logits = e_logits.reshape(n, N_GROUPS, EXPERTS_PER_GROUP)
    e_sel = jnp.take_along_axis(e_logits, g_idx[:, None, None], axis=1)[:, 0]
    e_prob = jax.nn.softmax(e_sel, axis=-1)
    top_w, top_i = lax.top_k(e_prob, TOP_K)
    top_w = top_w / jnp.sum(top_w, axis=-1, keepdims=True)
    expert_id = g_idx[:, None] * EXPERTS_PER_GROUP + top_i
    weights = g_w * top_w
    combine = jnp.sum(jax.nn.one_hot(expert_id, N_EXPERTS, dtype=jnp.float32) * weights[..., None], axis=1)
    tb = math.gcd(n, TOKEN_BLOCK)
    nb = n // tb

    def expert_block(args):
        xb, cb = args
        gate = jnp.einsum('td,edf->tef', xb, w_gate)
        up = jnp.einsum('td,edf->tef', xb, w_up)
        act = jax.nn.silu(gate) * up * cb[..., None].astype(xb.dtype)
        return jnp.einsum('tef,efd->td', act, w_down)

    y = lax.map(expert_block, (t.reshape(nb, tb, d), combine.reshape(nb, tb, N_EXPERTS)))
    return y.reshape(b, s, d)


def setup_inputs(seed: int = 0) -> dict:
    key = jax.random.key(seed)
    ks = iter(jax.random.split(key, 40))
    f32 = jnp.float32

    def nrm(shape, scale):
        return jax.random.normal(next(ks), shape, f32) * scale

    def gain(shape):
        return 1.0 + 0.05 * jax.random.normal(next(ks), shape, f32)

    D = D_MODEL
    a_in = 2 * A_WIDTH + MEM_W
    a_out = A_WIDTH + MEM_W
    b_in = Q_LORA + KV_LORA + ROPE_DIM + MEM_W
    b_out = MLA_HEADS * V_DIM + MEM_W
    x = jax.random.normal(next(ks), (BATCH, SEQ, D), f32)
    mem = jax.random.normal(next(ks), (BATCH, MEM_LEN, D), f32)
    offsets = jax.random.randint(next(ks), (BATCH, 1), 0, 4096, dtype=jnp.int32)
    positions = offsets + jnp.arange(SEQ, dtype=jnp.int32)[None, :]
    return {
        "x": x,
        "mem": mem,
        "positions": positions,
        "mem_norm_g": gain((D,)),
        "w_mem_kv": nrm((D, 2 * MEM_W), D ** -0.5),
        "mem_qn_g": gain((DEPTH, MEM_HEAD_DIM)),
        "mem_kn_g": gain((DEPTH, MEM_HEAD_DIM)),
        "norm1_g": gain((DEPTH, D)),
        "norm2_g": gain((DEPTH, D)),
        "a_w_in": nrm((N_LAYERS_A, D, a_in), D ** -0.5),
        "a_ln_g": gain((N_LAYERS_A, A_WIDTH)),
        "a_ln_b": nrm((N_LAYERS_A, A_WIDTH), 0.02),
        "a_w_s": nrm((N_LAYERS_A, A_GROUPS, CHUNK, CHUNK), CHUNK ** -0.5),
        "a_b_s": gain((N_LAYERS_A, A_GROUPS, CHUNK)),
        "a_w_out": nrm((N_LAYERS_A, a_out, D), a_out ** -0.5),
        "b_w_in": nrm((N_LAYERS_B, D, b_in), D ** -0.5),
        "b_q_norm_g": gain((N_LAYERS_B, Q_LORA)),
        "b_kv_norm_g": gain((N_LAYERS_B, KV_LORA)),
        "b_w_q_up": nrm((N_LAYERS_B, Q_LORA, MLA_HEADS * QK_DIM), Q_LORA ** -0.5),
        "b_w_kv_up": nrm((N_LAYERS_B, KV_LORA, MLA_HEADS * (NOPE_DIM + V_DIM)), KV_LORA ** -0.5),
        "b_qn_g": gain((N_LAYERS_B, QK_DIM)),
        "b_kn_g": gain((N_LAYERS_B, QK_DIM)),
        "b_w_out": nrm((N_LAYERS_B, b_out, D), b_out ** -0.5),
        "moe_w_group": nrm((DEPTH, D, N_GROUPS), D ** -0.5),
        "moe_b_group": nrm((DEPTH, N_GROUPS), 0.01),
        "moe_w_expert": nrm((DEPTH, D, N_EXPERTS), D ** -0.5),
        "moe_b_expert": nrm((DEPTH, N_EXPERTS), 0.01),
        "moe_w_gate": nrm((DEPTH, N_EXPERTS, D, EXPERT_FF), D ** -0.5),
        "moe_w_up": nrm((DEPTH, N_EXPERTS, D, EXPERT_FF), D ** -0.5),
        "moe_w_down": nrm((DEPTH, N_EXPERTS, EXPERT_FF, D), EXPERT_FF ** -0.5),
    }


def reference(x, mem, positions, mem_norm_g, w_mem_kv, mem_qn_g, mem_kn_g, norm1_g, norm2_g,
              a_w_in, a_ln_g, a_ln_b, a_w_s, a_b_s, a_w_out,
              b_w_in, b_q_norm_g, b_kv_norm_g, b_w_q_up, b_w_kv_up, b_qn_g, b_kn_g, b_w_out,
              moe_w_group, moe_b_group, moe_w_expert, moe_b_expert, moe_w_gate, moe_w_up, moe_w_down):
    b = x.shape[0]
    m = mem.shape[1]
    mem_kv = rms_norm(mem, mem_norm_g) @ w_mem_kv
    mem_k = mem_kv[..., :MEM_W].reshape(b, m, MEM_HEADS, MEM_HEAD_DIM)
    mem_v = mem_kv[..., MEM_W:].reshape(b, m, MEM_HEADS, MEM_HEAD_DIM)
    for i in range(DEPTH):
        h = rms_norm(x, norm1_g[i])
        j = i // N_MIXERS
        if i % N_MIXERS == 0:
            mix, q_mem = gmlp_mixer(h, a_w_in[j], a_ln_g[j], a_ln_b[j], a_w_s[j], a_b_s[j])
            w_out = a_w_out[j]
        else:
            mix, q_mem = mla_mixer(h, positions, b_w_in[j], b_q_norm_g[j], b_kv_norm_g[j],
                                   b_w_q_up[j], b_w_kv_up[j], b_qn_g[j], b_kn_g[j])
            w_out = b_w_out[j]
        mem_out = memory_attention(q_mem, mem_k, mem_v, mem_qn_g[i], mem_kn_g[i])
        x = x + jnp.concatenate([mix, mem_out], axis=-1) @ w_out
        x = x + hierarchical_moe(rms_norm(x, norm2_g[i]), moe_w_group[i], moe_b_group[i],
                                 moe_w_expert[i], moe_b_expert[i], moe_w_gate[i],
                                 moe_w_up[i], moe_w_down[i])
    return x
```

```python
import contextlib
import numpy as np
import concourse.bass as bass
import concourse.mybir as mybir
from concourse.bass_utils import run_bass_kernel_spmd

F32 = mybir.dt.float32
BF16 = mybir.dt.bfloat16
I32 = mybir.dt.int32
AF = mybir.ActivationFunctionType
ALU = mybir.AluOpType
AX = mybir.AxisListType

D = 1024
EPS = 1e-6
NE = 32


class Buf:
    __slots__ = ("name", "w", "rl", "psum")

    def __init__(self, name="", psum=False):
        self.name = name
        self.w = None
        self.rl = []
        self.psum = psum


class Node:
    __slots__ = ("eng", "fn", "kw", "sync", "order", "dur", "occ", "isdma", "act_set")


def _fsize(ap):
    try:
        return int(ap.free_size())
    except Exception:
        return 512


class Ctx:
    NDSEM = 24
    import os as _os
    WINDOW = int(_os.environ.get('SCHEDW', '48'))

    def __init__(self, nc):
        self.nc = nc
        self.es = contextlib.ExitStack()
        self.engs = ("pe", "dve", "act", "pool", "sp")
        self.sems = {}
        for k in self.engs:
            self.sems[k] = self.es.enter_context(nc.semaphore("s_" + k))
        self.dsem = {}
        for q in ("sp", "pool", "act"):
            self.dsem[q] = [self.es.enter_context(nc.semaphore(f"d_{q}{i}")) for i in range(self.NDSEM)]
        self.nodes = []
        self.cp_segs = {}
        self.segs = [0]
        self.scopes = [self.es]
        self.nalloc = 0
        self.ninst = 0
        self.nwaits = 0

    def sb(self, name, shape, dt):
        self.nalloc += 1
        return self.scopes[-1].enter_context(self.nc.sbuf_tensor(f"{name}_{self.nalloc}", list(shape), dt))

    def ps(self, name, shape, dt=F32):
        return self.es.enter_context(self.nc.psum_tensor(name, list(shape), dt))

    @contextlib.contextmanager
    def scope(self):
        st = contextlib.ExitStack()
        self.scopes.append(st)
        try:
            yield
        finally:
            self.barrier()
            self.scopes.pop()
            st.close()

    def barrier(self):
        if self.segs[-1] != len(self.nodes):
            self.segs.append(len(self.nodes))

    def close(self):
        self.es.close()

    def _record(self, eng, fn, kw, reads, writes, isdma, dur, occ, act_set=None):
        nid = len(self.nodes)
        seg0 = self.segs[-1]
        n = Node()
        n.eng, n.fn, n.kw, n.isdma, n.dur, n.occ, n.act_set = eng, fn, kw, isdma, dur, occ, act_set
        sync, order = set(), set()
        nodes = self.nodes

        def dep(m):
            if m is None or m < seg0:
                return
            mn = nodes[m]
            if eng == "pe" and not isdma and not mn.isdma and mn.eng == "pe":
                order.add(m)
            else:
                sync.add(m)
        for b in reads:
            dep(b.w)
            if b.psum:
                for r in b.rl:
                    if nodes[r].eng != eng:
                        dep(r)
        for b in writes:
            dep(b.w)
            for r in b.rl:
                dep(r)
        n.sync, n.order = sync, order
        nodes.append(n)
        for b in reads:
            b.rl.append(nid)
        for b in writes:
            b.w = nid
            b.rl = []
        self.ninst += 1
        return nid

    def op(self, e, name, reads=(), writes=(), **kw):
        act_set = None
        if e == "pe":
            if name == "matmul":
                nn = _fsize(kw["rhs"])
                f = 4.0 if kw["rhs"].dtype == F32 else 1.0
            else:
                nn = 128
                f = 1.0
            dur = f * max(64, nn) / 2.4 + 8
        elif e == "act":
            dur = (_fsize(kw["out"]) + 200) / 1.2
            fnc = kw.get("func")
            if fnc in (AF.Exp, AF.Gelu, AF.Silu, AF.Sin):
                act_set = fnc
        elif e == "dve":
            nn = _fsize(kw["out"] if "out" in kw else kw["ap"])
            f = 8.0 if name == "reciprocal" else (2.0 if name in ("tensor_tensor", "scalar_tensor_tensor") else 1.0)
            dur = (f * nn + 110) / 0.96
        else:
            nn = _fsize(kw["out"] if "out" in kw else kw["ap"])
            dur = (2.0 * nn + 200) / 0.96 + 400
        self._record(e, name, kw, reads, writes, False, dur, dur, act_set)

    def dma(self, q, out, in_, reads=(), writes=(), **kw):
        kw = dict(kw)
        kw["out"] = out
        kw["in_"] = in_
        try:
            nb = int(out.nbytes())
        except Exception:
            nb = 1 << 16
        occ = 1000.0 if q == "pool" else 70.0
        self._record(q, "dma_start", kw, reads, writes, True, 2200.0 + nb / 120.0, occ)

    def idma(self, out, out_off, in_, in_off, reads=(), writes=(), **extra):
        kw = dict(out=out, out_offset=out_off, in_=in_, in_offset=in_off)
        kw.update(extra)
        self._record("pool", "indirect_dma_start", kw, reads, writes, True, 4000.0, 1500.0)

    def wait_all(self, e, bufs):
        pass

    def _schedule_segment(self, a, b, order_out):
        nodes = self.nodes
        W = self.WINDOW
        pend = {e: [] for e in self.engs}
        succ_eng = {}
        for i in range(a, b):
            pend[nodes[i].eng].append(i)
        for i in range(a, b):
            for d in nodes[i].sync | nodes[i].order:
                succ_eng.setdefault(d, set()).add(nodes[i].eng)
        cp = {}
        for i in range(b - 1, a - 1, -1):
            cp[i] = cp.get(i, 0.0) + nodes[i].dur
            for d in nodes[i].sync | nodes[i].order:
                if d >= a and cp[i] > cp.get(d, 0.0):
                    cp[d] = cp[i]
        head = {e: 0 for e in self.engs}
        done = set()
        finish = {}
        etime = {e: 0.0 for e in self.engs}
        last_act = [None]
        cache = {e: None for e in self.engs}
        dirty = set(self.engs)
        remaining = b - a
        while remaining:
            for e in dirty:
                lst = pend[e]
                h = head[e]
                while h < len(lst) and lst[h] in done:
                    h += 1
                head[e] = h
                best = None
                cnt = 0
                k = h
                while k < len(lst) and cnt < W:
                    i = lst[k]
                    k += 1
                    if i in done:
                        continue
                    cnt += 1
                    nd = nodes[i]
                    ok = True
                    st = etime[e]
                    for d in nd.sync:
                        if d not in done:
                            ok = False
                            break
                        t = finish[d] + 80.0
                        if t > st:
                            st = t
                    if not ok:
                        continue
                    for d in nd.order:
                        if d not in done:
                            ok = False
                            break
                    if not ok:
                        continue
                    if e == "act" and nd.act_set is not None and nd.act_set != last_act[0]:
                        st += 1300.0
                    key = (st, -cp[i] if self.cp_segs.get(a, False) else 0.0)
                    if best is None or key < best[2]:
                        best = (st, i, key)
                cache[e] = best
            dirty = set()
            pick = None
            for e in self.engs:
                c_ = cache[e]
                if c_ is not None and (pick is None or c_[0] < pick[0]):
                    pick = (c_[0], c_[1], e)
            st, i, e = pick
            nd = nodes[i]
            done.add(i)
            order_out[e].append(i)
            finish[i] = st + nd.dur
            etime[e] = st + nd.occ
            if e == "act" and nd.act_set is not None:
                last_act[0] = nd.act_set
            dirty.add(e)
            for se in succ_eng.get(i, ()):
                dirty.add(se)
            remaining -= 1

    def emit(self):
        nodes = self.nodes
        segs = self.segs + ([len(nodes)] if self.segs[-1] != len(nodes) else [])
        prog = {e: [] for e in self.engs}
        cnt = {e: 0 for e in self.engs}
        seen = {e: {} for e in self.engs}
        duse = {q: [0] * self.NDSEM for q in self.dsem}
        dn = {q: 0 for q in self.dsem}
        tok = {}

        def wait(e, key, val):
            if seen[e].get(key, 0) >= val:
                return
            semobj = self.dsem[key[0]][key[1]] if isinstance(key, tuple) else self.sems[key]
            prog[e].append((None, semobj, val))
            seen[e][key] = val
            self.nwaits += 1

        def full_barrier():
            for e in self.engs:
                for f in self.engs:
                    if f != e and cnt[f]:
                        wait(e, f, cnt[f])
                for q in self.dsem:
                    for slot in range(self.NDSEM):
                        if duse[q][slot]:
                            wait(e, (q, slot), 16 * duse[q][slot])

        for si in range(len(segs) - 1):
            a, b = segs[si], segs[si + 1]
            order = {e: [] for e in self.engs}
            self._schedule_segment(a, b, order)
            for e in self.engs:
                c0 = cnt[e]
                d0 = dn.get(e, 0)
                du = list(duse[e]) if e in duse else None
                for i in order[e]:
                    nd = nodes[i]
                    if nd.isdma:
                        slot = d0 % self.NDSEM
                        d0 += 1
                        du[slot] += 1
                        tok[i] = ((e, slot), 16 * du[slot])
                    else:
                        c0 += 1
                        tok[i] = (e, c0)
            for e in self.engs:
                for i in order[e]:
                    nd = nodes[i]
                    for d in nd.sync:
                        k_, v_ = tok[d]
                        wait(e, k_, v_)
                    if nd.isdma:
                        slot = dn[e] % self.NDSEM
                        dn[e] += 1
                        prev = duse[e][slot]
                        if prev:
                            wait(e, (e, slot), 16 * prev)
                        duse[e][slot] = prev + 1
                        prog[e].append(((nd.fn, nd.kw), self.dsem[e][slot], 16))
                    else:
                        cnt[e] += 1
                        prog[e].append(((nd.fn, nd.kw), self.sems[e], 1))
            full_barrier()

        def run(eng, items):
            regs = {}
            for fn, sem, v in items:
                if fn is None:
                    eng.wait_ge(sem, v)
                else:
                    kw = fn[1]
                    bc = kw.get("bounds_check")
                    if isinstance(bc, int):
                        if bc not in regs:
                            regs[bc] = eng.to_reg(bc)
                        kw = dict(kw)
                        kw["bounds_check"] = regs[bc]
                    getattr(eng, fn[0])(**kw).then_inc(sem, v)

        with self.nc.Block() as block:
            @block.tensor
            def _(e):
                run(e, prog["pe"])

            @block.vector
            def _(e):
                run(e, prog["dve"])

            @block.scalar
            def _(e):
                run(e, prog["act"])

            @block.gpsimd
            def _(e):
                run(e, prog["pool"])

            @block.sync
            def _(e):
                run(e, prog["sp"])


class Rot:
    def __init__(self, items):
        self.items = items
        self.i = 0

    def next(self):
        r = self.items[self.i % len(self.items)]
        self.i += 1
        return r


WEIGHT_NAMES = [
    ("mem_norm_g", [1024]), ("w_mem_kv", [1024, 1024]), ("mem_qn_g", [2, 128]), ("mem_kn_g", [2, 128]),
    ("norm1_g", [2, 1024]), ("norm2_g", [2, 1024]),
    ("a_w_in", [1, 1024, 2560]), ("a_ln_g", [1, 1024]), ("a_ln_b", [1, 1024]), ("a_w_s", [1, 8, 128, 128]),
    ("a_b_s", [1, 8, 128]), ("a_w_out", [1, 1536, 1024]),
    ("b_w_in", [1, 1024, 1344]), ("b_q_norm_g", [1, 512]), ("b_kv_norm_g", [1, 256]),
    ("b_w_q_up", [1, 512, 1536]), ("b_w_kv_up", [1, 256, 2048]), ("b_qn_g", [1, 192]), ("b_kn_g", [1, 192]),
    ("b_w_out", [1, 1536, 1024]),
    ("moe_w_group", [2, 1024, 4]), ("moe_b_group", [2, 4]), ("moe_w_expert", [2, 1024, 32]),
    ("moe_b_expert", [2, 32]),
    ("moe_wgL", [2, 4096, 2048]), ("moe_wuL", [2, 4096, 2048]), ("moe_wdL", [2, 4096, 2048]),
]


class Prog:
    def __init__(self, S, phases=("p0", "p1", "p2", "p3", "p4", "p5", "p6"), dbg=()):
        self.S = S
        self.NT = S // 512
        self.NCH = S // 128
        self.ST = min(2048, S)
        self.phases = phases
        nc = self.nc = bass.Bass("TRN2", target_bir_lowering=False)
        c = self.c = Ctx(nc)
        self.W = {}
        self.x = nc.dram_tensor("x", [S, D], F32, kind="ExternalInput").ap()
        self.mem = nc.dram_tensor("mem", [256, D], F32, kind="ExternalInput").ap()
        self.pos = nc.dram_tensor("positions", [S], I32, kind="ExternalInput").ap()
        self.invf = nc.dram_tensor("inv_freq", [32], F32, kind="ExternalInput").ap()
        for n, shp in WEIGHT_NAMES:
            self.W[n] = nc.dram_tensor(n, shp, F32, kind="ExternalInput").ap()
        self.out = nc.dram_tensor("out", [S, D], F32, kind="ExternalOutput").ap()
        self.db = {}
        self.dbg = dbg

        def scratch(name, shape, dt):
            kind = "ExternalOutput" if name in dbg else "Internal"
            return nc.dram_tensor(name, list(shape), dt, kind=kind).ap()

        self.xmid = [scratch("xmid0", [S, D], F32), scratch("xmid1", [S, D], F32)]
        self.x1 = scratch("x1", [S, D], F32)
        self.h2T = [scratch("h2T0", [8, 128, S], BF16), scratch("h2T1", [8, 128, S], BF16)]
        self.comb = [scratch("comb0", [S, NE], F32), scratch("comb1", [S, NE], F32)]
        self.NTILES = (2 * S) // 512 + 32
        self.NCAP = self.NTILES * 512
        self.rinfo = [scratch(f"rinfo{i}", [S, 66], F32) for i in range(2)]
        self.h2b = [scratch(f"h2b{i}", [S, D], BF16) for i in range(2)]
        xs_shared = scratch("Xs0", [self.NCAP, D], BF16)
        self.Xs = [xs_shared, xs_shared]
        self.Ys = [scratch(f"Ys{i}", [self.NCAP, D], BF16) for i in range(2)]
        self.QT = scratch("QT", [8, 192, S], BF16)
        self.KT = scratch("KT", [8, 192, S], BF16)
        self.Vd = scratch("Vd", [S, 1024], BF16)
        self.OT = scratch("OT", [8, 128, S], BF16)
        self.MO = scratch("MO", [4, 128, S], BF16)

        self.pmm = [(c.ps(f"pmm{i}", [128, 512], F32), Buf(f"pmm{i}", True)) for i in range(4)]
        self.pw = (c.ps("pw", [128, 1024], F32), Buf("pw", True))
        self.pT = (c.ps("pT", [128, 512], F32), Buf("pT", True))
        self.pmisc = (c.ps("pmisc", [128, 512], F32), Buf("pmisc", True))
        self.mmrot = Rot(self.pmm)

        self.setup_consts()
        self.zf = [[], []]
        for ph, fn in (("p0", self.p0_memkv), ("p1", self.p1_gmlp), ("p2", lambda: self.moe_sparse(0, self.xmid[0], self.x1)),
                       ("p3", self.p3_mla_proj), ("p4", self.p4_attn), ("p5", self.p5_out),
                       ("p6", lambda: self.moe_sparse(1, self.xmid[1], self.out))):
            if ph in phases:
                with c.scope():
                    fn()
        c.wait_all("sp", list(self.db.values()))
        c.emit()
        c.close()

    def dbuf(self, name, idx):
        k = (name, idx)
        if k not in self.db:
            self.db[k] = Buf(f"{name}{idx}")
        return self.db[k]

    def T(self, name, shape, dt):
        return (self.c.sb(name, shape, dt), Buf(name))

    def rsqrt_(self, t_ap, bt, scale, n):
        c = self.c
        c.op("pool", "tensor_scalar", reads=[bt], writes=[bt], out=t_ap, in0=t_ap, scalar1=scale, scalar2=EPS,
             op0=ALU.mult, op1=ALU.add)
        c.op("pool", "tensor_tensor", reads=[bt, self.mh[1]], writes=[bt], out=t_ap, in0=t_ap,
             in1=self.mh[0][:, 0:n], op=ALU.pow)

    def load_small_cols(self, name, src1d, k):
        t, b = self.T(name, [128, k], F32)
        self.c.dma("sp", t[:], src1d.rearrange("(k p) -> p k", p=128), writes=[b], allow_slow_non_contiguous=True)
        return t, b

    def load_bcast(self, name, src1d, n):
        t, b = self.T(name, [128, n], F32)
        self.c.dma("sp", t[:], src1d.partition_broadcast(128), writes=[b])
        return t, b

    def load_w_cast(self, dst, bdst, src2d, K):
        for k in range(K):
            self.c.dma("pool", dst[:, k, :], src2d[k * 128:(k + 1) * 128, :], writes=[bdst])

    def load_w_gain(self, dst, bdst, src2d, K, N, gain, bgain):
        c = self.c
        for k in range(K):
            st, bst = self.wstage.next()
            c.dma("sp", st[:, 0:N], src2d[k * 128:(k + 1) * 128, :], writes=[bst])
            c.op("dve", "tensor_scalar", reads=[bst, bgain], writes=[bdst], out=dst[:, k, :], in0=st[:, 0:N],
                 scalar1=gain[:, k:k + 1], scalar2=None, op0=ALU.mult)

    def setup_consts(self):
        c = self.c
        self.identb = self.T("identb", [128, 128], BF16)
        self.identf = self.T("identf", [128, 128], F32)
        self.onesb = self.T("onesb", [128, 128], BF16)
        self.mh = self.T("mh", [128, 16], F32)
        for t, b in (self.identb, self.identf):
            c.op("pool", "memset", writes=[b], ap=t[:], constant=0.0)
            c.op("pool", "affine_select", reads=[b], writes=[b], out=t[:], in_=t[:], pattern=[[-1, 128]],
                 compare_op=ALU.not_equal, fill=1.0, base=0, channel_multiplier=1)
        c.op("pool", "memset", writes=[self.onesb[1]], ap=self.onesb[0][:], constant=1.0)
        c.op("pool", "memset", writes=[self.mh[1]], ap=self.mh[0][:], constant=-0.5)
        self.kT = [self.T(f"kT{i}", [128, 4, 256], BF16) for i in range(2)]
        self.Vaug = self.T("Vaug", [128, 2, 4, 130], BF16)
        self.load_router_weights()

    def p0_memkv(self):
        c = self.c
        W = self.W
        g, bg = self.load_small_cols("memg", W["mem_norm_g"], 8)
        self.wstage = Rot([self.T(f"wstage{i}", [128, 2560], F32) for i in range(2)])
        wkv, bwkv = self.T("wkv", [128, 8, 1024], BF16)
        self.load_w_gain(wkv, bwkv, W["w_mem_kv"], 8, 1024, g, bg)
        gq, bgq = self.T("gq", [128, 2], F32)
        gk, bgk = self.T("gk", [128, 2], F32)
        c.dma("sp", gq[:], W["mem_qn_g"].rearrange("l d -> d l"), writes=[bgq], allow_slow_non_contiguous=True)
        c.dma("sp", gk[:], W["mem_kn_g"].rearrange("l d -> d l"), writes=[bgk], allow_slow_non_contiguous=True)
        c.op("dve", "scalar_tensor_tensor", reads=[bgq, bgk], writes=[bgq], out=gq[:], in0=gq[:],
             scalar=float(128 ** -0.5), in1=gk[:], op0=ALU.mult, op1=ALU.mult)
        mt, bmt = self.T("memt", [128, 2, 1024], F32)
        c.dma("sp", mt[:], self.mem.rearrange("(c p) d -> p c d", p=128), writes=[bmt])
        ms, bms = self.T("mems", [128, 1024], BF16)
        junk, bjunk = self.T("junk0", [128, 1024], BF16)
        ss, bss = self.T("ss0", [128, 2], F32)
        mT, bmT = self.T("memT", [128, 8, 256], BF16)
        pTv = self.pT[0][:].bitcast(BF16)
        bpT = self.pT[1]
        for ch in range(2):
            c.op("act", "activation", reads=[bmt], writes=[bjunk, bss], out=junk[:], in_=mt[:, ch, :], func=AF.Square,
                 accum_out=ss[:, ch:ch + 1])
        self.rsqrt_(ss[:], bss, 1.0 / D, 2)
        for ch in range(2):
            c.op("dve", "tensor_scalar", reads=[bmt, bss], writes=[bms], out=ms[:], in0=mt[:, ch, :],
                 scalar1=ss[:, ch:ch + 1], scalar2=None, op0=ALU.mult)
            for k in range(8):
                c.op("pe", "transpose", reads=[bms, self.identb[1]], writes=[bpT], out=pTv[:, k * 128:(k + 1) * 128],
                     in_=ms[:, k * 128:(k + 1) * 128], identity=self.identb[0][:])
            c.op("act", "copy", reads=[bpT], writes=[bmT], out=mT[:, :, ch * 128:(ch + 1) * 128],
                 in_=pTv.rearrange("p (k t) -> p k t", k=8))
        c.op("pool", "memset", writes=[self.Vaug[1]], ap=self.Vaug[0][:, :, :, 128:130], constant=1.0)
        kss, bkss = self.T("kss", [128, 4], F32)
        kn, bkn = self.T("kn", [128, 4, 128], BF16)
        for ch in range(2):
            pk, bpk = self.mmrot.next()
            for k in range(8):
                c.op("pe", "matmul", reads=[bmT, bwkv], writes=[bpk], out=pk[:], lhsT=mT[:, k, ch * 128:(ch + 1) * 128],
                     rhs=wkv[:, k, 0:512], start=(k == 0), stop=(k == 7))
            for h in range(4):
                c.op("act", "activation", reads=[bpk], writes=[bjunk, bkss], out=junk[:, 0:128], in_=pk[:, h * 128:(h + 1) * 128],
                     func=AF.Square, accum_out=kss[:, h:h + 1])
            self.rsqrt_(kss[:], bkss, 1.0 / 128, 4)
            c.op("dve", "tensor_tensor", reads=[bpk, bkss], writes=[bkn], out=kn[:],
                 in0=pk[:].rearrange("p (h d) -> p h d", h=4), in1=kss[:].unsqueeze(2).to_broadcast([128, 4, 128]), op=ALU.mult)
            for h in range(4):
                c.op("pe", "transpose", reads=[bkn, self.identb[1]], writes=[bpT], out=pTv[:, h * 128:(h + 1) * 128],
                     in_=kn[:, h, :], identity=self.identb[0][:])
            for li in range(2):
                c.op("dve", "tensor_scalar", reads=[bpT, bgq], writes=[self.kT[li][1]],
                     out=self.kT[li][0][:, :, ch * 128:(ch + 1) * 128], in0=pTv[:, 0:512].rearrange("p (h m) -> p h m", h=4),
                     scalar1=gq[:, li:li + 1], scalar2=None, op0=ALU.mult)
            pv, bpv = self.mmrot.next()
            for k in range(8):
                c.op("pe", "matmul", reads=[bmT, bwkv], writes=[bpv], out=pv[:], lhsT=mT[:, k, ch * 128:(ch + 1) * 128],
                     rhs=wkv[:, k, 512:1024], start=(k == 0), stop=(k == 7))
            c.op("act", "copy", reads=[bpv], writes=[self.Vaug[1]], out=self.Vaug[0][:, ch, :, 0:128],
                 in_=pv[:].rearrange("p (h d) -> p h d", h=4))

    def alloc_tile_bufs(self, full=True, deep=False):
        self.xt = Rot([self.T(f"xt{i}", [128, 4, 1024], F32) for i in range((3 if deep else 2) if full else 1)])
        self.hTrot = Rot([self.T(f"hT{i}", [128, 8, 512], BF16) for i in range(2)])
        self.hT = self.hTrot.items[0]
        self.xs = Rot([self.T(f"xs{i}", [128, 1024], BF16) for i in range(2)])
        self.junk = self.T("junk", [128, 1024], BF16)
        self.ssn = self.T("ssn", [128, 4], F32)
        self.qT = self.T("qT", [128, 4, 512], BF16)
        self.qn = Rot([self.T(f"qn{i}", [128, 4, 128], BF16) for i in range(2)])
        self.qss = Rot([self.T(f"qss{i}", [128, 4], F32) for i in range(2)])
        self.PTm = Rot([self.T(f"PTm{i}", [128, 512], BF16) for i in range(3)])
        self.mrd = Rot([self.T(f"mrd{i}", [128, 4], F32) for i in range(2)])
        self.mon = Rot([self.T(f"mon{i}", [128, 4, 128], BF16) for i in range(2)])
        if not full:
            return
        self.catTr = Rot([self.T(f"catT{i}", [128, 12, 512], BF16) for i in range(2 if deep else 1)])
        self.catT = self.catTr.items[0]
        self.h2 = Rot([self.T(f"h2_{i}", [128, 1024], F32) for i in range(2 if deep else 1)])
        self.h2Tf = Rot([self.T(f"h2Tf{i}", [128, 8, 128], F32) for i in range(2 if deep else 1)])
        self.h2bt = Rot([self.T(f"h2bt{i}", [128, 1024], BF16) for i in range(2)])
        self.rt = Rot([dict((n, self.T(f"rt_{n}{i}", [128, w], F32)) for n, w in
                            (("lg", 36), ("gmax", 1), ("ngmax", 1), ("gexp", 4), ("gsum", 1), ("oh", 4), ("tmp", 32),
                             ("esel", 8), ("m1", 1), ("nm1", 1), ("sel1", 8), ("es2", 8), ("m2", 1), ("sel2", 8),
                             ("p2", 1), ("w1", 1), ("w2", 1), ("R", 66))) for i in range(4)])

    def norm_to_hT(self, xt, bxt):
        c = self.c
        ss, bss = self.ssn
        junk, bjunk = self.junk
        self.hT = self.hTrot.next()
        hT, bhT = self.hT
        pTv = self.pT[0][:].bitcast(BF16)
        bpT = self.pT[1]
        for ch in range(4):
            c.op("act", "activation", reads=[bxt], writes=[bjunk, bss], out=junk[:], in_=xt[:, ch, :], func=AF.Square,
                 accum_out=ss[:, ch:ch + 1])
        self.rsqrt_(ss[:], bss, 1.0 / D, 4)
        for ch in range(4):
            xs, bxs = self.xs.next()
            c.op("dve", "tensor_scalar", reads=[bxt, bss], writes=[bxs], out=xs[:], in0=xt[:, ch, :],
                 scalar1=ss[:, ch:ch + 1], scalar2=None, op0=ALU.mult)
            for k in range(8):
                c.op("pe", "transpose", reads=[bxs, self.identb[1]], writes=[bpT], out=pTv[:, k * 128:(k + 1) * 128],
                     in_=xs[:, k * 128:(k + 1) * 128], identity=self.identb[0][:])
            c.op("act", "copy", reads=[bpT], writes=[bhT], out=hT[:, :, ch * 128:(ch + 1) * 128],
                 in_=pTv.rearrange("p (k t) -> p k t", k=8))

    def qmem_and_attn(self, li, win, bwin, col0, dst, bdst, dst_k0):
        c = self.c
        hT, bhT = self.hT
        qT, bqT = self.qT
        junk, bjunk = self.junk
        pTv = self.pT[0][:].bitcast(BF16)
        bpT = self.pT[1]
        for ch in range(4):
            pq, bpq = self.mmrot.next()
            for k in range(8):
                c.op("pe", "matmul", reads=[bhT, bwin], writes=[bpq], out=pq[:], lhsT=hT[:, k, ch * 128:(ch + 1) * 128],
                     rhs=win[:, k, col0:col0 + 512], start=(k == 0), stop=(k == 7))
            qss, bqss = self.qss.next()
            for h in range(4):
                c.op("act", "activation", reads=[bpq], writes=[bjunk, bqss], out=junk[:, 0:128], in_=pq[:, h * 128:(h + 1) * 128],
                     func=AF.Square, accum_out=qss[:, h:h + 1])
            self.rsqrt_(qss[:], bqss, 1.0 / 128, 4)
            qn, bqn = self.qn.next()
            c.op("dve", "tensor_tensor", reads=[bpq, bqss], writes=[bqn], out=qn[:],
                 in0=pq[:].rearrange("p (h d) -> p h d", h=4), in1=qss[:].unsqueeze(2).to_broadcast([128, 4, 128]), op=ALU.mult)
            for h in range(4):
                c.op("pe", "transpose", reads=[bqn, self.identb[1]], writes=[bpT], out=pTv[:, h * 128:(h + 1) * 128],
                     in_=qn[:, h, :], identity=self.identb[0][:])
            c.op("act", "copy", reads=[bpT], writes=[bqT], out=qT[:, :, ch * 128:(ch + 1) * 128],
                 in_=pTv[:, 0:512].rearrange("p (h t) -> p h t", h=4))
        kT, bkT = self.kT[li]
        Va, bVa = self.Vaug
        for h in range(4):
            pts = []
            for mc in range(2):
                psc, bpsc = self.mmrot.next()
                c.op("pe", "matmul", reads=[bkT, bqT], writes=[bpsc], out=psc[:], lhsT=kT[:, h, mc * 128:(mc + 1) * 128],
                     rhs=qT[:, h, :], start=True, stop=True)
                PT, bPT = self.PTm.next()
                c.op("act", "activation", reads=[bpsc], writes=[bPT], out=PT[:], in_=psc[:], func=AF.Exp)
                pts.append((PT, bPT))
            banks = [self.mmrot.next(), self.mmrot.next()]
            for qc in range(4):
                ob, bob = banks[qc // 2]
                col = (qc % 2) * 130
                for mc in range(2):
                    c.op("pe", "matmul", reads=[bVa, pts[mc][1]], writes=[bob], out=ob[:, col:col + 130],
                         lhsT=pts[mc][0][:, qc * 128:(qc + 1) * 128], rhs=Va[:, mc, h, :], start=(mc == 0 and qc % 2 == 0),
                         stop=(mc == 1), skip_group_check=True)
            rd, brd = self.mrd.next()
            on, bon = self.mon.next()
            for qc in range(4):
                ob, bob = banks[qc // 2]
                col = (qc % 2) * 130
                c.op("dve", "reciprocal", reads=[bob], writes=[brd], out=rd[:, qc:qc + 1], in_=ob[:, col + 128:col + 129])
                c.op("dve", "tensor_scalar", reads=[bob, brd], writes=[bon], out=on[:, qc, :], in0=ob[:, col:col + 128],
                     scalar1=rd[:, qc:qc + 1], scalar2=None, op0=ALU.mult)
                c.op("pe", "transpose", reads=[bon, self.identb[1]], writes=[bpT], out=pTv[:, qc * 128:(qc + 1) * 128],
                     in_=on[:, qc, :], identity=self.identb[0][:])
            c.op("act", "copy", reads=[bpT], writes=[bdst], out=dst[:, dst_k0 + h, :], in_=pTv[:, 0:512])

    def out_proj_norm2_router(self, li, j, xt, bxt, wout, bwout):
        c = self.c
        catT, bcat = self.catT
        S = self.S
        junk, bjunk = self.junk
        for ch in range(4):
            for half in range(2):
                py, bpy = self.mmrot.next()
                for k in range(12):
                    c.op("pe", "matmul", reads=[bcat, bwout], writes=[bpy], out=py[:], lhsT=catT[:, k, ch * 128:(ch + 1) * 128],
                         rhs=wout[:, k, half * 512:(half + 1) * 512], start=(k == 0), stop=(k == 11))
                c.op("dve", "tensor_tensor", reads=[bpy, bxt], writes=[bxt], out=xt[:, ch, half * 512:(half + 1) * 512],
                     in0=py[:], in1=xt[:, ch, half * 512:(half + 1) * 512], op=ALU.add)
        c.dma("sp", self.xmid[li][j * 512:(j + 1) * 512, :].rearrange("(c p) d -> p c d", p=128), xt[:], reads=[bxt],
              writes=[self.dbuf(f"xmid{li}", j)])
        import os
        STG = float(os.environ.get("STG", "99"))
        if STG < 6:
            return
        ss, bss = self.ssn
        for ch in range(4):
            c.op("act", "activation", reads=[bxt], writes=[bjunk, bss], out=junk[:], in_=xt[:, ch, :], func=AF.Square,
                 accum_out=ss[:, ch:ch + 1])
        self.rsqrt_(ss[:], bss, 1.0 / D, 4)
        g2, bg2 = self.g2bc[li]
        wr, bwr = self.wr[li]
        rb, brb = self.rbias[li]
        pw, bpw = self.pw
        for ch in range(4):
            h2, bh2 = self.h2.next()
            c.op("dve", "scalar_tensor_tensor", reads=[bxt, bss, bg2], writes=[bh2], out=h2[:], in0=xt[:, ch, :],
                 scalar=ss[:, ch:ch + 1], in1=g2[:], op0=ALU.mult, op1=ALU.mult)
            if STG < 6.2:
                continue
            for k in range(8):
                c.op("pe", "transpose", reads=[bh2, self.identf[1]], writes=[bpw], out=pw[:, k * 128:(k + 1) * 128],
                     in_=h2[:, k * 128:(k + 1) * 128], identity=self.identf[0][:])
            if STG < 6.4:
                continue
            h2Tf, bh2Tf = self.h2Tf.next()
            c.op("act", "copy", reads=[bpw], writes=[bh2Tf], out=h2Tf[:], in_=pw[:].rearrange("p (k t) -> p k t", k=8))
            hb, bhb = self.h2bt.next()
            c.op("pool", "tensor_copy", reads=[bh2], writes=[bhb], out=hb[:], in_=h2[:])
            gch_ = j * 4 + ch
            c.dma("sp", self.h2b[li][gch_ * 128:(gch_ + 1) * 128, :], hb[:], reads=[bhb], writes=[self.dbuf(f"h2b{li}", gch_)])
            if STG < 7:
                continue
            pl, bpl = self.pmisc
            for k in range(8):
                c.op("pe", "matmul", reads=[bh2Tf, bwr], writes=[bpl], out=pl[:, 0:36], lhsT=h2Tf[:, k, :], rhs=wr[:, k, :],
                     start=(k == 0), stop=(k == 7))
            if STG < 8:
                continue
            self.router(li, j * 4 + ch, pl, bpl, rb, brb)

    def router(self, li, gch, pl, bpl, rb, brb):
        c = self.c
        R = self.rt.next()

        def t(n):
            return R[n][0]

        def b(n):
            return R[n][1]
        c.op("dve", "tensor_tensor", reads=[bpl, brb], writes=[b("lg")], out=t("lg")[:], in0=pl[:, 0:36], in1=rb[:], op=ALU.add)
        gl = t("lg")[:, 0:4]
        el = t("lg")[:, 4:36]
        c.op("dve", "tensor_reduce", reads=[b("lg")], writes=[b("gmax")], out=t("gmax")[:], in_=gl, axis=AX.X, op=ALU.max)
        c.op("dve", "tensor_scalar", reads=[b("gmax")], writes=[b("ngmax")], out=t("ngmax")[:], in0=t("gmax")[:], scalar1=-1.0,
             scalar2=None, op0=ALU.mult)
        c.op("act", "activation", reads=[b("lg"), b("ngmax")], writes=[b("gexp"), b("gsum")], out=t("gexp")[:], in_=gl,
             func=AF.Exp, bias=t("ngmax")[:], accum_out=t("gsum")[:])
        c.op("dve", "reciprocal", reads=[b("gsum")], writes=[b("gsum")], out=t("gsum")[:], in_=t("gsum")[:])
        c.op("dve", "tensor_scalar", reads=[b("lg"), b("gmax")], writes=[b("oh")], out=t("oh")[:], in0=gl, scalar1=t("gmax")[:],
             scalar2=None, op0=ALU.is_equal)
        c.op("dve", "tensor_tensor", reads=[b("lg"), b("oh")], writes=[b("tmp")], out=t("tmp")[:].rearrange("p (g e) -> p g e", g=4),
             in0=el.rearrange("p (g e) -> p g e", g=4), in1=t("oh")[:].unsqueeze(2).to_broadcast([128, 4, 8]), op=ALU.mult)
        c.op("dve", "tensor_reduce", reads=[b("tmp")], writes=[b("esel")], out=t("esel")[:],
             in_=t("tmp")[:].rearrange("p (g e) -> p e g", g=4), axis=AX.X, op=ALU.add)
        c.op("dve", "tensor_reduce", reads=[b("esel")], writes=[b("m1")], out=t("m1")[:], in_=t("esel")[:], axis=AX.X, op=ALU.max)
        c.op("dve", "tensor_scalar", reads=[b("esel"), b("m1")], writes=[b("sel1")], out=t("sel1")[:], in0=t("esel")[:],
             scalar1=t("m1")[:], scalar2=None, op0=ALU.is_equal)
        c.op("dve", "scalar_tensor_tensor", reads=[b("sel1"), b("esel")], writes=[b("es2")], out=t("es2")[:], in0=t("sel1")[:],
             scalar=-1e30, in1=t("esel")[:], op0=ALU.mult, op1=ALU.add)
        c.op("dve", "tensor_reduce", reads=[b("es2")], writes=[b("m2")], out=t("m2")[:], in_=t("es2")[:], axis=AX.X, op=ALU.max)
        c.op("dve", "tensor_scalar", reads=[b("es2"), b("m2")], writes=[b("sel2")], out=t("sel2")[:], in0=t("es2")[:],
             scalar1=t("m2")[:], scalar2=None, op0=ALU.is_equal)
        c.op("dve", "tensor_scalar", reads=[b("m1")], writes=[b("nm1")], out=t("nm1")[:], in0=t("m1")[:], scalar1=-1.0,
             scalar2=None, op0=ALU.mult)
        c.op("act", "activation", reads=[b("m2"), b("nm1")], writes=[b("p2")], out=t("p2")[:], in_=t("m2")[:], func=AF.Exp,
             bias=t("nm1")[:])
        c.op("dve", "tensor_scalar", reads=[b("p2")], writes=[b("w1")], out=t("w1")[:], in0=t("p2")[:], scalar1=1.0, scalar2=None,
             op0=ALU.add)
        c.op("dve", "reciprocal", reads=[b("w1")], writes=[b("w1")], out=t("w1")[:], in_=t("w1")[:])
        c.op("dve", "tensor_tensor", reads=[b("w1"), b("gsum")], writes=[b("w1")], out=t("w1")[:], in0=t("w1")[:], in1=t("gsum")[:],
             op=ALU.mult)
        c.op("dve", "tensor_tensor", reads=[b("w1"), b("p2")], writes=[b("w2")], out=t("w2")[:], in0=t("w1")[:], in1=t("p2")[:],
             op=ALU.mult)
        ohb = t("oh")[:].unsqueeze(2).to_broadcast([128, 4, 8])
        c.op("dve", "tensor_tensor", reads=[b("oh"), b("sel1")], writes=[b("R")], out=t("R")[:, 0:32].rearrange("p (g e) -> p g e", g=4),
             in0=ohb, in1=t("sel1")[:].unsqueeze(1).to_broadcast([128, 4, 8]), op=ALU.mult)
        c.op("dve", "tensor_tensor", reads=[b("oh"), b("sel2")], writes=[b("R")], out=t("R")[:, 32:64].rearrange("p (g e) -> p g e", g=4),
             in0=ohb, in1=t("sel2")[:].unsqueeze(1).to_broadcast([128, 4, 8]), op=ALU.mult)
        c.op("dve", "tensor_copy", reads=[b("w1")], writes=[b("R")], out=t("R")[:, 64:65], in_=t("w1")[:])
        c.op("dve", "tensor_copy", reads=[b("w2")], writes=[b("R")], out=t("R")[:, 65:66], in_=t("w2")[:])
        c.dma("sp", self.rinfo[li][gch * 128:(gch + 1) * 128, :], t("R")[:], reads=[b("R")], writes=[self.dbuf(f"rinfo{li}", gch)])

    def load_router_weights(self):
        if hasattr(self, "g2bc"):
            return
        c = self.c
        W = self.W
        self.g2bc, self.wr, self.rbias = [], [], []
        for li in range(2):
            self.g2bc.append(self.load_bcast(f"g2bc{li}", W["norm2_g"][li], 1024))
            wr, bwr = self.T(f"wr{li}", [128, 8, 36], F32)
            c.dma("sp", wr[:, :, 0:4], W["moe_w_group"][li].rearrange("(k p) n -> p k n", p=128), writes=[bwr])
            c.dma("sp", wr[:, :, 4:36], W["moe_w_expert"][li].rearrange("(k p) n -> p k n", p=128), writes=[bwr])
            self.wr.append((wr, bwr))
            rb, brb = self.T(f"rbias{li}", [128, 36], F32)
            c.dma("sp", rb[:, 0:4], W["moe_b_group"][li].partition_broadcast(128), writes=[brb])
            c.dma("sp", rb[:, 4:36], W["moe_b_expert"][li].partition_broadcast(128), writes=[brb])
            self.rbias.append((rb, brb))

    def p1_gmlp(self):
        c = self.c
        W = self.W
        win, bwin = self.T("a_win", [128, 8, 2560], BF16)
        wout, bwout = self.T("a_wout", [128, 12, 1024], BF16)
        WsT, bWsT = self.T("WsT", [128, 8, 128], BF16)
        Cg, bCg = self.T("Cg", [128, 8, 128], F32)
        lng, blng = self.load_small_cols("lng", W["a_ln_g"][0], 8)
        self.load_w_cast(wout, bwout, W["a_w_out"][0], 12)
        with c.scope():
            self.p1_setup(win, bwin, WsT, bWsT, Cg, bCg)
        self.alloc_tile_bufs()
        uT, buT = self.T("uT", [128, 8, 512], BF16)
        vrot = Rot([self.T(f"v{i}", [128, 1024], F32) for i in range(1)])
        vnrot = Rot([self.T(f"vn{i}", [128, 1024], BF16) for i in range(2)])
        strot = Rot([self.T(f"bnst{i}", [128, 2, 6], F32) for i in range(2)])
        mvrot = Rot([self.T(f"mv{i}", [128, 2], F32) for i in range(2)])
        gtrot = Rot([self.T(f"gt{i}", [128, 8, 128], F32) for i in range(1)])
        self.zero_fill_xs([bwin, bwout])
        self.p1_main(win, bwin, wout, bwout, WsT, bWsT, Cg, bCg, lng, blng, uT, buT, vrot, vnrot, strot, mvrot, gtrot)

    def p1_setup(self, win, bwin, WsT, bWsT, Cg, bCg):
        c = self.c
        W = self.W
        self.wstage = Rot([self.T(f"wstage{i}", [128, 2560], F32) for i in range(2)])
        n1, bn1 = self.load_small_cols("n1g0", W["norm1_g"][0], 8)
        self.load_w_gain(win, bwin, W["a_w_in"][0], 8, 2560, n1, bn1)
        wsf, bwsf = self.T("wsf", [128, 8, 128], F32)
        c.dma("sp", wsf[:], W["a_w_s"][0].rearrange("g t s -> t g s"), writes=[bwsf])
        for g in range(8):
            c.op("pool", "affine_select", reads=[bwsf], writes=[bwsf], out=wsf[:, g, :], in_=wsf[:, g, :], pattern=[[-1, 128]],
                 compare_op=ALU.is_ge, fill=0.0, base=0, channel_multiplier=1)
        wsb, bwsb = self.T("wsb", [128, 8, 128], BF16)
        c.op("dve", "tensor_copy", reads=[bwsf], writes=[bwsb], out=wsb[:], in_=wsf[:])
        pTv = self.pT[0][:].bitcast(BF16)
        bpT = self.pT[1]
        for g in range(8):
            c.op("pe", "transpose", reads=[bwsb, self.identb[1]], writes=[bpT], out=pTv[:, g * 128:(g + 1) * 128],
                 in_=wsb[:, g, :], identity=self.identb[0][:])
        c.op("act", "copy", reads=[bpT], writes=[bWsT], out=WsT[:], in_=pTv.rearrange("p (g t) -> p g t", g=8))
        betab, bbetab = self.T("betab", [128, 1024], F32)
        c.dma("sp", betab[:], W["a_ln_b"][0].partition_broadcast(128), writes=[bbetab])
        betabb, bbetabb = self.T("betabb", [128, 1024], BF16)
        c.op("dve", "tensor_copy", reads=[bbetab], writes=[bbetabb], out=betabb[:], in_=betab[:])
        bsr, bbsr = self.T("bsr", [1, 1024], F32)
        c.dma("sp", bsr[:], W["a_b_s"][0].rearrange("g t -> (g t)").unsqueeze(0), writes=[bbsr])
        bsrb, bbsrb = self.T("bsrb", [1, 1024], BF16)
        c.op("dve", "tensor_copy", reads=[bbsr], writes=[bbsrb], out=bsrb[:], in_=bsr[:])
        pw, bpw = self.pw
        for g in range(8):
            c.op("pe", "matmul", reads=[bbetabb, bWsT], writes=[bpw], out=pw[:, g * 128:(g + 1) * 128],
                 lhsT=betabb[:, g * 128:(g + 1) * 128], rhs=WsT[:, g, :], start=True, stop=False)
            c.op("pe", "matmul", reads=[self.onesb[1], bbsrb], writes=[bpw], out=pw[:, g * 128:(g + 1) * 128],
                 lhsT=self.onesb[0][0:1, :], rhs=bsrb[0:1, g * 128:(g + 1) * 128], start=False, stop=True)
        c.op("act", "copy", reads=[bpw], writes=[bCg], out=Cg[:], in_=pw[:].rearrange("p (g t) -> p g t", g=8))

    def p1_main(self, win, bwin, wout, bwout, WsT, bWsT, Cg, bCg, lng, blng, uT, buT, vrot, vnrot, strot, mvrot, gtrot):
        c = self.c
        import os
        STG = float(os.environ.get("STG", "99"))
        if STG < 1:
            return
        pw, bpw = self.pw
        catT, bcat = self.catT
        for j in range(self.NT):
            xt, bxt = self.xt.next()
            c.dma("sp", xt[:], self.x[j * 512:(j + 1) * 512, :].rearrange("(c p) d -> p c d", p=128), writes=[bxt])
            self.norm_to_hT(xt, bxt)
            hT, bhT = self.hT
            if STG < 2:
                continue
            for n in range(8):
                pu, bpu = self.mmrot.next()
                for k in range(8):
                    c.op("pe", "matmul", reads=[bwin, bhT], writes=[bpu], out=pu[:], lhsT=win[:, k, n * 128:(n + 1) * 128],
                         rhs=hT[:, k, :], start=(k == 0), stop=(k == 7))
                c.op("act", "activation", reads=[bpu], writes=[buT], out=uT[:, n, :], in_=pu[:], func=AF.Gelu)
            if STG < 3:
                continue
            for ch in range(4):
                v, bv = vrot.next()
                for half in range(2):
                    pv, bpv = self.mmrot.next()
                    for k in range(8):
                        c.op("pe", "matmul", reads=[bhT, bwin], writes=[bpv], out=pv[:], lhsT=hT[:, k, ch * 128:(ch + 1) * 128],
                             rhs=win[:, k, 1024 + half * 512:1024 + (half + 1) * 512], start=(k == 0), stop=(k == 7))
                    c.op("act", "activation", reads=[bpv], writes=[bv], out=v[:, half * 512:(half + 1) * 512], in_=pv[:], func=AF.Gelu)
                st, bst = strot.next()
                mv, bmv = mvrot.next()
                for half in range(2):
                    c.op("dve", "bn_stats", reads=[bv], writes=[bst], out=st[:, half, :], in_=v[:, half * 512:(half + 1) * 512])
                c.op("dve", "bn_aggr", reads=[bst], writes=[bmv], out=mv[:], in_=st[:].rearrange("p a b -> p (a b)"))
                self.rsqrt_(mv[:, 1:2], bmv, 1.0, 1)
                vn, bvn = vnrot.next()
                c.op("dve", "tensor_scalar", reads=[bv, bmv], writes=[bvn], out=vn[:], in0=v[:], scalar1=mv[:, 0:1], scalar2=mv[:, 1:2],
                     op0=ALU.subtract, op1=ALU.mult)
                for g in range(8):
                    c.op("pe", "matmul", reads=[bvn, bWsT], writes=[bpw], out=pw[:, g * 128:(g + 1) * 128],
                         lhsT=vn[:, g * 128:(g + 1) * 128], rhs=WsT[:, g, :], start=True, stop=True)
                gt, bgt = gtrot.next()
                for g in range(8):
                    c.op("dve", "scalar_tensor_tensor", reads=[bpw, blng, bCg], writes=[bgt], out=gt[:, g, :],
                         in0=pw[:, g * 128:(g + 1) * 128], scalar=lng[:, g:g + 1], in1=Cg[:, g, :], op0=ALU.mult, op1=ALU.add)
                c.op("pool", "tensor_tensor", reads=[bgt, buT], writes=[bcat], out=catT[:, 0:8, ch * 128:(ch + 1) * 128],
                     in0=gt[:], in1=uT[:, :, ch * 128:(ch + 1) * 128], op=ALU.mult)
            if STG < 4:
                continue
            self.qmem_and_attn(0, win, bwin, 2048, catT, bcat, 8)
            if STG < 5:
                continue
            self.out_proj_norm2_router(0, j, xt, bxt, wout, bwout)

    def moe(self, li, xin, xout):
        c = self.c
        W = self.W
        S, ST = self.S, self.ST
        nst = S // ST
        ncs = ST // 128
        nts = ST // 512
        if True:
            self.moe_bufs = dict(
                xacc=self.T("xacc", [128, ncs, 1024], F32),
                h2s=self.T("h2s", [128, 8, ST], BF16),
                cmb=self.T("cmb", [128, ncs, NE], F32),
                wg=Rot([self.T(f"wg{i}", [128, 8, 256], BF16) for i in range(2)]),
                wu=Rot([self.T(f"wu{i}", [128, 8, 256], BF16) for i in range(2)]),
                wd=Rot([self.T(f"wd{i}", [128, 2, 1024], BF16) for i in range(2)]),
                sg=Rot([self.T(f"sg{i}", [128, 512], BF16) for i in range(2)]),
                a=Rot([self.T(f"a{i}", [128, 512], BF16) for i in range(4)]),
            )
        mb = self.moe_bufs
        xacc, bxacc = mb["xacc"]
        h2s, bh2s = mb["h2s"]
        cmb, bcmb = mb["cmb"]
        pg = [self.pmm[0], self.pmm[1]]
        pu = [self.pmm[2], self.pmm[3]]
        yrot = Rot([(self.pw[0][:, 0:512], self.pw[1]), (self.pT[0][:], self.pT[1]), (self.pmisc[0][:], self.pmisc[1])])
        for st in range(nst):
            r0 = st * ST
            tiles = range(st * nts, (st + 1) * nts)
            c.dma("sp", xacc[:], xin[r0:r0 + ST, :].rearrange("(c p) d -> p c d", p=128),
                  reads=[self.dbuf(f"xmid{li}", t) for t in tiles], writes=[bxacc])
            c.dma("sp", h2s[:], self.h2T[li][:, :, r0:r0 + ST].rearrange("k p t -> p k t"),
                  reads=[self.dbuf(f"h2T{li}", t) for t in tiles], writes=[bh2s])
            c.dma("sp", cmb[:], self.comb[li][r0:r0 + ST, :].rearrange("(c p) e -> p c e", p=128),
                  reads=[self.dbuf(f"comb{li}", ch) for ch in range(st * ncs, (st + 1) * ncs)], writes=[bcmb])
            for e in range(NE):
                wg, bwg = mb["wg"].next()
                wu, bwu = mb["wu"].next()
                wd, bwd = mb["wd"].next()
                c.dma("pool", wg[:], W["moe_w_gate"][li, e].rearrange("(k p) f -> p k f", p=128), writes=[bwg])
                c.dma("pool", wu[:], W["moe_w_up"][li, e].rearrange("(k p) f -> p k f", p=128), writes=[bwu])
                c.dma("pool", wd[:], W["moe_w_down"][li, e].rearrange("(k p) n -> p k n", p=128), writes=[bwd])
                for t in range(nts):
                    acts = []
                    for fc in range(2):
                        g_, bg_ = pg[fc]
                        u_, bu_ = pu[fc]
                        for k in range(8):
                            c.op("pe", "matmul", reads=[bwg, bh2s], writes=[bg_], out=g_[:], lhsT=wg[:, k, fc * 128:(fc + 1) * 128],
                                 rhs=h2s[:, k, t * 512:(t + 1) * 512], start=(k == 0), stop=(k == 7))
                        for k in range(8):
                            c.op("pe", "matmul", reads=[bwu, bh2s], writes=[bu_], out=u_[:], lhsT=wu[:, k, fc * 128:(fc + 1) * 128],
                                 rhs=h2s[:, k, t * 512:(t + 1) * 512], start=(k == 0), stop=(k == 7))
                        sg, bsg = mb["sg"].next()
                        c.op("act", "activation", reads=[bg_], writes=[bsg], out=sg[:], in_=g_[:], func=AF.Silu)
                        a, ba = mb["a"].next()
                        c.op("dve", "tensor_tensor", reads=[bsg, bu_], writes=[ba], out=a[:], in0=sg[:], in1=u_[:], op=ALU.mult)
                        acts.append((a, ba))
                    for ch in range(4):
                        gc = t * 4 + ch
                        for half in range(2):
                            py, bpy = yrot.next()
                            for fc in range(2):
                                c.op("pe", "matmul", reads=[acts[fc][1], bwd], writes=[bpy], out=py,
                                     lhsT=acts[fc][0][:, ch * 128:(ch + 1) * 128], rhs=wd[:, fc, half * 512:(half + 1) * 512],
                                     start=(fc == 0), stop=(fc == 1))
                            c.op("dve", "scalar_tensor_tensor", reads=[bpy, bcmb, bxacc], writes=[bxacc],
                                 out=xacc[:, gc, half * 512:(half + 1) * 512], in0=py, scalar=cmb[:, gc, e:e + 1],
                                 in1=xacc[:, gc, half * 512:(half + 1) * 512], op0=ALU.mult, op1=ALU.add)
            oname = "x1" if li == 0 else "out"
            c.dma("sp", xout[r0:r0 + ST, :].rearrange("(c p) d -> p c d", p=128), xacc[:], reads=[bxacc],
                  writes=[self.dbuf(oname, t) for t in tiles])

    def zero_fill_xs(self, after):
        c = self.c
        self.zt = self.T("zt", [128, 2048], BF16)
        c.op("pool", "memset", writes=[self.zt[1]], ap=self.zt[0][:], constant=0.0)
        for li in range(1):
            if "p2" not in self.phases and "p6" not in self.phases:
                continue
            rows_per = 128 * 2
            for i in range(self.NCAP // rows_per):
                bz = Buf()
                c.dma("act", self.Xs[li][i * rows_per:(i + 1) * rows_per, :].rearrange("(p a) d -> p (a d)", p=128), self.zt[0][:],
                      reads=[self.zt[1]] + list(after), writes=[bz])
                self.zf[0].append(bz)
                self.zf[1].append(bz)

    def moe_sparse(self, li, xin, xout):
        c = self.c
        W = self.W
        S, NCH, NT_, NCAP = self.S, self.NCH, self.NTILES, self.NCAP
        IOA = bass.IndirectOffsetOnAxis
        MT = max(1, (2 * S) // 512)
        FL = NCH * 32
        R, bR = self.T("mR", [128, NCH, 66], F32)
        c.dma("sp", R[:], self.rinfo[li].rearrange("(c p) e -> p c e", p=128),
              reads=[self.dbuf(f"rinfo{li}", g) for g in range(NCH)], writes=[bR])
        posi, bposi = self.T("mposi", [128, 2, NCH], I32)
        widx, bwidx = self.T("mwidx", [128, NT_], I32)
        with c.scope():
            Mb, bMb = self.T("mMb", [128, NCH, 32], BF16)
            c.op("dve", "tensor_tensor", reads=[bR], writes=[bMb], out=Mb[:], in0=R[:, :, 0:32], in1=R[:, :, 32:64], op=ALU.add)
            Ls, bLs = self.T("mLs", [128, 128], BF16)
            c.op("pool", "memset", writes=[bLs], ap=Ls[:], constant=1.0)
            c.op("pool", "affine_select", reads=[bLs], writes=[bLs], out=Ls[:], in_=Ls[:], pattern=[[1, 128]], compare_op=ALU.is_gt,
                 fill=0.0, base=0, channel_multiplier=-1)
            rank, brank = self.T("mrank", [128, NCH, 32], F32)
            cnt, bcnt = self.T("mcnt", [128, NCH, 32], F32)
            Mbf = Mb[:].rearrange("p c e -> p (c e)")
            rankf = rank[:].rearrange("p c e -> p (c e)")
            cntf = cnt[:].rearrange("p c e -> p (c e)")
            for g0 in range(0, FL, 512):
                n = min(512, FL - g0)
                p1, bp1 = self.mmrot.next()
                c.op("pe", "matmul", reads=[bLs, bMb], writes=[bp1], out=p1[:, 0:n], lhsT=Ls[:], rhs=Mbf[:, g0:g0 + n], start=True, stop=True)
                c.op("act", "copy", reads=[bp1], writes=[brank], out=rankf[:, g0:g0 + n], in_=p1[:, 0:n])
                p2, bp2 = self.mmrot.next()
                c.op("pe", "matmul", reads=[self.onesb[1], bMb], writes=[bp2], out=p2[:, 0:n], lhsT=self.onesb[0][:], rhs=Mbf[:, g0:g0 + n],
                     start=True, stop=True)
                c.op("dve", "tensor_copy", reads=[bp2], writes=[bcnt], out=cntf[:, g0:g0 + n], in_=p2[:, 0:n])
            pre, bpre = self.T("mpre", [128, NCH, 32], F32)
            c.op("dve", "memset", writes=[bpre], ap=pre[:, 0, :], constant=0.0)
            for ch in range(1, NCH):
                c.op("dve", "tensor_tensor", reads=[bpre, bcnt], writes=[bpre], out=pre[:, ch, :], in0=pre[:, ch - 1, :], in1=cnt[:, ch - 1, :],
                     op=ALU.add)
            ne, bne = self.T("mne", [128, 32], F32)
            c.op("dve", "tensor_tensor", reads=[bpre, bcnt], writes=[bne], out=ne[:], in0=pre[:, NCH - 1, :], in1=cnt[:, NCH - 1, :], op=ALU.add)
            thr, bthr = self.T("mthr", [128, 32, MT], F32)
            c.op("pool", "iota", writes=[bthr], out=thr[:], pattern=[[0, 32], [512, MT]], base=0, channel_multiplier=0,
                 allow_small_or_imprecise_dtypes=True)
            cmp_, bcmp = self.T("mcmp", [128, 32, MT], F32)
            c.op("dve", "tensor_tensor", reads=[bne, bthr], writes=[bcmp], out=cmp_[:], in0=ne[:].unsqueeze(2).to_broadcast([128, 32, MT]),
                 in1=thr[:], op=ALU.is_gt)
            tl, btl = self.T("mtl", [128, 32], F32)
            c.op("dve", "tensor_reduce", reads=[bcmp], writes=[btl], out=tl[:], in_=cmp_[:], axis=AX.X, op=ALU.add)
            Lt, bLt = self.T("mLt", [128, 32, 32], F32)
            c.op("pool", "memset", writes=[bLt], ap=Lt[:], constant=1.0)
            c.op("pool", "affine_select", reads=[bLt], writes=[bLt], out=Lt[:], in_=Lt[:], pattern=[[1, 32], [-1, 32]], compare_op=ALU.is_gt,
                 fill=0.0, base=0, channel_multiplier=0)
            t32, bt32 = self.T("mt32", [128, 32, 32], F32)
            c.op("dve", "tensor_tensor", reads=[btl, bLt], writes=[bt32], out=t32[:], in0=tl[:].unsqueeze(1).to_broadcast([128, 32, 32]),
                 in1=Lt[:], op=ALU.mult)
            ot, bot = self.T("mot", [128, 32], F32)
            c.op("dve", "tensor_reduce", reads=[bt32], writes=[bot], out=ot[:], in_=t32[:], axis=AX.X, op=ALU.add)
            up, bup = self.T("mup", [128, 32], F32)
            c.op("dve", "tensor_tensor", reads=[bot, btl], writes=[bup], out=up[:], in0=ot[:], in1=tl[:], op=ALU.add)
            off, boff = self.T("moff", [128, 32], F32)
            c.op("dve", "tensor_scalar", reads=[bot], writes=[boff], out=off[:], in0=ot[:], scalar1=512.0, scalar2=None, op0=ALU.mult)
            c.op("dve", "tensor_tensor", reads=[brank, bpre], writes=[brank], out=rank[:], in0=rank[:], in1=pre[:], op=ALU.add)
            c.op("dve", "tensor_tensor", reads=[brank, boff], writes=[brank], out=rank[:], in0=rank[:],
                 in1=off[:].unsqueeze(1).to_broadcast([128, NCH, 32]), op=ALU.add)
            posf, bposf = self.T("mposf", [128, 2, NCH], F32)
            for sl in range(2):
                c.op("dve", "tensor_tensor", reads=[bR, brank], writes=[bpre], out=pre[:], in0=R[:, :, sl * 32:(sl + 1) * 32], in1=rank[:],
                     op=ALU.mult)
                c.op("dve", "tensor_reduce", reads=[bpre], writes=[bposf], out=posf[:, sl, :], in_=pre[:], axis=AX.X, op=ALU.add)
            c.op("dve", "tensor_copy", reads=[bposf], writes=[bposi], out=posi[:], in_=posf[:])
            ti, bti = self.T("mti", [128, NT_], F32)
            c.op("pool", "iota", writes=[bti], out=ti[:], pattern=[[1, NT_]], base=0, channel_multiplier=0, allow_small_or_imprecise_dtypes=True)
            ei, bei = self.T("mei", [128, 32], F32)
            c.op("pool", "iota", writes=[bei], out=ei[:], pattern=[[1, 32]], base=0, channel_multiplier=0, allow_small_or_imprecise_dtypes=True)
            pidx, bpidx = self.T("mpidx", [128, 1], F32)
            c.op("pool", "iota", writes=[bpidx], out=pidx[:], pattern=[[0, 1]], base=0, channel_multiplier=1, allow_small_or_imprecise_dtypes=True)
            A1, bA1 = self.T("mA1", [128, NT_, 32], F32)
            A2, bA2 = self.T("mA2", [128, NT_, 32], F32)
            tib = ti[:].unsqueeze(2).to_broadcast([128, NT_, 32])
            c.op("dve", "tensor_tensor", reads=[bti, bot], writes=[bA1], out=A1[:], in0=tib, in1=ot[:].unsqueeze(1).to_broadcast([128, NT_, 32]),
                 op=ALU.is_ge)
            c.op("dve", "tensor_tensor", reads=[bti, bup], writes=[bA2], out=A2[:], in0=tib, in1=up[:].unsqueeze(1).to_broadcast([128, NT_, 32]),
                 op=ALU.is_lt)
            c.op("dve", "tensor_tensor", reads=[bA1, bA2], writes=[bA1], out=A1[:], in0=A1[:], in1=A2[:], op=ALU.mult)
            used, bused = self.T("mused", [128, NT_], F32)
            c.op("dve", "tensor_reduce", reads=[bA1], writes=[bused], out=used[:], in_=A1[:], axis=AX.X, op=ALU.add)
            c.op("dve", "tensor_tensor", reads=[bA1, bei], writes=[bA1], out=A1[:], in0=A1[:], in1=ei[:].unsqueeze(1).to_broadcast([128, NT_, 32]),
                 op=ALU.mult)
            eid, beid = self.T("meid", [128, NT_], F32)
            c.op("dve", "tensor_reduce", reads=[bA1], writes=[beid], out=eid[:], in_=A1[:], axis=AX.X, op=ALU.add)
            c.op("dve", "tensor_scalar", reads=[beid, bpidx], writes=[beid], out=eid[:], in0=eid[:], scalar1=128.0, scalar2=pidx[:, 0:1],
                 op0=ALU.mult, op1=ALU.add)
            if li:
                c.op("dve", "tensor_scalar", reads=[beid], writes=[beid], out=eid[:], in0=eid[:], scalar1=float(li * 4096), scalar2=None,
                     op0=ALU.add)
            c.op("dve", "tensor_scalar", reads=[bused], writes=[bused], out=used[:], in0=used[:], scalar1=-1.0e6, scalar2=1.0e6, op0=ALU.mult,
                 op1=ALU.add)
            c.op("dve", "tensor_tensor", reads=[beid, bused], writes=[beid], out=eid[:], in0=eid[:], in1=used[:], op=ALU.add)
            c.op("dve", "tensor_copy", reads=[beid], writes=[bwidx], out=widx[:], in_=eid[:])
        hrot = Rot([self.T(f"mh{i}", [128, 1024], BF16) for i in range(6)])
        scb = []
        for ch in range(NCH):
            hc, bhc = hrot.next()
            c.dma("sp", hc[:], self.h2b[li][ch * 128:(ch + 1) * 128, :], reads=[self.dbuf(f"h2b{li}", ch)], writes=[bhc])
            for sl in range(2):
                bs = Buf()
                c.idma(self.Xs[li], IOA(ap=posi[:, sl, ch:ch + 1], axis=0), hc[:], None, reads=[bhc, bposi] + self.zf[li], writes=[bs])
                scb.append(bs)
        wg = Rot([self.T(f"mwg{i}", [128, 8, 256], BF16) for i in range(2)])
        wu = Rot([self.T(f"mwu{i}", [128, 8, 256], BF16) for i in range(2)])
        wd = Rot([self.T(f"mwd{i}", [128, 2, 1024], BF16) for i in range(2)])
        xsr = Rot([self.T(f"mxs{i}", [128, 4, 1024], BF16) for i in range(2)])
        XTr = Rot([self.T(f"mXT{i}", [128, 8, 512], BF16) for i in range(2)])
        sgr = Rot([self.T(f"msg{i}", [128, 512], BF16) for i in range(2)])
        ar = Rot([self.T(f"ma{i}", [128, 512], BF16) for i in range(4)])
        ytr = Rot([self.T(f"myt{i}", [128, 4, 1024], BF16) for i in range(2)])
        pg = [self.pmm[0], self.pmm[1]]
        pu = [self.pmm[2], self.pmm[3]]
        pwt = self.pw[0]
        yrot = Rot([(pwt[:, 0:512], Buf("pwa", True)), (pwt[:, 512:1024], Buf("pwb", True))])
        trot = Rot([(self.pT[0][:].bitcast(BF16), self.pT[1]), (self.pmisc[0][:].bitcast(BF16), self.pmisc[1])])
        ysb = []
        loaded = {}

        def issue(i):
            g_, u_, d_, x_ = wg.next(), wu.next(), wd.next(), xsr.next()
            ioa = IOA(ap=widx[:, i:i + 1], axis=0)
            c.idma(g_[0][:].rearrange("p k f -> p (k f)"), None, W["moe_wgL"].rearrange("l r c -> (l r) c"), ioa, reads=[bwidx], writes=[g_[1]],
                   bounds_check=8191, oob_is_err=False)
            c.idma(u_[0][:].rearrange("p k f -> p (k f)"), None, W["moe_wuL"].rearrange("l r c -> (l r) c"), ioa, reads=[bwidx], writes=[u_[1]],
                   bounds_check=8191, oob_is_err=False)
            c.idma(d_[0][:].rearrange("p k f -> p (k f)"), None, W["moe_wdL"].rearrange("l r c -> (l r) c"), ioa, reads=[bwidx], writes=[d_[1]],
                   bounds_check=8191, oob_is_err=False)
            c.dma("sp", x_[0][:], self.Xs[li][i * 512:(i + 1) * 512, :].rearrange("(c p) d -> p c d", p=128),
                  reads=scb + self.zf[li], writes=[x_[1]])
            loaded[i] = (g_, u_, d_, x_)

        issue(0)
        for i in range(NT_):
            if i + 1 < NT_:
                issue(i + 1)
            (wg_, bwg), (wu_, bwu), (wd_, bwd), (x_, bx_) = loaded.pop(i)
            XT, bXT = XTr.next()
            for ch in range(4):
                pTv, bpT = trot.next()
                for k in range(8):
                    c.op("pe", "transpose", reads=[bx_, self.identb[1]], writes=[bpT], out=pTv[:, k * 128:(k + 1) * 128],
                         in_=x_[:, ch, k * 128:(k + 1) * 128], identity=self.identb[0][:])
                c.op("act", "copy", reads=[bpT], writes=[bXT], out=XT[:, 0:4, ch * 128:(ch + 1) * 128],
                     in_=pTv[:, 0:512].rearrange("p (k t) -> p k t", k=4))
                c.op("dve", "tensor_copy", reads=[bpT], writes=[bXT], out=XT[:, 4:8, ch * 128:(ch + 1) * 128],
                     in_=pTv[:, 512:1024].rearrange("p (k t) -> p k t", k=4))
            acts = []
            for fc in range(2):
                g_, bg_ = pg[fc]
                u_, bu_ = pu[fc]
                for k in range(8):
                    c.op("pe", "matmul", reads=[bwg, bXT], writes=[bg_], out=g_[:], lhsT=wg_[:, k, fc * 128:(fc + 1) * 128], rhs=XT[:, k, :],
                         start=(k == 0), stop=(k == 7))
                for k in range(8):
                    c.op("pe", "matmul", reads=[bwu, bXT], writes=[bu_], out=u_[:], lhsT=wu_[:, k, fc * 128:(fc + 1) * 128], rhs=XT[:, k, :],
                         start=(k == 0), stop=(k == 7))
                sg, bsg = sgr.next()
                c.op("act", "activation", reads=[bg_], writes=[bsg], out=sg[:], in_=g_[:], func=AF.Silu)
                a, ba = ar.next()
                c.op("dve", "tensor_tensor", reads=[bsg, bu_], writes=[ba], out=a[:], in0=sg[:], in1=u_[:], op=ALU.mult)
                acts.append((a, ba))
            yt, byt = ytr.next()
            n_ev = 0
            for ch in range(4):
                for half in range(2):
                    py, bpy = yrot.next()
                    for fc in range(2):
                        c.op("pe", "matmul", reads=[acts[fc][1], bwd], writes=[bpy], out=py, lhsT=acts[fc][0][:, ch * 128:(ch + 1) * 128],
                             rhs=wd_[:, fc, half * 512:(half + 1) * 512], start=(fc == 0), stop=(fc == 1))
                    if n_ev % 2 == 0:
                        c.op("act", "copy", reads=[bpy], writes=[byt], out=yt[:, ch, half * 512:(half + 1) * 512], in_=py)
                    else:
                        c.op("dve", "tensor_copy", reads=[bpy], writes=[byt], out=yt[:, ch, half * 512:(half + 1) * 512], in_=py)
                    n_ev += 1
            by_ = Buf()
            c.dma("sp", self.Ys[li][i * 512:(i + 1) * 512, :].rearrange("(c p) d -> p c d", p=128), yt[:], reads=[byt], writes=[by_])
            ysb.append(by_)
        y1r = Rot([self.T(f"my1{i}", [128, 1024], BF16) for i in range(4)])
        y2r = Rot([self.T(f"my2{i}", [128, 1024], BF16) for i in range(4)])
        xmr = Rot([self.T(f"mxm{i}", [128, 1024], F32) for i in range(4)])
        dgr = Rot([self.T(f"mdg{i}", [128, 2, 128], BF16) for i in range(4)])
        oname = "x1" if li == 0 else "out"
        for ch in range(NCH):
            y1, by1 = y1r.next()
            y2, by2 = y2r.next()
            xm, bxm = xmr.next()
            dg, bdg = dgr.next()
            c.idma(y1[:], None, self.Ys[li], IOA(ap=posi[:, 0, ch:ch + 1], axis=0), reads=ysb + [bposi], writes=[by1])
            c.idma(y2[:], None, self.Ys[li], IOA(ap=posi[:, 1, ch:ch + 1], axis=0), reads=ysb + [bposi], writes=[by2])
            c.dma("sp", xm[:], xin[ch * 128:(ch + 1) * 128, :], reads=[self.dbuf(f"xmid{li}", ch // 4)], writes=[bxm])
            for sl in range(2):
                c.op("act", "activation", reads=[self.identb[1], bR], writes=[bdg], out=dg[:, sl, :], in_=self.identb[0][:], func=AF.Copy,
                     scale=R[:, ch, 64 + sl:65 + sl])
            for half in range(2):
                pc, bpc = self.mmrot.next()
                hs_ = slice(half * 512, (half + 1) * 512)
                c.op("pe", "matmul", reads=[bdg, by1], writes=[bpc], out=pc[:], lhsT=dg[:, 0, :], rhs=y1[:, hs_], start=True, stop=False)
                c.op("pe", "matmul", reads=[bdg, by2], writes=[bpc], out=pc[:], lhsT=dg[:, 1, :], rhs=y2[:, hs_], start=False, stop=True)
                c.op("dve", "tensor_tensor", reads=[bpc, bxm], writes=[bxm], out=xm[:, hs_], in0=pc[:], in1=xm[:, hs_], op=ALU.add)
            if li == 0:
                wr_b = self.dbuf("x1c", ch)
            else:
                wr_b = self.dbuf("outc", ch)
            c.dma("sp", xout[ch * 128:(ch + 1) * 128, :], xm[:], reads=[bxm], writes=[wr_b])
        if li == 0:
            for t_ in range(self.NT):
                self.db[("x1", t_)] = self.db[("x1c", t_ * 4 + 3)]

    def rope_tables(self):
        c = self.c
        NCH = self.NCH
        cosd = self.nc.dram_tensor("cosd", [128, NCH, 32], F32).ap()
        sind = self.nc.dram_tensor("sind", [128, NCH, 32], F32).ap()
        with c.scope():
            cos, bcos = self.T("cos", [128, NCH, 32], F32)
            sin, bsin = self.T("sin", [128, NCH, 32], F32)
            pi_, bpi = self.T("posi", [NCH, 128], I32)
            c.dma("sp", pi_[:], self.pos.rearrange("(c p) -> c p", p=128), writes=[bpi])
            pf, bpf = self.T("posf", [NCH, 128], F32)
            c.op("dve", "tensor_copy", reads=[bpi], writes=[bpf], out=pf[:], in_=pi_[:])
            pw, bpw = self.pw
            c.op("pe", "transpose", reads=[bpf, self.identf[1]], writes=[bpw], out=pw[:, 0:NCH], in_=pf[:],
                 identity=self.identf[0][0:NCH, 0:NCH])
            pT_, bpT_ = self.T("posT", [128, NCH], F32)
            c.op("act", "copy", reads=[bpw], writes=[bpT_], out=pT_[:], in_=pw[:, 0:NCH])
            iv, biv = self.load_bcast("invf", self.invf, 32)
            ang, bang = self.T("ang", [128, NCH, 32], F32)
            c.op("dve", "tensor_tensor", reads=[bpT_, biv], writes=[bang], out=ang[:],
                 in0=pT_[:].unsqueeze(2).to_broadcast([128, NCH, 32]), in1=iv[:].unsqueeze(1).to_broadcast([128, NCH, 32]), op=ALU.mult)
            t, bt = self.T("rr_t", [128, NCH, 32], F32)
            ti, bti = self.T("rr_ti", [128, NCH, 32], I32)
            r, br = self.T("rr_r", [128, NCH, 32], F32)
            TWO_PI = 2.0 * np.pi
            C1 = 6.28125
            C2 = TWO_PI - C1
            c.op("dve", "tensor_scalar", reads=[bang], writes=[bt], out=t[:], in0=ang[:], scalar1=float(1.0 / TWO_PI), scalar2=0.5,
                 op0=ALU.mult, op1=ALU.add)
            c.op("dve", "tensor_copy", reads=[bt], writes=[bti], out=ti[:], in_=t[:])
            c.op("dve", "tensor_copy", reads=[bti], writes=[bt], out=t[:], in_=ti[:])
            c.op("dve", "scalar_tensor_tensor", reads=[bt, bang], writes=[br], out=r[:], in0=t[:], scalar=float(-C1), in1=ang[:],
                 op0=ALU.mult, op1=ALU.add)
            c.op("dve", "scalar_tensor_tensor", reads=[bt, br], writes=[br], out=r[:], in0=t[:], scalar=float(-C2), in1=r[:],
                 op0=ALU.mult, op1=ALU.add)
            c.op("dve", "tensor_scalar", reads=[br], writes=[bt], out=t[:], in0=r[:], scalar1=float(-np.pi), scalar2=float(TWO_PI),
                 op0=ALU.is_lt, op1=ALU.mult)
            c.op("dve", "tensor_tensor", reads=[br, bt], writes=[br], out=r[:], in0=r[:], in1=t[:], op=ALU.add)
            c.op("dve", "tensor_scalar", reads=[br], writes=[bt], out=t[:], in0=r[:], scalar1=float(np.pi), scalar2=float(-TWO_PI),
                 op0=ALU.is_gt, op1=ALU.mult)
            c.op("dve", "tensor_tensor", reads=[br, bt], writes=[br], out=r[:], in0=r[:], in1=t[:], op=ALU.add)
            c.op("dve", "tensor_scalar", reads=[br], writes=[br], out=r[:], in0=r[:], scalar1=float(-3.1415925), scalar2=float(3.1415925),
                 op0=ALU.max, op1=ALU.min)
            c.op("act", "activation", reads=[br], writes=[bsin], out=sin[:], in_=r[:], func=AF.Sin)
            c.op("dve", "scalar_tensor_tensor", reads=[br], writes=[bt], out=t[:], in0=r[:], scalar=-1.0, in1=r[:], op0=ALU.mult,
                 op1=ALU.max)
            c.op("dve", "tensor_scalar", reads=[bt], writes=[bt], out=t[:], in0=t[:], scalar1=-1.0, scalar2=float(np.pi / 2),
                 op0=ALU.mult, op1=ALU.add)
            c.op("act", "activation", reads=[bt], writes=[bcos], out=cos[:], in_=t[:], func=AF.Sin)
            c.dma("sp", cosd, cos[:], reads=[bcos], writes=[self.dbuf("cosd", 0)])
            c.dma("sp", sind, sin[:], reads=[bsin], writes=[self.dbuf("sind", 0)])
        return cosd, sind

    def rope_apply(self, eng_a, eng_b, x1, x2, cosb, sinb, o1, o2, tmp, btmp, rd, wr, shape):
        c = self.c
        n = int(np.prod(shape[1:]))
        t = [tmp[:, i, 0:n].rearrange("p (a b) -> p a b", a=shape[1]) if len(shape) == 3 else tmp[:, i, 0:n] for i in range(4)]
        c.op(eng_a, "tensor_tensor", reads=rd, writes=[btmp], out=t[0], in0=x1, in1=cosb, op=ALU.mult)
        c.op(eng_a, "tensor_tensor", reads=rd, writes=[btmp], out=t[1], in0=x2, in1=sinb, op=ALU.mult)
        c.op(eng_b, "tensor_tensor", reads=rd, writes=[btmp], out=t[2], in0=x1, in1=sinb, op=ALU.mult)
        c.op(eng_b, "tensor_tensor", reads=rd, writes=[btmp], out=t[3], in0=x2, in1=cosb, op=ALU.mult)
        c.op(eng_a, "tensor_tensor", reads=[btmp], writes=wr, out=o1, in0=t[0], in1=t[1], op=ALU.subtract)
        c.op(eng_b, "tensor_tensor", reads=[btmp], writes=wr, out=o2, in0=t[2], in1=t[3], op=ALU.add)

    def p3_mla_proj(self):
        c = self.c
        W = self.W
        S = self.S
        win, bwin = self.T("b_win", [128, 8, 1344], BF16)
        wq, bwq = self.T("b_wq", [128, 4, 1536], BF16)
        wkv, bwkv = self.T("b_wkv", [128, 2, 2048], BF16)
        gqr, bgqr = self.load_bcast("gqr", W["b_qn_g"][0], 192)
        gkr, bgkr = self.load_bcast("gkr", W["b_kn_g"][0], 192)
        c.op("dve", "tensor_scalar", reads=[bgqr], writes=[bgqr], out=gqr[:], in0=gqr[:], scalar1=float(192 ** -0.5), scalar2=None,
             op0=ALU.mult)
        with c.scope():
            self.wstage = Rot([self.T(f"wstage{i}", [128, 2560], F32) for i in range(2)])
            n1, bn1 = self.load_small_cols("n1g1", W["norm1_g"][1], 8)
            self.load_w_gain(win, bwin, W["b_w_in"][0], 8, 1344, n1, bn1)
            gq_, bgq_ = self.load_small_cols("qng", W["b_q_norm_g"][0], 4)
            self.load_w_gain(wq, bwq, W["b_w_q_up"][0], 4, 1536, gq_, bgq_)
            gkv_, bgkv_ = self.load_small_cols("kvng", W["b_kv_norm_g"][0], 2)
            self.load_w_gain(wkv, bwkv, W["b_w_kv_up"][0], 2, 2048, gkv_, bgkv_)
        cosd, sind = self.rope_tables()
        self.alloc_tile_bufs(full=False)
        csr = Rot([self.T(f"cs{i}", [128, 4, 64], F32) for i in range(2)])
        cqT, bcqT = self.T("cqT", [128, 4, 512], BF16)
        ckvT, bckvT = self.T("ckvT", [128, 2, 512], BF16)
        cqn = Rot([self.T(f"cqn{i}", [128, 512], BF16) for i in range(2)])
        ckvn = Rot([self.T(f"ckvn{i}", [128, 256], BF16) for i in range(2)])
        krr, bkrr = self.T("krr", [128, 4, 64], F32)
        zs, bzs = self.T("zs", [128, 4, 3], F32)
        qfr = Rot([self.T(f"qf{i}", [128, 8, 192], F32) for i in range(2)])
        sqfr = Rot([self.T(f"sqf{i}", [128, 8, 192], F32) for i in range(2)])
        qbr = Rot([self.T(f"qb{i}", [128, 8, 192], BF16) for i in range(2)])
        hsr = Rot([self.T(f"hs{i}", [128, 8], F32) for i in range(4)])
        kfr = Rot([self.T(f"kf{i}", [128, 8, 128], F32) for i in range(2)])
        kbr = Rot([self.T(f"kb{i}", [128, 8, 192], BF16) for i in range(2)])
        vb = Rot([self.T(f"vb{i}", [128, 8, 128], BF16) for i in range(1)])
        krgr = Rot([self.T(f"krg{i}", [128, 64], F32) for i in range(2)])
        krotr = Rot([self.T(f"krot{i}", [128, 64], F32) for i in range(2)])
        rtmpr = Rot([self.T(f"rtmp{i}", [128, 4, 256], F32) for i in range(2)])
        QTnr = Rot([self.T(f"QTn{i}", [128, 8, 128], BF16) for i in range(2)])
        QTrr = Rot([self.T(f"QTr{i}", [64, 8, 128], BF16) for i in range(2)])
        KTnr = Rot([self.T(f"KTn{i}", [128, 8, 128], BF16) for i in range(2)])
        KTrr = Rot([self.T(f"KTr{i}", [64, 8, 128], BF16) for i in range(2)])
        moT, bmoT = self.T("moT", [128, 4, 512], BF16)
        junk, bjunk = self.junk
        pTv = self.pT[0][:].bitcast(BF16)
        bpT = self.pT[1]
        pwv = self.pw[0][:].bitcast(BF16)
        bpw = self.pw[1]

        def to_T(src, bsrc, dn, bdn, dr, bdr, ch):
            for h in range(8):
                c.op("pe", "transpose", reads=[bsrc, self.identb[1]], writes=[bpT], out=pTv[:, h * 128:(h + 1) * 128],
                     in_=src[:, h, 0:128], identity=self.identb[0][:])
            c.op("act", "copy", reads=[bpT], writes=[bdn], out=dn[:], in_=pTv.rearrange("p (h t) -> p h t", h=8))
            for h in range(8):
                c.op("pe", "transpose", reads=[bsrc, self.identb[1]], writes=[bpw], out=pwv[0:64, h * 128:(h + 1) * 128],
                     in_=src[:, h, 128:192], identity=self.identb[0][:])
            c.op("dve", "tensor_copy", reads=[bpw], writes=[bdr], out=dr[:], in_=pwv[0:64, 0:1024].rearrange("p (h t) -> p h t", h=8))

        for j in range(self.NT):
            xt, bxt = self.xt.next()
            c.dma("sp", xt[:], self.x1[j * 512:(j + 1) * 512, :].rearrange("(c p) d -> p c d", p=128),
                  reads=[self.dbuf("x1", j)], writes=[bxt])
            self.norm_to_hT(xt, bxt)
            hT, bhT = self.hT
            cst, bcst = csr.next()
            c.dma("sp", cst[:, :, 0:32], cosd[:, j * 4:(j + 1) * 4, :], reads=[self.dbuf("cosd", 0)], writes=[bcst])
            c.dma("sp", cst[:, :, 32:64], sind[:, j * 4:(j + 1) * 4, :], reads=[self.dbuf("sind", 0)], writes=[bcst])
            for ch in range(4):
                p1, bp1 = self.mmrot.next()
                for k in range(8):
                    c.op("pe", "matmul", reads=[bhT, bwin], writes=[bp1], out=p1[:], lhsT=hT[:, k, ch * 128:(ch + 1) * 128],
                         rhs=win[:, k, 0:512], start=(k == 0), stop=(k == 7))
                p2, bp2 = self.mmrot.next()
                for k in range(8):
                    c.op("pe", "matmul", reads=[bhT, bwin], writes=[bp2], out=p2[:, 0:320], lhsT=hT[:, k, ch * 128:(ch + 1) * 128],
                         rhs=win[:, k, 512:832], start=(k == 0), stop=(k == 7))
                c.op("act", "activation", reads=[bp1], writes=[bjunk, bzs], out=junk[:, 0:512], in_=p1[:], func=AF.Square,
                     accum_out=zs[:, ch, 0:1])
                c.op("act", "activation", reads=[bp2], writes=[bjunk, bzs], out=junk[:, 0:256], in_=p2[:, 0:256], func=AF.Square,
                     accum_out=zs[:, ch, 1:2])
                c.op("act", "activation", reads=[bp2], writes=[bjunk, bzs], out=junk[:, 0:64], in_=p2[:, 256:320], func=AF.Square,
                     accum_out=zs[:, ch, 2:3])
                c.op("act", "copy", reads=[bp2], writes=[bkrr], out=krr[:, ch, :], in_=p2[:, 256:320])
                self.rsqrt_(zs[:, ch, 0:1], bzs, 1.0 / 512, 1)
                self.rsqrt_(zs[:, ch, 1:2], bzs, 1.0 / 256, 1)
                a, ba = cqn.next()
                c.op("dve", "tensor_scalar", reads=[bp1, bzs], writes=[ba], out=a[:], in0=p1[:], scalar1=zs[:, ch, 0:1], scalar2=None,
                     op0=ALU.mult)
                b_, bb_ = ckvn.next()
                c.op("dve", "tensor_scalar", reads=[bp2, bzs], writes=[bb_], out=b_[:], in0=p2[:, 0:256], scalar1=zs[:, ch, 1:2],
                     scalar2=None, op0=ALU.mult)
                for k in range(4):
                    c.op("pe", "transpose", reads=[ba, self.identb[1]], writes=[bpT], out=pTv[:, k * 128:(k + 1) * 128],
                         in_=a[:, k * 128:(k + 1) * 128], identity=self.identb[0][:])
                for k in range(2):
                    c.op("pe", "transpose", reads=[bb_, self.identb[1]], writes=[bpT], out=pTv[:, (4 + k) * 128:(5 + k) * 128],
                         in_=b_[:, k * 128:(k + 1) * 128], identity=self.identb[0][:])
                c.op("act", "copy", reads=[bpT], writes=[bcqT], out=cqT[:, :, ch * 128:(ch + 1) * 128],
                     in_=pTv[:, 0:512].rearrange("p (k t) -> p k t", k=4))
                c.op("act", "copy", reads=[bpT], writes=[bckvT], out=ckvT[:, :, ch * 128:(ch + 1) * 128],
                     in_=pTv[:, 512:768].rearrange("p (k t) -> p k t", k=2))
            for ch in range(4):
                gc = j * 4 + ch
                cosb = cst[:, ch, 0:32].unsqueeze(1).to_broadcast([128, 8, 32])
                sinb = cst[:, ch, 32:64].unsqueeze(1).to_broadcast([128, 8, 32])
                bcos = bsin = bcst
                qf, bqf = qfr.next()
                sqf, bsqf = sqfr.next()
                qb, bqb = qbr.next()
                hs, bhs = hsr.next()
                kf, bkf = kfr.next()
                kb, bkb = kbr.next()
                krg, bkrg = krgr.next()
                krot, bkrot = krotr.next()
                rtmp, brtmp = rtmpr.next()
                QTn, bQTn = QTnr.next()
                QTr, bQTr = QTrr.next()
                KTn, bKTn = KTnr.next()
                KTr, bKTr = KTrr.next()
                gs = slice(gc * 128, (gc + 1) * 128)
                for grp in range(4):
                    pq, bpq = self.mmrot.next()
                    for k in range(4):
                        c.op("pe", "matmul", reads=[bcqT, bwq], writes=[bpq], out=pq[:, 0:384], lhsT=cqT[:, k, ch * 128:(ch + 1) * 128],
                             rhs=wq[:, k, grp * 384:(grp + 1) * 384], start=(k == 0), stop=(k == 3))
                    c.op("act", "copy", reads=[bpq], writes=[bqf], out=qf[:, 2 * grp:2 * grp + 2, :],
                         in_=pq[:, 0:384].rearrange("p (h d) -> p h d", h=2))
                c.op("dve", "tensor_tensor", reads=[bqf], writes=[bsqf], out=sqf[:], in0=qf[:], in1=qf[:], op=ALU.mult)
                c.op("dve", "tensor_reduce", reads=[bsqf], writes=[bhs], out=hs[:], in_=sqf[:], axis=AX.X, op=ALU.add)
                self.rsqrt_(hs[:], bhs, 1.0 / 192, 8)
                c.op("dve", "tensor_tensor", reads=[bqf, bhs], writes=[bqf], out=qf[:], in0=qf[:],
                     in1=hs[:].unsqueeze(2).to_broadcast([128, 8, 192]), op=ALU.mult)
                c.op("pool", "tensor_tensor", reads=[bqf, bgqr], writes=[bqf], out=qf[:], in0=qf[:],
                     in1=gqr[:].unsqueeze(1).to_broadcast([128, 8, 192]), op=ALU.mult)
                c.op("act", "copy", reads=[bqf], writes=[bqb], out=qb[:, :, 0:128], in_=qf[:, :, 0:128])
                self.rope_apply("dve", "pool", qf[:, :, 128:160], qf[:, :, 160:192], cosb, sinb, qb[:, :, 128:160], qb[:, :, 160:192],
                                rtmp, brtmp, [bqf, bcos, bsin], [bqb], [128, 8, 32])
                to_T(qb, bqb, QTn, bQTn, QTr, bQTr, ch)
                c.dma("sp", self.QT[:, 0:128, gs].rearrange("h p t -> p h t"), QTn[:], reads=[bQTn], writes=[self.dbuf("QTn", gc)])
                c.dma("sp", self.QT[:, 128:192, gs].rearrange("h p t -> p h t"), QTr[:], reads=[bQTr], writes=[self.dbuf("QTr", gc)])
                sqf, bsqf = sqfr.next()
                hs, bhs = hsr.next()
                rtmp, brtmp = rtmpr.next()
                v_, bv_ = vb.next()
                for grp in range(4):
                    pk, bpk = self.mmrot.next()
                    for k in range(2):
                        c.op("pe", "matmul", reads=[bckvT, bwkv], writes=[bpk], out=pk[:], lhsT=ckvT[:, k, ch * 128:(ch + 1) * 128],
                             rhs=wkv[:, k, grp * 512:(grp + 1) * 512], start=(k == 0), stop=(k == 1))
                    pkv = pk[:].rearrange("p (h d) -> p h d", h=2)
                    c.op("act", "copy", reads=[bpk], writes=[bkf], out=kf[:, 2 * grp:2 * grp + 2, :], in_=pkv[:, :, 0:128])
                    c.op("dve", "tensor_copy", reads=[bpk], writes=[bv_], out=v_[:, 2 * grp:2 * grp + 2, :], in_=pkv[:, :, 128:256])
                c.dma("sp", self.Vd[gc * 128:(gc + 1) * 128, :], v_[:].rearrange("p h d -> p (h d)"), reads=[bv_],
                      writes=[self.dbuf("Vd", gc)])
                c.op("dve", "tensor_tensor", reads=[bkf], writes=[bsqf], out=sqf[:, :, 0:128], in0=kf[:], in1=kf[:], op=ALU.mult)
                c.op("dve", "tensor_reduce", reads=[bsqf], writes=[bhs], out=hs[:], in_=sqf[:, :, 0:128], axis=AX.X, op=ALU.add)
                c.op("dve", "tensor_scalar", reads=[bhs, bzs], writes=[bhs], out=hs[:], in0=hs[:], scalar1=zs[:, ch, 2:3], scalar2=None,
                     op0=ALU.add)
                self.rsqrt_(hs[:], bhs, 1.0 / 192, 8)
                c.op("dve", "tensor_tensor", reads=[bkf, bhs], writes=[bkf], out=kf[:], in0=kf[:],
                     in1=hs[:].unsqueeze(2).to_broadcast([128, 8, 128]), op=ALU.mult)
                c.op("pool", "tensor_tensor", reads=[bkf, bgkr], writes=[bkb], out=kb[:, :, 0:128], in0=kf[:],
                     in1=gkr[:, 0:128].unsqueeze(1).to_broadcast([128, 8, 128]), op=ALU.mult)
                c.op("pool", "tensor_tensor", reads=[bkrr, bgkr], writes=[bkrg], out=krg[:], in0=krr[:, ch, :], in1=gkr[:, 128:192],
                     op=ALU.mult)
                self.rope_apply("dve", "pool", krg[:, 0:32], krg[:, 32:64], cst[:, ch, 0:32], cst[:, ch, 32:64], krot[:, 0:32], krot[:, 32:64],
                                rtmp, brtmp, [bkrg, bcos, bsin], [bkrot], [128, 32])
                c.op("dve", "tensor_tensor", reads=[bkrot, bhs], writes=[bkb], out=kb[:, :, 128:192],
                     in0=krot[:].unsqueeze(1).to_broadcast([128, 8, 64]), in1=hs[:].unsqueeze(2).to_broadcast([128, 8, 64]), op=ALU.mult)
                to_T(kb, bkb, KTn, bKTn, KTr, bKTr, ch)
                c.dma("sp", self.KT[:, 0:128, gs].rearrange("h p t -> p h t"), KTn[:], reads=[bKTn], writes=[self.dbuf("KTn", gc)])
                c.dma("sp", self.KT[:, 128:192, gs].rearrange("h p t -> p h t"), KTr[:], reads=[bKTr], writes=[self.dbuf("KTr", gc)])
            cs = slice(j * 512, (j + 1) * 512)
            self.qmem_and_attn(1, win, bwin, 832, moT, bmoT, 0)
            c.dma("sp", self.MO[:, :, cs].rearrange("h p t -> p h t"), moT[:], reads=[bmoT], writes=[self.dbuf("MO", j)])

    def p4_attn(self):
        c = self.c
        S, NT, NCH = self.S, self.NT, self.NCH
        c.barrier()
        c.cp_segs[c.segs[-1]] = True
        kn = Rot([self.T(f"A_kn{i}", [128, S], BF16) for i in range(2)])
        kr = Rot([self.T(f"A_kr{i}", [128, S], BF16) for i in range(2)])
        va = Rot([self.T(f"A_v{i}", [128, NCH, 130], BF16) for i in range(2)])
        qn = Rot([self.T(f"A_qn{i}", [128, 512], BF16) for i in range(2)])
        qr = Rot([self.T(f"A_qr{i}", [128, 512], BF16) for i in range(2)])
        PT = Rot([self.T(f"A_P{i}", [128, 512], BF16) for i in range(8)])
        osb = Rot([self.T(f"A_osb{i}", [128, 4, 128], BF16) for i in range(2)])
        rden = Rot([self.T(f"A_rd{i}", [128, 4], F32) for i in range(2)])
        ot = Rot([self.T(f"A_o{i}", [128, 512], BF16) for i in range(2)])
        tri, btri = self.T("A_tri", [128, 128], BF16)
        c.op("pool", "memset", writes=[btri], ap=tri[:], constant=1.0)
        c.op("pool", "affine_select", reads=[btri], writes=[btri], out=tri[:], in_=tri[:], pattern=[[1, 128]], compare_op=ALU.is_ge,
             fill=0.0, base=0, channel_multiplier=-1)
        for t, b in kr.items + qr.items:
            c.op("pool", "memset", writes=[b], ap=t[:], constant=0.0)
        for t, b in va.items:
            c.op("pool", "memset", writes=[b], ap=t[:, :, 128:130], constant=1.0)
        srot = Rot(self.pmm)
        pwt = self.pw[0]
        osets = [[(pwt[:, 0:512], Buf("oA0", True)), (pwt[:, 512:1024], Buf("oA1", True))],
                 [(self.pT[0][:], Buf("oB0", True)), (self.pmisc[0][:], Buf("oB1", True))]]
        it = 0
        for h in range(8):
            Kn, bKn = kn.next()
            Kr, bKr = kr.next()
            V, bV = va.next()
            c.dma("sp", Kn[:], self.KT[h, 0:128, :], reads=[self.dbuf("KTn", g) for g in range(NCH)], writes=[bKn])
            c.dma("sp", Kr[0:64, :], self.KT[h, 128:192, :], reads=[self.dbuf("KTr", g) for g in range(NCH)], writes=[bKr])
            c.dma("sp", V[:, :, 0:128], self.Vd[:, h * 128:(h + 1) * 128].rearrange("(c p) d -> p c d", p=128),
                  reads=[self.dbuf("Vd", g) for g in range(NCH)], writes=[bV])
            for qt in range(NT):
                Qn, bQn = qn.next()
                Qr, bQr = qr.next()
                cs = slice(qt * 512, (qt + 1) * 512)
                c.dma("sp", Qn[:], self.QT[h, 0:128, cs], reads=[self.dbuf("QTn", g) for g in range(4 * qt, 4 * qt + 4)], writes=[bQn])
                c.dma("sp", Qr[0:64, :], self.QT[h, 128:192, cs], reads=[self.dbuf("QTr", g) for g in range(4 * qt, 4 * qt + 4)], writes=[bQr])
                nk = 4 * (qt + 1)
                oset = osets[it % 2]
                it += 1
                for kc in range(nk):
                    di = max(kc - 4 * qt, 0)
                    q0 = di * 128
                    ps_, bps = srot.next()
                    c.op("pe", "matmul", reads=[bKn, bQn], writes=[bps], out=ps_[:, q0:512], lhsT=Kn[:, kc * 128:(kc + 1) * 128],
                         rhs=Qn[:, q0:512], start=True, stop=False)
                    c.op("pe", "matmul", reads=[bKr, bQr], writes=[bps], out=ps_[:, q0:512], lhsT=Kr[:, kc * 128:(kc + 1) * 128],
                         rhs=Qr[:, q0:512], start=False, stop=True)
                    P, bP = PT.next()
                    c.op("act", "activation", reads=[bps], writes=[bP], out=P[:, q0:512], in_=ps_[:, q0:512], func=AF.Exp)
                    if kc >= 4 * qt:
                        c.op("pool", "tensor_tensor", reads=[bP, btri], writes=[bP], out=P[:, q0:q0 + 128], in0=P[:, q0:q0 + 128],
                             in1=tri[:], op=ALU.mult)
                    for qc in range(di, 4):
                        ob, bob = oset[qc // 2]
                        col = (qc % 2) * 130
                        c.op("pe", "matmul", reads=[bP, bV], writes=[bob], out=ob[:, col:col + 130], lhsT=P[:, qc * 128:(qc + 1) * 128],
                             rhs=V[:, kc, :], start=(kc == 0 and qc % 2 == 0), stop=(kc == 4 * qt + qc), skip_group_check=True)
                rd, brd = rden.next()
                ob_, bob_ = osb.next()
                pt_, bpt = srot.next()
                ptv = pt_[:].bitcast(BF16)
                for qc in range(4):
                    ob, bob = oset[qc // 2]
                    col = (qc % 2) * 130
                    c.op("dve", "reciprocal", reads=[bob], writes=[brd], out=rd[:, qc:qc + 1], in_=ob[:, col + 128:col + 129])
                    c.op("dve", "tensor_scalar", reads=[bob, brd], writes=[bob_], out=ob_[:, qc, :], in0=ob[:, col:col + 128],
                         scalar1=rd[:, qc:qc + 1], scalar2=None, op0=ALU.mult)
                    c.op("pe", "transpose", reads=[bob_, self.identb[1]], writes=[bpt], out=ptv[:, qc * 128:(qc + 1) * 128],
                         in_=ob_[:, qc, :], identity=self.identb[0][:])
                o, bo = ot.next()
                c.op("act", "copy", reads=[bpt], writes=[bo], out=o[:], in_=ptv[:, 0:512])
                c.dma("sp", self.OT[h, :, cs], o[:], reads=[bo], writes=[self.dbuf("OT", (h, qt))])

    def p5_out(self):
        c = self.c
        W = self.W
        wout, bwout = self.T("b_wout", [128, 12, 1024], BF16)
        self.load_w_cast(wout, bwout, W["b_w_out"][0], 12)
        self.alloc_tile_bufs(deep=True)
        for j in range(self.NT):
            self.catT = self.catTr.next()
            catT, bcat = self.catT
            xt, bxt = self.xt.next()
            cs = slice(j * 512, (j + 1) * 512)
            c.dma("sp", xt[:], self.x1[j * 512:(j + 1) * 512, :].rearrange("(c p) d -> p c d", p=128),
                  reads=[self.dbuf("x1", j)], writes=[bxt])
            c.dma("sp", catT[:, 0:8, :], self.OT[:, :, cs].rearrange("h p t -> p h t"),
                  reads=[self.dbuf("OT", (h, j)) for h in range(8)], writes=[bcat])
            c.dma("sp", catT[:, 8:12, :], self.MO[:, :, cs].rearrange("h p t -> p h t"), reads=[self.dbuf("MO", j)], writes=[bcat])
            self.out_proj_norm2_router(1, j, xt, bxt, wout, bwout)


INV_FREQ = (10000.0 ** (-(np.arange(32, dtype=np.float32) * 2.0 / 64))).astype(np.float32)


def relayout_experts(inputs):
    g = np.asarray(inputs["moe_w_gate"]).reshape(2, 32, 8, 128, 256).transpose(0, 1, 3, 2, 4).reshape(2, 4096, 2048)
    u = np.asarray(inputs["moe_w_up"]).reshape(2, 32, 8, 128, 256).transpose(0, 1, 3, 2, 4).reshape(2, 4096, 2048)
    d = np.asarray(inputs["moe_w_down"]).reshape(2, 32, 2, 128, 1024).transpose(0, 1, 3, 2, 4).reshape(2, 4096, 2048)
    return {"moe_wgL": np.ascontiguousarray(g), "moe_wuL": np.ascontiguousarray(u), "moe_wdL": np.ascontiguousarray(d)}


def make_in_maps(inputs, S, ncores):
    maps = []
    inputs = dict(inputs)
    inputs.update(relayout_experts(inputs))
    for b in range(ncores):
        m = {n: np.ascontiguousarray(inputs[n]) for n, _ in WEIGHT_NAMES}
        m["x"] = np.ascontiguousarray(inputs["x"][b])
        m["mem"] = np.ascontiguousarray(inputs["mem"][b])
        m["positions"] = np.ascontiguousarray(inputs["positions"][b]).astype(np.int32)
        m["inv_freq"] = INV_FREQ
        maps.append(m)
    return maps


def kernel(**inputs):
    B, S, _ = inputs["x"].shape
    prog = Prog(S)
    maps = make_in_maps(inputs, S, B)
    res = run_bass_kernel_spmd(prog.nc, maps, core_ids=list(range(B)))
    return np.stack([r["out"] for r in res.results], axis=0).astype(np.float32)
```

```python
import contextlib
import numpy as np
import concourse.bass as bass
import concourse.mybir as mybir
from concourse.bass_utils import run_bass_kernel_spmd

F32 = mybir.dt.float32
BF16 = mybir.dt.bfloat16
I32 = mybir.dt.int32
AF = mybir.ActivationFunctionType
ALU = mybir.AluOpType
AX = mybir.AxisListType

D = 1024
EPS = 1e-6
NE = 32


class Buf:
    __slots__ = ("name", "w", "rl", "psum")

    def __init__(self, name="", psum=False):
        self.name = name
        self.w = None
        self.rl = []
        self.psum = psum


class Node:
    __slots__ = ("eng", "fn", "kw", "sync", "order", "dur", "occ", "isdma", "act_set")


def _fsize(ap):
    try:
        return int(ap.free_size())
    except Exception:
        return 512


class Ctx:
    NDSEM = 24
    import os as _os
    WINDOW = int(_os.environ.get('SCHEDW', '48'))

    def __init__(self, nc):
        self.nc = nc
        self.es = contextlib.ExitStack()
        self.engs = ("pe", "dve", "act", "pool", "sp")
        self.sems = {}
        for k in self.engs:
            self.sems[k] = self.es.enter_context(nc.semaphore("s_" + k))
        self.dsem = {}
        for q in ("sp", "pool", "act"):
            self.dsem[q] = [self.es.enter_context(nc.semaphore(f"d_{q}{i}")) for i in range(self.NDSEM)]
        self.nodes = []
        self.cp_segs = {}
        self.warm_segs = {}
        self.filler_kw = None
        self.segs = [0]
        self.scopes = [self.es]
        self.nalloc = 0
        self.ninst = 0
        self.nwaits = 0

    def sb(self, name, shape, dt):
        self.nalloc += 1
        return self.scopes[-1].enter_context(self.nc.sbuf_tensor(f"{name}_{self.nalloc}", list(shape), dt))

    def ps(self, name, shape, dt=F32):
        return self.es.enter_context(self.nc.psum_tensor(name, list(shape), dt))

    @contextlib.contextmanager
    def scope(self):
        st = contextlib.ExitStack()
        self.scopes.append(st)
        try:
            yield
        finally:
            self.barrier()
            self.scopes.pop()
            st.close()

    def barrier(self):
        if self.segs[-1] != len(self.nodes):
            self.segs.append(len(self.nodes))

    def close(self):
        self.es.close()

    def _record(self, eng, fn, kw, reads, writes, isdma, dur, occ, act_set=None):
        nid = len(self.nodes)
        seg0 = self.segs[-1]
        n = Node()
        n.eng, n.fn, n.kw, n.isdma, n.dur, n.occ, n.act_set = eng, fn, kw, isdma, dur, occ, act_set
        sync, order = set(), set()
        nodes = self.nodes

        def dep(m):
            if m is None or m < seg0:
                return
            mn = nodes[m]
            if eng == "pe" and not isdma and not mn.isdma and mn.eng == "pe":
                order.add(m)
            else:
                sync.add(m)
        for b in reads:
            dep(b.w)
            if b.psum:
                for r in b.rl:
                    if nodes[r].eng != eng:
                        dep(r)
        for b in writes:
            dep(b.w)
            for r in b.rl:
                dep(r)
        n.sync, n.order = sync, order
        nodes.append(n)
        for b in reads:
            b.rl.append(nid)
        for b in writes:
            b.w = nid
            b.rl = []
        self.ninst += 1
        return nid

    def op(self, e, name, reads=(), writes=(), **kw):
        act_set = None
        if e == "pe":
            if name == "matmul":
                nn = _fsize(kw["rhs"])
                f = 4.0 if kw["rhs"].dtype == F32 else 1.0
            else:
                nn = 128
                f = 1.0
            dur = f * max(64, nn) / 2.4 + 8
        elif e == "act":
            dur = (_fsize(kw["out"]) + 200) / 1.2
            fnc = kw.get("func")
            if fnc in (AF.Exp, AF.Gelu, AF.Silu, AF.Sin):
                act_set = fnc
        elif e == "dve":
            nn = _fsize(kw["out"] if "out" in kw else kw["ap"])
            f = 8.0 if name == "reciprocal" else (2.0 if name in ("tensor_tensor", "scalar_tensor_tensor") else 1.0)
            dur = (f * nn + 110) / 0.96
        else:
            nn = _fsize(kw["out"] if "out" in kw else kw["ap"])
            dur = (2.0 * nn + 200) / 0.96 + 400
        self._record(e, name, kw, reads, writes, False, dur, dur, act_set)

    def dma(self, q, out, in_, reads=(), writes=(), **kw):
        kw = dict(kw)
        kw["out"] = out
        kw["in_"] = in_
        try:
            nb = int(out.nbytes())
        except Exception:
            nb = 1 << 16
        occ = 1000.0 if q == "pool" else 70.0
        self._record(q, "dma_start", kw, reads, writes, True, 2200.0 + nb / 120.0, occ)

    def idma(self, out, out_off, in_, in_off, reads=(), writes=(), **extra):
        kw = dict(out=out, out_offset=out_off, in_=in_, in_offset=in_off)
        kw.update(extra)
        self._record("pool", "indirect_dma_start", kw, reads, writes, True, 4000.0, 1500.0)

    def wait_all(self, e, bufs):
        pass

    def _schedule_segment(self, a, b, order_out):
        nodes = self.nodes
        W = self.WINDOW
        pend = {e: [] for e in self.engs}
        succ_eng = {}
        for i in range(a, b):
            pend[nodes[i].eng].append(i)
        for i in range(a, b):
            for d in nodes[i].sync | nodes[i].order:
                succ_eng.setdefault(d, set()).add(nodes[i].eng)
        cp = {}
        for i in range(b - 1, a - 1, -1):
            cp[i] = cp.get(i, 0.0) + nodes[i].dur
            for d in nodes[i].sync | nodes[i].order:
                if d >= a and cp[i] > cp.get(d, 0.0):
                    cp[d] = cp[i]
        head = {e: 0 for e in self.engs}
        done = set()
        finish = {}
        etime = {e: 0.0 for e in self.engs}
        last_act = [None]
        cache = {e: None for e in self.engs}
        dirty = set(self.engs)
        remaining = b - a
        while remaining:
            for e in dirty:
                lst = pend[e]
                h = head[e]
                while h < len(lst) and lst[h] in done:
                    h += 1
                head[e] = h
                best = None
                cnt = 0
                k = h
                while k < len(lst) and cnt < W:
                    i = lst[k]
                    k += 1
                    if i in done:
                        continue
                    cnt += 1
                    nd = nodes[i]
                    ok = True
                    st = etime[e]
                    for d in nd.sync:
                        if d not in done:
                            ok = False
                            break
                        t = finish[d] + 80.0
                        if t > st:
                            st = t
                    if not ok:
                        continue
                    for d in nd.order:
                        if d not in done:
                            ok = False
                            break
                    if not ok:
                        continue
                    if e == "act" and nd.act_set is not None and nd.act_set != last_act[0]:
                        st += 1300.0
                    key = (st, -cp[i] if self.cp_segs.get(a, False) else 0.0)
                    if best is None or key < best[2]:
                        best = (st, i, key)
                cache[e] = best
            dirty = set()
            pick = None
            for e in self.engs:
                c_ = cache[e]
                if c_ is not None and (pick is None or c_[0] < pick[0]):
                    pick = (c_[0], c_[1], e)
            st, i, e = pick
            nd = nodes[i]
            if e == "pe" and self.filler_kw is not None and self.warm_segs.get(a, False):
                gap = st - etime["pe"]
                if 350.0 < gap:
                    nfill = min(int(gap * 0.9 / 230.0), 48)
                    for _ in range(nfill):
                        f = Node()
                        f.eng, f.fn, f.kw, f.isdma, f.dur, f.occ, f.act_set = "pe", "matmul", self.filler_kw, False, 230.0, 230.0, None
                        f.sync, f.order = set(), set()
                        nodes.append(f)
                        order_out["pe"].append(len(nodes) - 1)
                        self.nfill = getattr(self, "nfill", 0) + 1
            done.add(i)
            order_out[e].append(i)
            finish[i] = st + nd.dur
            etime[e] = st + nd.occ
            if e == "act" and nd.act_set is not None:
                last_act[0] = nd.act_set
            dirty.add(e)
            for se in succ_eng.get(i, ()):
                dirty.add(se)
            remaining -= 1

    def emit(self):
        nodes = self.nodes
        segs = self.segs + ([len(nodes)] if self.segs[-1] != len(nodes) else [])
        prog = {e: [] for e in self.engs}
        cnt = {e: 0 for e in self.engs}
        seen = {e: {} for e in self.engs}
        duse = {q: [0] * self.NDSEM for q in self.dsem}
        dn = {q: 0 for q in self.dsem}
        tok = {}

        def wait(e, key, val):
            if seen[e].get(key, 0) >= val:
                return
            semobj = self.dsem[key[0]][key[1]] if isinstance(key, tuple) else self.sems[key]
            prog[e].append((None, semobj, val))
            seen[e][key] = val
            self.nwaits += 1

        def full_barrier():
            for e in self.engs:
                for f in self.engs:
                    if f != e and cnt[f]:
                        wait(e, f, cnt[f])
                for q in self.dsem:
                    for slot in range(self.NDSEM):
                        if duse[q][slot]:
                            wait(e, (q, slot), 16 * duse[q][slot])

        for si in range(len(segs) - 1):
            a, b = segs[si], segs[si + 1]
            order = {e: [] for e in self.engs}
            self._schedule_segment(a, b, order)
            for e in self.engs:
                c0 = cnt[e]
                d0 = dn.get(e, 0)
                du = list(duse[e]) if e in duse else None
                for i in order[e]:
                    nd = nodes[i]
                    if nd.isdma:
                        slot = d0 % self.NDSEM
                        d0 += 1
                        du[slot] += 1
                        tok[i] = ((e, slot), 16 * du[slot])
                    else:
                        c0 += 1
                        tok[i] = (e, c0)
            for e in self.engs:
                for i in order[e]:
                    nd = nodes[i]
                    for d in nd.sync:
                        k_, v_ = tok[d]
                        wait(e, k_, v_)
                    if nd.isdma:
                        slot = dn[e] % self.NDSEM
                        dn[e] += 1
                        prev = duse[e][slot]
                        if prev:
                            wait(e, (e, slot), 16 * prev)
                        duse[e][slot] = prev + 1
                        prog[e].append(((nd.fn, nd.kw), self.dsem[e][slot], 16))
                    else:
                        cnt[e] += 1
                        prog[e].append(((nd.fn, nd.kw), self.sems[e], 1))
            full_barrier()

        def run(eng, items):
            regs = {}
            for fn, sem, v in items:
                if fn is None:
                    eng.wait_ge(sem, v)
                else:
                    kw = fn[1]
                    bc = kw.get("bounds_check")
                    if isinstance(bc, int):
                        if bc not in regs:
                            regs[bc] = eng.to_reg(bc)
                        kw = dict(kw)
                        kw["bounds_check"] = regs[bc]
                    getattr(eng, fn[0])(**kw).then_inc(sem, v)

        with self.nc.Block() as block:
            @block.tensor
            def _(e):
                run(e, prog["pe"])

            @block.vector
            def _(e):
                run(e, prog["dve"])

            @block.scalar
            def _(e):
                run(e, prog["act"])

            @block.gpsimd
            def _(e):
                run(e, prog["pool"])

            @block.sync
            def _(e):
                run(e, prog["sp"])


class Rot:
    def __init__(self, items):
        self.items = items
        self.i = 0

    def next(self):
        r = self.items[self.i % len(self.items)]
        self.i += 1
        return r


WEIGHT_NAMES = [
    ("mem_norm_g", [1024]), ("w_mem_kv", [1024, 1024]), ("mem_qn_g", [2, 128]), ("mem_kn_g", [2, 128]),
    ("norm1_g", [2, 1024]), ("norm2_g", [2, 1024]),
    ("a_w_in", [1, 1024, 2560]), ("a_ln_g", [1, 1024]), ("a_ln_b", [1, 1024]), ("a_w_s", [1, 8, 128, 128]),
    ("a_b_s", [1, 8, 128]), ("a_w_out", [1, 1536, 1024]),
    ("b_w_in", [1, 1024, 1344]), ("b_q_norm_g", [1, 512]), ("b_kv_norm_g", [1, 256]),
    ("b_w_q_up", [1, 512, 1536]), ("b_w_kv_up", [1, 256, 2048]), ("b_qn_g", [1, 192]), ("b_kn_g", [1, 192]),
    ("b_w_out", [1, 1536, 1024]),
    ("moe_w_group", [2, 1024, 4]), ("moe_b_group", [2, 4]), ("moe_w_expert", [2, 1024, 32]),
    ("moe_b_expert", [2, 32]),
    ("moe_wgL", [2, 4096, 2048]), ("moe_wuL", [2, 4096, 2048]), ("moe_wdL", [2, 4096, 2048]),
]


class Prog:
    def __init__(self, S, phases=("p0", "p1", "p2", "p3", "p4", "p5", "p6"), dbg=()):
        self.S = S
        self.NT = S // 512
        self.NCH = S // 128
        self.ST = min(2048, S)
        self.phases = phases
        nc = self.nc = bass.Bass("TRN2", target_bir_lowering=False)
        c = self.c = Ctx(nc)
        self.W = {}
        self.x = nc.dram_tensor("x", [S, D], F32, kind="ExternalInput").ap()
        self.mem = nc.dram_tensor("mem", [256, D], F32, kind="ExternalInput").ap()
        self.pos = nc.dram_tensor("positions", [S], I32, kind="ExternalInput").ap()
        self.invf = nc.dram_tensor("inv_freq", [32], F32, kind="ExternalInput").ap()
        for n, shp in WEIGHT_NAMES:
            self.W[n] = nc.dram_tensor(n, shp, F32, kind="ExternalInput").ap()
        self.out = nc.dram_tensor("out", [S, D], F32, kind="ExternalOutput").ap()
        self.db = {}
        self.dbg = dbg

        def scratch(name, shape, dt):
            kind = "ExternalOutput" if name in dbg else "Internal"
            return nc.dram_tensor(name, list(shape), dt, kind=kind).ap()

        self.xmid = [scratch("xmid0", [S, D], F32), scratch("xmid1", [S, D], F32)]
        self.x1 = scratch("x1", [S, D], F32)
        self.h2T = [scratch("h2T0", [8, 128, S], BF16), scratch("h2T1", [8, 128, S], BF16)]
        self.comb = [scratch("comb0", [S, NE], F32), scratch("comb1", [S, NE], F32)]
        self.NTILES = (2 * S) // 512 + 32
        self.NCAP = self.NTILES * 512
        self.rinfo = [scratch(f"rinfo{i}", [S, 66], F32) for i in range(2)]
        self.h2b = [scratch(f"h2b{i}", [S, D], BF16) for i in range(2)]
        xs_shared = scratch("Xs0", [self.NCAP, D], BF16)
        self.Xs = [xs_shared, xs_shared]
        self.Ys = [scratch(f"Ys{i}", [self.NCAP, D], BF16) for i in range(2)]
        self.QT = scratch("QT", [8, 192, S], BF16)
        self.KT = scratch("KT", [8, 192, S], BF16)
        self.Vd = scratch("Vd", [S, 1024], BF16)
        self.OT = scratch("OT", [8, 128, S], BF16)
        self.MO = scratch("MO", [4, 128, S], BF16)

        self.pmm = [(c.ps(f"pmm{i}", [128, 512], F32), Buf(f"pmm{i}", True)) for i in range(4)]
        self.pw = (c.ps("pw", [128, 1024], F32), Buf("pw", True))
        self.pT = (c.ps("pT", [128, 512], F32), Buf("pT", True))
        self.pmisc = (c.ps("pmisc", [128, 512], F32), Buf("pmisc", True))
        self.mmrot = Rot(self.pmm)

        self.setup_consts()
        self.zf = [[], []]
        for ph, fn in (("p0", self.p0_memkv), ("p1", self.p1_gmlp), ("p2", lambda: self.moe_sparse(0, self.xmid[0], self.x1)),
                       ("p3", self.p3_mla_proj), ("p4", self.p4_attn), ("p5", self.p5_out),
                       ("p6", lambda: self.moe_sparse(1, self.xmid[1], self.out))):
            if ph in phases:
                with c.scope():
                    fn()
        c.wait_all("sp", list(self.db.values()))
        c.emit()
        c.close()

    def dbuf(self, name, idx):
        k = (name, idx)
        if k not in self.db:
            self.db[k] = Buf(f"{name}{idx}")
        return self.db[k]

    def T(self, name, shape, dt):
        return (self.c.sb(name, shape, dt), Buf(name))

    def rsqrt_(self, t_ap, bt, scale, n):
        c = self.c
        c.op("pool", "tensor_scalar", reads=[bt], writes=[bt], out=t_ap, in0=t_ap, scalar1=scale, scalar2=EPS,
             op0=ALU.mult, op1=ALU.add)
        c.op("pool", "tensor_tensor", reads=[bt, self.mh[1]], writes=[bt], out=t_ap, in0=t_ap,
             in1=self.mh[0][:, 0:n], op=ALU.pow)

    def load_small_cols(self, name, src1d, k):
        t, b = self.T(name, [128, k], F32)
        self.c.dma("sp", t[:], src1d.rearrange("(k p) -> p k", p=128), writes=[b], allow_slow_non_contiguous=True)
        return t, b

    def load_bcast(self, name, src1d, n):
        t, b = self.T(name, [128, n], F32)
        self.c.dma("sp", t[:], src1d.partition_broadcast(128), writes=[b])
        return t, b

    def load_w_cast(self, dst, bdst, src2d, K):
        for k in range(K):
            self.c.dma("pool", dst[:, k, :], src2d[k * 128:(k + 1) * 128, :], writes=[bdst])

    def load_w_gain(self, dst, bdst, src2d, K, N, gain, bgain):
        c = self.c
        for k in range(K):
            st, bst = self.wstage.next()
            c.dma("sp", st[:, 0:N], src2d[k * 128:(k + 1) * 128, :], writes=[bst])
            c.op("dve", "tensor_scalar", reads=[bst, bgain], writes=[bdst], out=dst[:, k, :], in0=st[:, 0:N],
                 scalar1=gain[:, k:k + 1], scalar2=None, op0=ALU.mult)

    def setup_consts(self):
        c = self.c
        self.identb = self.T("identb", [128, 128], BF16)
        self.identf = self.T("identf", [128, 128], F32)
        self.onesb = self.T("onesb", [128, 128], BF16)
        self.mh = self.T("mh", [128, 16], F32)
        for t, b in (self.identb, self.identf):
            c.op("pool", "memset", writes=[b], ap=t[:], constant=0.0)
            c.op("pool", "affine_select", reads=[b], writes=[b], out=t[:], in_=t[:], pattern=[[-1, 128]],
                 compare_op=ALU.not_equal, fill=1.0, base=0, channel_multiplier=1)
        c.op("pool", "memset", writes=[self.onesb[1]], ap=self.onesb[0][:], constant=1.0)
        c.op("pool", "memset", writes=[self.mh[1]], ap=self.mh[0][:], constant=-0.5)
        self.kT = [self.T(f"kT{i}", [128, 4, 256], BF16) for i in range(2)]
        self.Vaug = self.T("Vaug", [128, 2, 4, 130], BF16)
        self.load_router_weights()
        self.fillt = self.T("fillt", [128, 512], BF16)
        c.op("pool", "memset", writes=[self.fillt[1]], ap=self.fillt[0][:], constant=0.0)
        c.barrier()
        c.filler_kw = dict(out=self.pmisc[0][:], lhsT=self.identb[0][:], rhs=self.fillt[0][:], start=True, stop=True)

    def p0_memkv(self):
        c = self.c
        W = self.W
        g, bg = self.load_small_cols("memg", W["mem_norm_g"], 8)
        self.wstage = Rot([self.T(f"wstage{i}", [128, 2560], F32) for i in range(2)])
        wkv, bwkv = self.T("wkv", [128, 8, 1024], BF16)
        self.load_w_gain(wkv, bwkv, W["w_mem_kv"], 8, 1024, g, bg)
        gq, bgq = self.T("gq", [128, 2], F32)
        gk, bgk = self.T("gk", [128, 2], F32)
        c.dma("sp", gq[:], W["mem_qn_g"].rearrange("l d -> d l"), writes=[bgq], allow_slow_non_contiguous=True)
        c.dma("sp", gk[:], W["mem_kn_g"].rearrange("l d -> d l"), writes=[bgk], allow_slow_non_contiguous=True)
        c.op("dve", "scalar_tensor_tensor", reads=[bgq, bgk], writes=[bgq], out=gq[:], in0=gq[:],
             scalar=float(128 ** -0.5), in1=gk[:], op0=ALU.mult, op1=ALU.mult)
        mt, bmt = self.T("memt", [128, 2, 1024], F32)
        c.dma("sp", mt[:], self.mem.rearrange("(c p) d -> p c d", p=128), writes=[bmt])
        ms, bms = self.T("mems", [128, 1024], BF16)
        junk, bjunk = self.T("junk0", [128, 1024], BF16)
        ss, bss = self.T("ss0", [128, 2], F32)
        mT, bmT = self.T("memT", [128, 8, 256], BF16)
        pTv = self.pT[0][:].bitcast(BF16)
        bpT = self.pT[1]
        for ch in range(2):
            c.op("act", "activation", reads=[bmt], writes=[bjunk, bss], out=junk[:], in_=mt[:, ch, :], func=AF.Square,
                 accum_out=ss[:, ch:ch + 1])
        self.rsqrt_(ss[:], bss, 1.0 / D, 2)
        for ch in range(2):
            c.op("dve", "tensor_scalar", reads=[bmt, bss], writes=[bms], out=ms[:], in0=mt[:, ch, :],
                 scalar1=ss[:, ch:ch + 1], scalar2=None, op0=ALU.mult)
            for k in range(8):
                c.op("pe", "transpose", reads=[bms, self.identb[1]], writes=[bpT], out=pTv[:, k * 128:(k + 1) * 128],
                     in_=ms[:, k * 128:(k + 1) * 128], identity=self.identb[0][:])
            c.op("act", "copy", reads=[bpT], writes=[bmT], out=mT[:, :, ch * 128:(ch + 1) * 128],
                 in_=pTv.rearrange("p (k t) -> p k t", k=8))
        c.op("pool", "memset", writes=[self.Vaug[1]], ap=self.Vaug[0][:, :, :, 128:130], constant=1.0)
        kss, bkss = self.T("kss", [128, 4], F32)
        kn, bkn = self.T("kn", [128, 4, 128], BF16)
        for ch in range(2):
            pk, bpk = self.mmrot.next()
            for k in range(8):
                c.op("pe", "matmul", reads=[bmT, bwkv], writes=[bpk], out=pk[:], lhsT=mT[:, k, ch * 128:(ch + 1) * 128],
                     rhs=wkv[:, k, 0:512], start=(k == 0), stop=(k == 7))
            for h in range(4):
                c.op("act", "activation", reads=[bpk], writes=[bjunk, bkss], out=junk[:, 0:128], in_=pk[:, h * 128:(h + 1) * 128],
                     func=AF.Square, accum_out=kss[:, h:h + 1])
            self.rsqrt_(kss[:], bkss, 1.0 / 128, 4)
            c.op("dve", "tensor_tensor", reads=[bpk, bkss], writes=[bkn], out=kn[:],
                 in0=pk[:].rearrange("p (h d) -> p h d", h=4), in1=kss[:].unsqueeze(2).to_broadcast([128, 4, 128]), op=ALU.mult)
            for h in range(4):
                c.op("pe", "transpose", reads=[bkn, self.identb[1]], writes=[bpT], out=pTv[:, h * 128:(h + 1) * 128],
                     in_=kn[:, h, :], identity=self.identb[0][:])
            for li in range(2):
                c.op("dve", "tensor_scalar", reads=[bpT, bgq], writes=[self.kT[li][1]],
                     out=self.kT[li][0][:, :, ch * 128:(ch + 1) * 128], in0=pTv[:, 0:512].rearrange("p (h m) -> p h m", h=4),
                     scalar1=gq[:, li:li + 1], scalar2=None, op0=ALU.mult)
            pv, bpv = self.mmrot.next()
            for k in range(8):
                c.op("pe", "matmul", reads=[bmT, bwkv], writes=[bpv], out=pv[:], lhsT=mT[:, k, ch * 128:(ch + 1) * 128],
                     rhs=wkv[:, k, 512:1024], start=(k == 0), stop=(k == 7))
            c.op("act", "copy", reads=[bpv], writes=[self.Vaug[1]], out=self.Vaug[0][:, ch, :, 0:128],
                 in_=pv[:].rearrange("p (h d) -> p h d", h=4))

    def alloc_tile_bufs(self, full=True, deep=False, nht=2):
        self.xt = Rot([self.T(f"xt{i}", [128, 4, 1024], F32) for i in range((3 if deep else 2) if full else 1)])
        self.hTrot = Rot([self.T(f"hT{i}", [128, 8, 512], BF16) for i in range(nht)])
        self.hT = self.hTrot.items[0]
        self.xs = Rot([self.T(f"xs{i}", [128, 1024], BF16) for i in range(2)])
        self.junk = self.T("junk", [128, 1024], BF16)
        self.ssn = self.T("ssn", [128, 4], F32)
        self.qT = self.T("qT", [128, 4, 512], BF16)
        self.qn = Rot([self.T(f"qn{i}", [128, 4, 128], BF16) for i in range(2)])
        self.qss = Rot([self.T(f"qss{i}", [128, 4], F32) for i in range(2)])
        self.PTm = Rot([self.T(f"PTm{i}", [128, 512], BF16) for i in range(3)])
        self.mrd = Rot([self.T(f"mrd{i}", [128, 4], F32) for i in range(2)])
        self.mon = Rot([self.T(f"mon{i}", [128, 4, 128], BF16) for i in range(2)])
        if not full:
            return
        self.catTr = Rot([self.T(f"catT{i}", [128, 12, 512], BF16) for i in range(2 if deep else 1)])
        self.catT = self.catTr.items[0]
        self.h2 = Rot([self.T(f"h2_{i}", [128, 1024], F32) for i in range(2 if deep else 1)])
        self.h2Tf = Rot([self.T(f"h2Tf{i}", [128, 8, 128], F32) for i in range(2 if deep else 1)])
        self.h2bt = Rot([self.T(f"h2bt{i}", [128, 1024], BF16) for i in range(2)])
        self.rt = Rot([dict((n, self.T(f"rt_{n}{i}", [128, w], F32)) for n, w in
                            (("lg", 36), ("gmax", 1), ("ngmax", 1), ("gexp", 4), ("gsum", 1), ("oh", 4), ("tmp", 32),
                             ("esel", 8), ("m1", 1), ("nm1", 1), ("sel1", 8), ("es2", 8), ("m2", 1), ("sel2", 8),
                             ("p2", 1), ("w1", 1), ("w2", 1), ("R", 66))) for i in range(4)])

    def norm_to_hT(self, xt, bxt):
        c = self.c
        ss, bss = self.ssn
        junk, bjunk = self.junk
        self.hT = self.hTrot.next()
        hT, bhT = self.hT
        pTv = self.pT[0][:].bitcast(BF16)
        bpT = self.pT[1]
        for ch in range(4):
            c.op("act", "activation", reads=[bxt], writes=[bjunk, bss], out=junk[:], in_=xt[:, ch, :], func=AF.Square,
                 accum_out=ss[:, ch:ch + 1])
        self.rsqrt_(ss[:], bss, 1.0 / D, 4)
        for ch in range(4):
            xs, bxs = self.xs.next()
            c.op("dve", "tensor_scalar", reads=[bxt, bss], writes=[bxs], out=xs[:], in0=xt[:, ch, :],
                 scalar1=ss[:, ch:ch + 1], scalar2=None, op0=ALU.mult)
            for k in range(8):
                c.op("pe", "transpose", reads=[bxs, self.identb[1]], writes=[bpT], out=pTv[:, k * 128:(k + 1) * 128],
                     in_=xs[:, k * 128:(k + 1) * 128], identity=self.identb[0][:])
            c.op("act", "copy", reads=[bpT], writes=[bhT], out=hT[:, :, ch * 128:(ch + 1) * 128],
                 in_=pTv.rearrange("p (k t) -> p k t", k=8))

    def qmem_and_attn(self, li, win, bwin, col0, dst, bdst, dst_k0):
        c = self.c
        hT, bhT = self.hT
        qT, bqT = self.qT
        junk, bjunk = self.junk
        pTv = self.pT[0][:].bitcast(BF16)
        bpT = self.pT[1]
        for ch in range(4):
            pq, bpq = self.mmrot.next()
            for k in range(8):
                c.op("pe", "matmul", reads=[bhT, bwin], writes=[bpq], out=pq[:], lhsT=hT[:, k, ch * 128:(ch + 1) * 128],
                     rhs=win[:, k, col0:col0 + 512], start=(k == 0), stop=(k == 7))
            qss, bqss = self.qss.next()
            for h in range(4):
                c.op("act", "activation", reads=[bpq], writes=[bjunk, bqss], out=junk[:, 0:128], in_=pq[:, h * 128:(h + 1) * 128],
                     func=AF.Square, accum_out=qss[:, h:h + 1])
            self.rsqrt_(qss[:], bqss, 1.0 / 128, 4)
            qn, bqn = self.qn.next()
            c.op("dve", "tensor_tensor", reads=[bpq, bqss], writes=[bqn], out=qn[:],
                 in0=pq[:].rearrange("p (h d) -> p h d", h=4), in1=qss[:].unsqueeze(2).to_broadcast([128, 4, 128]), op=ALU.mult)
            for h in range(4):
                c.op("pe", "transpose", reads=[bqn, self.identb[1]], writes=[bpT], out=pTv[:, h * 128:(h + 1) * 128],
                     in_=qn[:, h, :], identity=self.identb[0][:])
            c.op("act", "copy", reads=[bpT], writes=[bqT], out=qT[:, :, ch * 128:(ch + 1) * 128],
                 in_=pTv[:, 0:512].rearrange("p (h t) -> p h t", h=4))
        kT, bkT = self.kT[li]
        Va, bVa = self.Vaug
        for h in range(4):
            pts = []
            for mc in range(2):
                psc, bpsc = self.mmrot.next()
                c.op("pe", "matmul", reads=[bkT, bqT], writes=[bpsc], out=psc[:], lhsT=kT[:, h, mc * 128:(mc + 1) * 128],
                     rhs=qT[:, h, :], start=True, stop=True)
                PT, bPT = self.PTm.next()
                c.op("act", "activation", reads=[bpsc], writes=[bPT], out=PT[:], in_=psc[:], func=AF.Exp)
                pts.append((PT, bPT))
            banks = [self.mmrot.next(), self.mmrot.next()]
            for qc in range(4):
                ob, bob = banks[qc // 2]
                col = (qc % 2) * 130
                for mc in range(2):
                    c.op("pe", "matmul", reads=[bVa, pts[mc][1]], writes=[bob], out=ob[:, col:col + 130],
                         lhsT=pts[mc][0][:, qc * 128:(qc + 1) * 128], rhs=Va[:, mc, h, :], start=(mc == 0 and qc % 2 == 0),
                         stop=(mc == 1), skip_group_check=True)
            rd, brd = self.mrd.next()
            on, bon = self.mon.next()
            for qc in range(4):
                ob, bob = banks[qc // 2]
                col = (qc % 2) * 130
                c.op("dve", "reciprocal", reads=[bob], writes=[brd], out=rd[:, qc:qc + 1], in_=ob[:, col + 128:col + 129])
                c.op("dve", "tensor_scalar", reads=[bob, brd], writes=[bon], out=on[:, qc, :], in0=ob[:, col:col + 128],
                     scalar1=rd[:, qc:qc + 1], scalar2=None, op0=ALU.mult)
                c.op("pe", "transpose", reads=[bon, self.identb[1]], writes=[bpT], out=pTv[:, qc * 128:(qc + 1) * 128],
                     in_=on[:, qc, :], identity=self.identb[0][:])
            c.op("act", "copy", reads=[bpT], writes=[bdst], out=dst[:, dst_k0 + h, :], in_=pTv[:, 0:512])

    def out_proj_norm2_router(self, li, j, xt, bxt, wout, bwout):
        c = self.c
        catT, bcat = self.catT
        S = self.S
        junk, bjunk = self.junk
        for ch in range(4):
            for half in range(2):
                py, bpy = self.mmrot.next()
                for k in range(12):
                    c.op("pe", "matmul", reads=[bcat, bwout], writes=[bpy], out=py[:], lhsT=catT[:, k, ch * 128:(ch + 1) * 128],
                         rhs=wout[:, k, half * 512:(half + 1) * 512], start=(k == 0), stop=(k == 11))
                c.op("dve", "tensor_tensor", reads=[bpy, bxt], writes=[bxt], out=xt[:, ch, half * 512:(half + 1) * 512],
                     in0=py[:], in1=xt[:, ch, half * 512:(half + 1) * 512], op=ALU.add)
        c.dma("sp", self.xmid[li][j * 512:(j + 1) * 512, :].rearrange("(c p) d -> p c d", p=128), xt[:], reads=[bxt],
              writes=[self.dbuf(f"xmid{li}", j)])
        import os
        STG = float(os.environ.get("STG", "99"))
        if STG < 6:
            return
        ss, bss = self.ssn
        for ch in range(4):
            c.op("act", "activation", reads=[bxt], writes=[bjunk, bss], out=junk[:], in_=xt[:, ch, :], func=AF.Square,
                 accum_out=ss[:, ch:ch + 1])
        self.rsqrt_(ss[:], bss, 1.0 / D, 4)
        g2, bg2 = self.g2bc[li]
        wr, bwr = self.wr[li]
        rb, brb = self.rbias[li]
        pw, bpw = self.pw
        for ch in range(4):
            h2, bh2 = self.h2.next()
            c.op("dve", "scalar_tensor_tensor", reads=[bxt, bss, bg2], writes=[bh2], out=h2[:], in0=xt[:, ch, :],
                 scalar=ss[:, ch:ch + 1], in1=g2[:], op0=ALU.mult, op1=ALU.mult)
            if STG < 6.2:
                continue
            for k in range(8):
                c.op("pe", "transpose", reads=[bh2, self.identf[1]], writes=[bpw], out=pw[:, k * 128:(k + 1) * 128],
                     in_=h2[:, k * 128:(k + 1) * 128], identity=self.identf[0][:])
            if STG < 6.4:
                continue
            h2Tf, bh2Tf = self.h2Tf.next()
            c.op("act", "copy", reads=[bpw], writes=[bh2Tf], out=h2Tf[:], in_=pw[:].rearrange("p (k t) -> p k t", k=8))
            hb, bhb = self.h2bt.next()
            c.op("pool", "tensor_copy", reads=[bh2], writes=[bhb], out=hb[:], in_=h2[:])
            gch_ = j * 4 + ch
            c.dma("sp", self.h2b[li][gch_ * 128:(gch_ + 1) * 128, :], hb[:], reads=[bhb], writes=[self.dbuf(f"h2b{li}", gch_)])
            if STG < 7:
                continue
            pl, bpl = self.mmrot.next()
            for k in range(8):
                c.op("pe", "matmul", reads=[bh2Tf, bwr], writes=[bpl], out=pl[:, 0:36], lhsT=h2Tf[:, k, :], rhs=wr[:, k, :],
                     start=(k == 0), stop=(k == 7))
            if STG < 8:
                continue
            self.router(li, j * 4 + ch, pl, bpl, rb, brb)

    def router(self, li, gch, pl, bpl, rb, brb):
        c = self.c
        R = self.rt.next()

        def t(n):
            return R[n][0]

        def b(n):
            return R[n][1]
        c.op("dve", "tensor_tensor", reads=[bpl, brb], writes=[b("lg")], out=t("lg")[:], in0=pl[:, 0:36], in1=rb[:], op=ALU.add)
        gl = t("lg")[:, 0:4]
        el = t("lg")[:, 4:36]
        c.op("dve", "tensor_reduce", reads=[b("lg")], writes=[b("gmax")], out=t("gmax")[:], in_=gl, axis=AX.X, op=ALU.max)
        c.op("dve", "tensor_scalar", reads=[b("gmax")], writes=[b("ngmax")], out=t("ngmax")[:], in0=t("gmax")[:], scalar1=-1.0,
             scalar2=None, op0=ALU.mult)
        c.op("act", "activation", reads=[b("lg"), b("ngmax")], writes=[b("gexp"), b("gsum")], out=t("gexp")[:], in_=gl,
             func=AF.Exp, bias=t("ngmax")[:], accum_out=t("gsum")[:])
        c.op("dve", "reciprocal", reads=[b("gsum")], writes=[b("gsum")], out=t("gsum")[:], in_=t("gsum")[:])
        c.op("dve", "tensor_scalar", reads=[b("lg"), b("gmax")], writes=[b("oh")], out=t("oh")[:], in0=gl, scalar1=t("gmax")[:],
             scalar2=None, op0=ALU.is_equal)
        c.op("dve", "tensor_tensor", reads=[b("lg"), b("oh")], writes=[b("tmp")], out=t("tmp")[:].rearrange("p (g e) -> p g e", g=4),
             in0=el.rearrange("p (g e) -> p g e", g=4), in1=t("oh")[:].unsqueeze(2).to_broadcast([128, 4, 8]), op=ALU.mult)
        c.op("dve", "tensor_reduce", reads=[b("tmp")], writes=[b("esel")], out=t("esel")[:],
             in_=t("tmp")[:].rearrange("p (g e) -> p e g", g=4), axis=AX.X, op=ALU.add)
        c.op("dve", "tensor_reduce", reads=[b("esel")], writes=[b("m1")], out=t("m1")[:], in_=t("esel")[:], axis=AX.X, op=ALU.max)
        c.op("dve", "tensor_scalar", reads=[b("esel"), b("m1")], writes=[b("sel1")], out=t("sel1")[:], in0=t("esel")[:],
             scalar1=t("m1")[:], scalar2=None, op0=ALU.is_equal)
        c.op("dve", "scalar_tensor_tensor", reads=[b("sel1"), b("esel")], writes=[b("es2")], out=t("es2")[:], in0=t("sel1")[:],
             scalar=-1e30, in1=t("esel")[:], op0=ALU.mult, op1=ALU.add)
        c.op("dve", "tensor_reduce", reads=[b("es2")], writes=[b("m2")], out=t("m2")[:], in_=t("es2")[:], axis=AX.X, op=ALU.max)
        c.op("dve", "tensor_scalar", reads=[b("es2"), b("m2")], writes=[b("sel2")], out=t("sel2")[:], in0=t("es2")[:],
             scalar1=t("m2")[:], scalar2=None, op0=ALU.is_equal)
        c.op("dve", "tensor_scalar", reads=[b("m1")], writes=[b("nm1")], out=t("nm1")[:], in0=t("m1")[:], scalar1=-1.0,
             scalar2=None, op0=ALU.mult)
        c.op("act", "activation", reads=[b("m2"), b("nm1")], writes=[b("p2")], out=t("p2")[:], in_=t("m2")[:], func=AF.Exp,
             bias=t("nm1")[:])
        c.op("dve", "tensor_scalar", reads=[b("p2")], writes=[b("w1")], out=t("w1")[:], in0=t("p2")[:], scalar1=1.0, scalar2=None,
             op0=ALU.add)
        c.op("dve", "reciprocal", reads=[b("w1")], writes=[b("w1")], out=t("w1")[:], in_=t("w1")[:])
        c.op("dve", "tensor_tensor", reads=[b("w1"), b("gsum")], writes=[b("w1")], out=t("w1")[:], in0=t("w1")[:], in1=t("gsum")[:],
             op=ALU.mult)
        c.op("dve", "tensor_tensor", reads=[b("w1"), b("p2")], writes=[b("w2")], out=t("w2")[:], in0=t("w1")[:], in1=t("p2")[:],
             op=ALU.mult)
        ohb = t("oh")[:].unsqueeze(2).to_broadcast([128, 4, 8])
        c.op("dve", "tensor_tensor", reads=[b("oh"), b("sel1")], writes=[b("R")], out=t("R")[:, 0:32].rearrange("p (g e) -> p g e", g=4),
             in0=ohb, in1=t("sel1")[:].unsqueeze(1).to_broadcast([128, 4, 8]), op=ALU.mult)
        c.op("dve", "tensor_tensor", reads=[b("oh"), b("sel2")], writes=[b("R")], out=t("R")[:, 32:64].rearrange("p (g e) -> p g e", g=4),
             in0=ohb, in1=t("sel2")[:].unsqueeze(1).to_broadcast([128, 4, 8]), op=ALU.mult)
        c.op("dve", "tensor_copy", reads=[b("w1")], writes=[b("R")], out=t("R")[:, 64:65], in_=t("w1")[:])
        c.op("dve", "tensor_copy", reads=[b("w2")], writes=[b("R")], out=t("R")[:, 65:66], in_=t("w2")[:])
        c.dma("sp", self.rinfo[li][gch * 128:(gch + 1) * 128, :], t("R")[:], reads=[b("R")], writes=[self.dbuf(f"rinfo{li}", gch)])

    def load_router_weights(self):
        if hasattr(self, "g2bc"):
            return
        c = self.c
        W = self.W
        self.g2bc, self.wr, self.rbias = [], [], []
        for li in range(2):
            self.g2bc.append(self.load_bcast(f"g2bc{li}", W["norm2_g"][li], 1024))
            wr, bwr = self.T(f"wr{li}", [128, 8, 36], F32)
            c.dma("sp", wr[:, :, 0:4], W["moe_w_group"][li].rearrange("(k p) n -> p k n", p=128), writes=[bwr])
            c.dma("sp", wr[:, :, 4:36], W["moe_w_expert"][li].rearrange("(k p) n -> p k n", p=128), writes=[bwr])
            self.wr.append((wr, bwr))
            rb, brb = self.T(f"rbias{li}", [128, 36], F32)
            c.dma("sp", rb[:, 0:4], W["moe_b_group"][li].partition_broadcast(128), writes=[brb])
            c.dma("sp", rb[:, 4:36], W["moe_b_expert"][li].partition_broadcast(128), writes=[brb])
            self.rbias.append((rb, brb))

    def p1_gmlp(self):
        c = self.c
        W = self.W
        win, bwin = self.T("a_win", [128, 8, 2560], BF16)
        wout, bwout = self.T("a_wout", [128, 12, 1024], BF16)
        WsT, bWsT = self.T("WsT", [128, 8, 128], BF16)
        Cg, bCg = self.T("Cg", [128, 8, 128], F32)
        lng, blng = self.load_small_cols("lng", W["a_ln_g"][0], 8)
        self.load_w_cast(wout, bwout, W["a_w_out"][0], 12)
        with c.scope():
            self.p1_setup(win, bwin, WsT, bWsT, Cg, bCg)
        self.alloc_tile_bufs(nht=1)
        uT, buT = self.T("uT", [128, 8, 512], BF16)
        vrot = Rot([self.T(f"v{i}", [128, 1024], F32) for i in range(2)])
        vnrot = Rot([self.T(f"vn{i}", [128, 1024], BF16) for i in range(2)])
        strot = Rot([self.T(f"bnst{i}", [128, 2, 6], F32) for i in range(2)])
        mvrot = Rot([self.T(f"mv{i}", [128, 2], F32) for i in range(2)])
        gtrot = Rot([self.T(f"gt{i}", [128, 8, 128], F32) for i in range(2)])
        self.zero_fill_xs([bwin, bwout])
        c.barrier()
        c.warm_segs[c.segs[-1]] = True
        self.p1_main(win, bwin, wout, bwout, WsT, bWsT, Cg, bCg, lng, blng, uT, buT, vrot, vnrot, strot, mvrot, gtrot)

    def p1_setup(self, win, bwin, WsT, bWsT, Cg, bCg):
        c = self.c
        W = self.W
        self.wstage = Rot([self.T(f"wstage{i}", [128, 2560], F32) for i in range(2)])
        n1, bn1 = self.load_small_cols("n1g0", W["norm1_g"][0], 8)
        self.load_w_gain(win, bwin, W["a_w_in"][0], 8, 2560, n1, bn1)
        wsf, bwsf = self.T("wsf", [128, 8, 128], F32)
        c.dma("sp", wsf[:], W["a_w_s"][0].rearrange("g t s -> t g s"), writes=[bwsf])
        for g in range(8):
            c.op("pool", "affine_select", reads=[bwsf], writes=[bwsf], out=wsf[:, g, :], in_=wsf[:, g, :], pattern=[[-1, 128]],
                 compare_op=ALU.is_ge, fill=0.0, base=0, channel_multiplier=1)
        wsb, bwsb = self.T("wsb", [128, 8, 128], BF16)
        c.op("dve", "tensor_copy", reads=[bwsf], writes=[bwsb], out=wsb[:], in_=wsf[:])
        pTv = self.pT[0][:].bitcast(BF16)
        bpT = self.pT[1]
        for g in range(8):
            c.op("pe", "transpose", reads=[bwsb, self.identb[1]], writes=[bpT], out=pTv[:, g * 128:(g + 1) * 128],
                 in_=wsb[:, g, :], identity=self.identb[0][:])
        c.op("act", "copy", reads=[bpT], writes=[bWsT], out=WsT[:], in_=pTv.rearrange("p (g t) -> p g t", g=8))
        betab, bbetab = self.T("betab", [128, 1024], F32)
        c.dma("sp", betab[:], W["a_ln_b"][0].partition_broadcast(128), writes=[bbetab])
        betabb, bbetabb = self.T("betabb", [128, 1024], BF16)
        c.op("dve", "tensor_copy", reads=[bbetab], writes=[bbetabb], out=betabb[:], in_=betab[:])
        bsr, bbsr = self.T("bsr", [1, 1024], F32)
        c.dma("sp", bsr[:], W["a_b_s"][0].rearrange("g t -> (g t)").unsqueeze(0), writes=[bbsr])
        bsrb, bbsrb = self.T("bsrb", [1, 1024], BF16)
        c.op("dve", "tensor_copy", reads=[bbsr], writes=[bbsrb], out=bsrb[:], in_=bsr[:])
        pw, bpw = self.pw
        for g in range(8):
            c.op("pe", "matmul", reads=[bbetabb, bWsT], writes=[bpw], out=pw[:, g * 128:(g + 1) * 128],
                 lhsT=betabb[:, g * 128:(g + 1) * 128], rhs=WsT[:, g, :], start=True, stop=False)
            c.op("pe", "matmul", reads=[self.onesb[1], bbsrb], writes=[bpw], out=pw[:, g * 128:(g + 1) * 128],
                 lhsT=self.onesb[0][0:1, :], rhs=bsrb[0:1, g * 128:(g + 1) * 128], start=False, stop=True)
        c.op("act", "copy", reads=[bpw], writes=[bCg], out=Cg[:], in_=pw[:].rearrange("p (g t) -> p g t", g=8))

    def p1_main(self, win, bwin, wout, bwout, WsT, bWsT, Cg, bCg, lng, blng, uT, buT, vrot, vnrot, strot, mvrot, gtrot):
        c = self.c
        import os
        STG = float(os.environ.get("STG", "99"))
        if STG < 1:
            return
        pw, bpw = self.pw
        catT, bcat = self.catT
        for j in range(self.NT):
            xt, bxt = self.xt.next()
            c.dma("sp", xt[:], self.x[j * 512:(j + 1) * 512, :].rearrange("(c p) d -> p c d", p=128), writes=[bxt])
            self.norm_to_hT(xt, bxt)
            hT, bhT = self.hT
            if STG < 2:
                continue
            for n in range(8):
                pu, bpu = self.mmrot.next()
                for k in range(8):
                    c.op("pe", "matmul", reads=[bwin, bhT], writes=[bpu], out=pu[:], lhsT=win[:, k, n * 128:(n + 1) * 128],
                         rhs=hT[:, k, :], start=(k == 0), stop=(k == 7))
                c.op("act", "activation", reads=[bpu], writes=[buT], out=uT[:, n, :], in_=pu[:], func=AF.Gelu)
            if STG < 3:
                continue
            for ch in range(4):
                v, bv = vrot.next()
                for half in range(2):
                    pv, bpv = self.mmrot.next()
                    for k in range(8):
                        c.op("pe", "matmul", reads=[bhT, bwin], writes=[bpv], out=pv[:], lhsT=hT[:, k, ch * 128:(ch + 1) * 128],
                             rhs=win[:, k, 1024 + half * 512:1024 + (half + 1) * 512], start=(k == 0), stop=(k == 7))
                    c.op("act", "activation", reads=[bpv], writes=[bv], out=v[:, half * 512:(half + 1) * 512], in_=pv[:], func=AF.Gelu)
                st, bst = strot.next()
                mv, bmv = mvrot.next()
                for half in range(2):
                    c.op("dve", "bn_stats", reads=[bv], writes=[bst], out=st[:, half, :], in_=v[:, half * 512:(half + 1) * 512])
                c.op("dve", "bn_aggr", reads=[bst], writes=[bmv], out=mv[:], in_=st[:].rearrange("p a b -> p (a b)"))
                self.rsqrt_(mv[:, 1:2], bmv, 1.0, 1)
                vn, bvn = vnrot.next()
                c.op("dve", "tensor_scalar", reads=[bv, bmv], writes=[bvn], out=vn[:], in0=v[:], scalar1=mv[:, 0:1], scalar2=mv[:, 1:2],
                     op0=ALU.subtract, op1=ALU.mult)
                for g in range(8):
                    c.op("pe", "matmul", reads=[bvn, bWsT], writes=[bpw], out=pw[:, g * 128:(g + 1) * 128],
                         lhsT=vn[:, g * 128:(g + 1) * 128], rhs=WsT[:, g, :], start=True, stop=True)
                gt, bgt = gtrot.next()
                for g in range(8):
                    c.op("dve", "scalar_tensor_tensor", reads=[bpw, blng, bCg], writes=[bgt], out=gt[:, g, :],
                         in0=pw[:, g * 128:(g + 1) * 128], scalar=lng[:, g:g + 1], in1=Cg[:, g, :], op0=ALU.mult, op1=ALU.add)
                c.op("pool", "tensor_tensor", reads=[bgt, buT], writes=[bcat], out=catT[:, 0:8, ch * 128:(ch + 1) * 128],
                     in0=gt[:], in1=uT[:, :, ch * 128:(ch + 1) * 128], op=ALU.mult)
            if STG < 4:
                continue
            self.qmem_and_attn(0, win, bwin, 2048, catT, bcat, 8)
            if STG < 5:
                continue
            self.out_proj_norm2_router(0, j, xt, bxt, wout, bwout)

    def moe(self, li, xin, xout):
        c = self.c
        W = self.W
        S, ST = self.S, self.ST
        nst = S // ST
        ncs = ST // 128
        nts = ST // 512
        if True:
            self.moe_bufs = dict(
                xacc=self.T("xacc", [128, ncs, 1024], F32),
                h2s=self.T("h2s", [128, 8, ST], BF16),
                cmb=self.T("cmb", [128, ncs, NE], F32),
                wg=Rot([self.T(f"wg{i}", [128, 8, 256], BF16) for i in range(2)]),
                wu=Rot([self.T(f"wu{i}", [128, 8, 256], BF16) for i in range(2)]),
                wd=Rot([self.T(f"wd{i}", [128, 2, 1024], BF16) for i in range(2)]),
                sg=Rot([self.T(f"sg{i}", [128, 512], BF16) for i in range(2)]),
                a=Rot([self.T(f"a{i}", [128, 512], BF16) for i in range(4)]),
            )
        mb = self.moe_bufs
        xacc, bxacc = mb["xacc"]
        h2s, bh2s = mb["h2s"]
        cmb, bcmb = mb["cmb"]
        pg = [self.pmm[0], self.pmm[1]]
        pu = [self.pmm[2], self.pmm[3]]
        yrot = Rot([(self.pw[0][:, 0:512], self.pw[1]), (self.pT[0][:], self.pT[1]), (self.pmisc[0][:], self.pmisc[1])])
        for st in range(nst):
            r0 = st * ST
            tiles = range(st * nts, (st + 1) * nts)
            c.dma("sp", xacc[:], xin[r0:r0 + ST, :].rearrange("(c p) d -> p c d", p=128),
                  reads=[self.dbuf(f"xmid{li}", t) for t in tiles], writes=[bxacc])
            c.dma("sp", h2s[:], self.h2T[li][:, :, r0:r0 + ST].rearrange("k p t -> p k t"),
                  reads=[self.dbuf(f"h2T{li}", t) for t in tiles], writes=[bh2s])
            c.dma("sp", cmb[:], self.comb[li][r0:r0 + ST, :].rearrange("(c p) e -> p c e", p=128),
                  reads=[self.dbuf(f"comb{li}", ch) for ch in range(st * ncs, (st + 1) * ncs)], writes=[bcmb])
            for e in range(NE):
                wg, bwg = mb["wg"].next()
                wu, bwu = mb["wu"].next()
                wd, bwd = mb["wd"].next()
                c.dma("pool", wg[:], W["moe_w_gate"][li, e].rearrange("(k p) f -> p k f", p=128), writes=[bwg])
                c.dma("pool", wu[:], W["moe_w_up"][li, e].rearrange("(k p) f -> p k f", p=128), writes=[bwu])
                c.dma("pool", wd[:], W["moe_w_down"][li, e].rearrange("(k p) n -> p k n", p=128), writes=[bwd])
                for t in range(nts):
                    acts = []
                    for fc in range(2):
                        g_, bg_ = pg[fc]
                        u_, bu_ = pu[fc]
                        for k in range(8):
                            c.op("pe", "matmul", reads=[bwg, bh2s], writes=[bg_], out=g_[:], lhsT=wg[:, k, fc * 128:(fc + 1) * 128],
                                 rhs=h2s[:, k, t * 512:(t + 1) * 512], start=(k == 0), stop=(k == 7))
                        for k in range(8):
                            c.op("pe", "matmul", reads=[bwu, bh2s], writes=[bu_], out=u_[:], lhsT=wu[:, k, fc * 128:(fc + 1) * 128],
                                 rhs=h2s[:, k, t * 512:(t + 1) * 512], start=(k == 0), stop=(k == 7))
                        sg, bsg = mb["sg"].next()
                        c.op("act", "activation", reads=[bg_], writes=[bsg], out=sg[:], in_=g_[:], func=AF.Silu)
                        a, ba = mb["a"].next()
                        c.op("dve", "tensor_tensor", reads=[bsg, bu_], writes=[ba], out=a[:], in0=sg[:], in1=u_[:], op=ALU.mult)
                        acts.append((a, ba))
                    for ch in range(4):
                        gc = t * 4 + ch
                        for half in range(2):
                            py, bpy = yrot.next()
                            for fc in range(2):
                                c.op("pe", "matmul", reads=[acts[fc][1], bwd], writes=[bpy], out=py,
                                     lhsT=acts[fc][0][:, ch * 128:(ch + 1) * 128], rhs=wd[:, fc, half * 512:(half + 1) * 512],
                                     start=(fc == 0), stop=(fc == 1))
                            c.op("dve", "scalar_tensor_tensor", reads=[bpy, bcmb, bxacc], writes=[bxacc],
                                 out=xacc[:, gc, half * 512:(half + 1) * 512], in0=py, scalar=cmb[:, gc, e:e + 1],
                                 in1=xacc[:, gc, half * 512:(half + 1) * 512], op0=ALU.mult, op1=ALU.add)
            oname = "x1" if li == 0 else "out"
            c.dma("sp", xout[r0:r0 + ST, :].rearrange("(c p) d -> p c d", p=128), xacc[:], reads=[bxacc],
                  writes=[self.dbuf(oname, t) for t in tiles])

    def zero_fill_xs(self, after):
        c = self.c
        self.zt = self.T("zt", [128, 2048], BF16)
        c.op("pool", "memset", writes=[self.zt[1]], ap=self.zt[0][:], constant=0.0)
        for li in range(1):
            if "p2" not in self.phases and "p6" not in self.phases:
                continue
            rows_per = 128 * 2
            for i in range(self.NCAP // rows_per):
                bz = Buf()
                c.dma("act", self.Xs[li][i * rows_per:(i + 1) * rows_per, :].rearrange("(p a) d -> p (a d)", p=128), self.zt[0][:],
                      reads=[self.zt[1]] + list(after), writes=[bz])
                self.zf[0].append(bz)
                self.zf[1].append(bz)

    def moe_sparse(self, li, xin, xout):
        c = self.c
        W = self.W
        S, NCH, NT_, NCAP = self.S, self.NCH, self.NTILES, self.NCAP
        IOA = bass.IndirectOffsetOnAxis
        MT = max(1, (2 * S) // 512)
        FL = NCH * 32
        R, bR = self.T("mR", [128, NCH, 66], F32)
        c.dma("sp", R[:], self.rinfo[li].rearrange("(c p) e -> p c e", p=128),
              reads=[self.dbuf(f"rinfo{li}", g) for g in range(NCH)], writes=[bR])
        posi, bposi = self.T("mposi", [128, 2, NCH], I32)
        widx, bwidx = self.T("mwidx", [128, NT_], I32)
        with c.scope():
            Mb, bMb = self.T("mMb", [128, NCH, 32], BF16)
            c.op("dve", "tensor_tensor", reads=[bR], writes=[bMb], out=Mb[:], in0=R[:, :, 0:32], in1=R[:, :, 32:64], op=ALU.add)
            Ls, bLs = self.T("mLs", [128, 128], BF16)
            c.op("pool", "memset", writes=[bLs], ap=Ls[:], constant=1.0)
            c.op("pool", "affine_select", reads=[bLs], writes=[bLs], out=Ls[:], in_=Ls[:], pattern=[[1, 128]], compare_op=ALU.is_gt,
                 fill=0.0, base=0, channel_multiplier=-1)
            rank, brank = self.T("mrank", [128, NCH, 32], F32)
            cnt, bcnt = self.T("mcnt", [128, NCH, 32], F32)
            Mbf = Mb[:].rearrange("p c e -> p (c e)")
            rankf = rank[:].rearrange("p c e -> p (c e)")
            cntf = cnt[:].rearrange("p c e -> p (c e)")
            for g0 in range(0, FL, 512):
                n = min(512, FL - g0)
                p1, bp1 = self.mmrot.next()
                c.op("pe", "matmul", reads=[bLs, bMb], writes=[bp1], out=p1[:, 0:n], lhsT=Ls[:], rhs=Mbf[:, g0:g0 + n], start=True, stop=True)
                c.op("act", "copy", reads=[bp1], writes=[brank], out=rankf[:, g0:g0 + n], in_=p1[:, 0:n])
                p2, bp2 = self.mmrot.next()
                c.op("pe", "matmul", reads=[self.onesb[1], bMb], writes=[bp2], out=p2[:, 0:n], lhsT=self.onesb[0][:], rhs=Mbf[:, g0:g0 + n],
                     start=True, stop=True)
                c.op("dve", "tensor_copy", reads=[bp2], writes=[bcnt], out=cntf[:, g0:g0 + n], in_=p2[:, 0:n])
            pre, bpre = self.T("mpre", [128, NCH, 32], F32)
            c.op("dve", "memset", writes=[bpre], ap=pre[:, 0, :], constant=0.0)
            for ch in range(1, NCH):
                c.op("dve", "tensor_tensor", reads=[bpre, bcnt], writes=[bpre], out=pre[:, ch, :], in0=pre[:, ch - 1, :], in1=cnt[:, ch - 1, :],
                     op=ALU.add)
            ne, bne = self.T("mne", [128, 32], F32)
            c.op("dve", "tensor_tensor", reads=[bpre, bcnt], writes=[bne], out=ne[:], in0=pre[:, NCH - 1, :], in1=cnt[:, NCH - 1, :], op=ALU.add)
            thr, bthr = self.T("mthr", [128, 32, MT], F32)
            c.op("pool", "iota", writes=[bthr], out=thr[:], pattern=[[0, 32], [512, MT]], base=0, channel_multiplier=0,
                 allow_small_or_imprecise_dtypes=True)
            cmp_, bcmp = self.T("mcmp", [128, 32, MT], F32)
            c.op("dve", "tensor_tensor", reads=[bne, bthr], writes=[bcmp], out=cmp_[:], in0=ne[:].unsqueeze(2).to_broadcast([128, 32, MT]),
                 in1=thr[:], op=ALU.is_gt)
            tl, btl = self.T("mtl", [128, 32], F32)
            c.op("dve", "tensor_reduce", reads=[bcmp], writes=[btl], out=tl[:], in_=cmp_[:], axis=AX.X, op=ALU.add)
            Lt, bLt = self.T("mLt", [128, 32, 32], F32)
            c.op("pool", "memset", writes=[bLt], ap=Lt[:], constant=1.0)
            c.op("pool", "affine_select", reads=[bLt], writes=[bLt], out=Lt[:], in_=Lt[:], pattern=[[1, 32], [-1, 32]], compare_op=ALU.is_gt,
                 fill=0.0, base=0, channel_multiplier=0)
            t32, bt32 = self.T("mt32", [128, 32, 32], F32)
            c.op("dve", "tensor_tensor", reads=[btl, bLt], writes=[bt32], out=t32[:], in0=tl[:].unsqueeze(1).to_broadcast([128, 32, 32]),
                 in1=Lt[:], op=ALU.mult)
            ot, bot = self.T("mot", [128, 32], F32)
            c.op("dve", "tensor_reduce", reads=[bt32], writes=[bot], out=ot[:], in_=t32[:], axis=AX.X, op=ALU.add)
            up, bup = self.T("mup", [128, 32], F32)
            c.op("dve", "tensor_tensor", reads=[bot, btl], writes=[bup], out=up[:], in0=ot[:], in1=tl[:], op=ALU.add)
            off, boff = self.T("moff", [128, 32], F32)
            c.op("dve", "tensor_scalar", reads=[bot], writes=[boff], out=off[:], in0=ot[:], scalar1=512.0, scalar2=None, op0=ALU.mult)
            c.op("dve", "tensor_tensor", reads=[brank, bpre], writes=[brank], out=rank[:], in0=rank[:], in1=pre[:], op=ALU.add)
            c.op("dve", "tensor_tensor", reads=[brank, boff], writes=[brank], out=rank[:], in0=rank[:],
                 in1=off[:].unsqueeze(1).to_broadcast([128, NCH, 32]), op=ALU.add)
            posf, bposf = self.T("mposf", [128, 2, NCH], F32)
            for sl in range(2):
                c.op("dve", "tensor_tensor", reads=[bR, brank], writes=[bpre], out=pre[:], in0=R[:, :, sl * 32:(sl + 1) * 32], in1=rank[:],
                     op=ALU.mult)
                c.op("dve", "tensor_reduce", reads=[bpre], writes=[bposf], out=posf[:, sl, :], in_=pre[:], axis=AX.X, op=ALU.add)
            c.op("dve", "tensor_copy", reads=[bposf], writes=[bposi], out=posi[:], in_=posf[:])
            ti, bti = self.T("mti", [128, NT_], F32)
            c.op("pool", "iota", writes=[bti], out=ti[:], pattern=[[1, NT_]], base=0, channel_multiplier=0, allow_small_or_imprecise_dtypes=True)
            ei, bei = self.T("mei", [128, 32], F32)
            c.op("pool", "iota", writes=[bei], out=ei[:], pattern=[[1, 32]], base=0, channel_multiplier=0, allow_small_or_imprecise_dtypes=True)
            pidx, bpidx = self.T("mpidx", [128, 1], F32)
            c.op("pool", "iota", writes=[bpidx], out=pidx[:], pattern=[[0, 1]], base=0, channel_multiplier=1, allow_small_or_imprecise_dtypes=True)
            A1, bA1 = self.T("mA1", [128, NT_, 32], F32)
            A2, bA2 = self.T("mA2", [128, NT_, 32], F32)
            tib = ti[:].unsqueeze(2).to_broadcast([128, NT_, 32])
            c.op("dve", "tensor_tensor", reads=[bti, bot], writes=[bA1], out=A1[:], in0=tib, in1=ot[:].unsqueeze(1).to_broadcast([128, NT_, 32]),
                 op=ALU.is_ge)
            c.op("dve", "tensor_tensor", reads=[bti, bup], writes=[bA2], out=A2[:], in0=tib, in1=up[:].unsqueeze(1).to_broadcast([128, NT_, 32]),
                 op=ALU.is_lt)
            c.op("dve", "tensor_tensor", reads=[bA1, bA2], writes=[bA1], out=A1[:], in0=A1[:], in1=A2[:], op=ALU.mult)
            used, bused = self.T("mused", [128, NT_], F32)
            c.op("dve", "tensor_reduce", reads=[bA1], writes=[bused], out=used[:], in_=A1[:], axis=AX.X, op=ALU.add)
            c.op("dve", "tensor_tensor", reads=[bA1, bei], writes=[bA1], out=A1[:], in0=A1[:], in1=ei[:].unsqueeze(1).to_broadcast([128, NT_, 32]),
                 op=ALU.mult)
            eid, beid = self.T("meid", [128, NT_], F32)
            c.op("dve", "tensor_reduce", reads=[bA1], writes=[beid], out=eid[:], in_=A1[:], axis=AX.X, op=ALU.add)
            c.op("dve", "tensor_scalar", reads=[beid, bpidx], writes=[beid], out=eid[:], in0=eid[:], scalar1=128.0, scalar2=pidx[:, 0:1],
                 op0=ALU.mult, op1=ALU.add)
            if li:
                c.op("dve", "tensor_scalar", reads=[beid], writes=[beid], out=eid[:], in0=eid[:], scalar1=float(li * 4096), scalar2=None,
                     op0=ALU.add)
            c.op("dve", "tensor_scalar", reads=[bused], writes=[bused], out=used[:], in0=used[:], scalar1=-1.0e6, scalar2=1.0e6, op0=ALU.mult,
                 op1=ALU.add)
            c.op("dve", "tensor_tensor", reads=[beid, bused], writes=[beid], out=eid[:], in0=eid[:], in1=used[:], op=ALU.add)
            c.op("dve", "tensor_copy", reads=[beid], writes=[bwidx], out=widx[:], in_=eid[:])
        hrot = Rot([self.T(f"mh{i}", [128, 1024], BF16) for i in range(6)])
        scb = []
        for ch in range(NCH):
            hc, bhc = hrot.next()
            c.dma("sp", hc[:], self.h2b[li][ch * 128:(ch + 1) * 128, :], reads=[self.dbuf(f"h2b{li}", ch)], writes=[bhc])
            for sl in range(2):
                bs = Buf()
                c.idma(self.Xs[li], IOA(ap=posi[:, sl, ch:ch + 1], axis=0), hc[:], None, reads=[bhc, bposi] + self.zf[li], writes=[bs])
                scb.append(bs)
        wg = Rot([self.T(f"mwg{i}", [128, 8, 256], BF16) for i in range(2)])
        wu = Rot([self.T(f"mwu{i}", [128, 8, 256], BF16) for i in range(2)])
        wd = Rot([self.T(f"mwd{i}", [128, 2, 1024], BF16) for i in range(2)])
        xsr = Rot([self.T(f"mxs{i}", [128, 4, 1024], BF16) for i in range(2)])
        XTr = Rot([self.T(f"mXT{i}", [128, 8, 512], BF16) for i in range(2)])
        sgr = Rot([self.T(f"msg{i}", [128, 512], BF16) for i in range(2)])
        ar = Rot([self.T(f"ma{i}", [128, 512], BF16) for i in range(4)])
        ytr = Rot([self.T(f"myt{i}", [128, 4, 1024], BF16) for i in range(2)])
        pg = [self.pmm[0], self.pmm[1]]
        pu = [self.pmm[2], self.pmm[3]]
        pwt = self.pw[0]
        yrot = Rot([(pwt[:, 0:512], Buf("pwa", True)), (pwt[:, 512:1024], Buf("pwb", True))])
        trot = Rot([(self.pT[0][:].bitcast(BF16), self.pT[1]), (self.pmisc[0][:].bitcast(BF16), self.pmisc[1])])
        ysb = []
        loaded = {}

        def issue(i):
            g_, u_, d_, x_ = wg.next(), wu.next(), wd.next(), xsr.next()
            ioa = IOA(ap=widx[:, i:i + 1], axis=0)
            c.idma(g_[0][:].rearrange("p k f -> p (k f)"), None, W["moe_wgL"].rearrange("l r c -> (l r) c"), ioa, reads=[bwidx], writes=[g_[1]],
                   bounds_check=8191, oob_is_err=False)
            c.idma(u_[0][:].rearrange("p k f -> p (k f)"), None, W["moe_wuL"].rearrange("l r c -> (l r) c"), ioa, reads=[bwidx], writes=[u_[1]],
                   bounds_check=8191, oob_is_err=False)
            c.idma(d_[0][:].rearrange("p k f -> p (k f)"), None, W["moe_wdL"].rearrange("l r c -> (l r) c"), ioa, reads=[bwidx], writes=[d_[1]],
                   bounds_check=8191, oob_is_err=False)
            c.dma("sp", x_[0][:], self.Xs[li][i * 512:(i + 1) * 512, :].rearrange("(c p) d -> p c d", p=128),
                  reads=scb + self.zf[li], writes=[x_[1]])
            loaded[i] = (g_, u_, d_, x_)

        issue(0)
        for i in range(NT_):
            if i + 1 < NT_:
                issue(i + 1)
            (wg_, bwg), (wu_, bwu), (wd_, bwd), (x_, bx_) = loaded.pop(i)
            XT, bXT = XTr.next()
            for ch in range(4):
                pTv, bpT = trot.next()
                for k in range(8):
                    c.op("pe", "transpose", reads=[bx_, self.identb[1]], writes=[bpT], out=pTv[:, k * 128:(k + 1) * 128],
                         in_=x_[:, ch, k * 128:(k + 1) * 128], identity=self.identb[0][:])
                c.op("act", "copy", reads=[bpT], writes=[bXT], out=XT[:, 0:4, ch * 128:(ch + 1) * 128],
                     in_=pTv[:, 0:512].rearrange("p (k t) -> p k t", k=4))
                c.op("dve", "tensor_copy", reads=[bpT], writes=[bXT], out=XT[:, 4:8, ch * 128:(ch + 1) * 128],
                     in_=pTv[:, 512:1024].rearrange("p (k t) -> p k t", k=4))
            acts = []
            for fc in range(2):
                g_, bg_ = pg[fc]
                u_, bu_ = pu[fc]
                for k in range(8):
                    c.op("pe", "matmul", reads=[bwg, bXT], writes=[bg_], out=g_[:], lhsT=wg_[:, k, fc * 128:(fc + 1) * 128], rhs=XT[:, k, :],
                         start=(k == 0), stop=(k == 7))
                for k in range(8):
                    c.op("pe", "matmul", reads=[bwu, bXT], writes=[bu_], out=u_[:], lhsT=wu_[:, k, fc * 128:(fc + 1) * 128], rhs=XT[:, k, :],
                         start=(k == 0), stop=(k == 7))
                sg, bsg = sgr.next()
                c.op("act", "activation", reads=[bg_], writes=[bsg], out=sg[:], in_=g_[:], func=AF.Silu)
                a, ba = ar.next()
                c.op("dve", "tensor_tensor", reads=[bsg, bu_], writes=[ba], out=a[:], in0=sg[:], in1=u_[:], op=ALU.mult)
                acts.append((a, ba))
            yt, byt = ytr.next()
            n_ev = 0
            for ch in range(4):
                for half in range(2):
                    py, bpy = yrot.next()
                    for fc in range(2):
                        c.op("pe", "matmul", reads=[acts[fc][1], bwd], writes=[bpy], out=py, lhsT=acts[fc][0][:, ch * 128:(ch + 1) * 128],
                             rhs=wd_[:, fc, half * 512:(half + 1) * 512], start=(fc == 0), stop=(fc == 1))
                    if n_ev % 2 == 0:
                        c.op("act", "copy", reads=[bpy], writes=[byt], out=yt[:, ch, half * 512:(half + 1) * 512], in_=py)
                    else:
                        c.op("dve", "tensor_copy", reads=[bpy], writes=[byt], out=yt[:, ch, half * 512:(half + 1) * 512], in_=py)
                    n_ev += 1
            by_ = Buf()
            c.dma("sp", self.Ys[li][i * 512:(i + 1) * 512, :].rearrange("(c p) d -> p c d", p=128), yt[:], reads=[byt], writes=[by_])
            ysb.append(by_)
        y1r = Rot([self.T(f"my1{i}", [128, 1024], BF16) for i in range(4)])
        y2r = Rot([self.T(f"my2{i}", [128, 1024], BF16) for i in range(4)])
        xmr = Rot([self.T(f"mxm{i}", [128, 1024], F32) for i in range(4)])
        dgr = Rot([self.T(f"mdg{i}", [128, 2, 128], BF16) for i in range(4)])
        oname = "x1" if li == 0 else "out"
        for ch in range(NCH):
            y1, by1 = y1r.next()
            y2, by2 = y2r.next()
            xm, bxm = xmr.next()
            dg, bdg = dgr.next()
            c.idma(y1[:], None, self.Ys[li], IOA(ap=posi[:, 0, ch:ch + 1], axis=0), reads=ysb + [bposi], writes=[by1])
            c.idma(y2[:], None, self.Ys[li], IOA(ap=posi[:, 1, ch:ch + 1], axis=0), reads=ysb + [bposi], writes=[by2])
            c.dma("sp", xm[:], xin[ch * 128:(ch + 1) * 128, :], reads=[self.dbuf(f"xmid{li}", ch // 4)], writes=[bxm])
            for sl in range(2):
                c.op("act", "activation", reads=[self.identb[1], bR], writes=[bdg], out=dg[:, sl, :], in_=self.identb[0][:], func=AF.Copy,
                     scale=R[:, ch, 64 + sl:65 + sl])
            for half in range(2):
                pc, bpc = self.mmrot.next()
                hs_ = slice(half * 512, (half + 1) * 512)
                c.op("pe", "matmul", reads=[bdg, by1], writes=[bpc], out=pc[:], lhsT=dg[:, 0, :], rhs=y1[:, hs_], start=True, stop=False)
                c.op("pe", "matmul", reads=[bdg, by2], writes=[bpc], out=pc[:], lhsT=dg[:, 1, :], rhs=y2[:, hs_], start=False, stop=True)
                c.op("dve", "tensor_tensor", reads=[bpc, bxm], writes=[bxm], out=xm[:, hs_], in0=pc[:], in1=xm[:, hs_], op=ALU.add)
            if li == 0:
                wr_b = self.dbuf("x1c", ch)
            else:
                wr_b = self.dbuf("outc", ch)
            c.dma("sp", xout[ch * 128:(ch + 1) * 128, :], xm[:], reads=[bxm], writes=[wr_b])
        if li == 0:
            for t_ in range(self.NT):
                self.db[("x1", t_)] = self.db[("x1c", t_ * 4 + 3)]

    def rope_tables(self):
        c = self.c
        NCH = self.NCH
        cosd = self.nc.dram_tensor("cosd", [128, NCH, 32], F32).ap()
        sind = self.nc.dram_tensor("sind", [128, NCH, 32], F32).ap()
        with c.scope():
            cos, bcos = self.T("cos", [128, NCH, 32], F32)
            sin, bsin = self.T("sin", [128, NCH, 32], F32)
            pi_, bpi = self.T("posi", [NCH, 128], I32)
            c.dma("sp", pi_[:], self.pos.rearrange("(c p) -> c p", p=128), writes=[bpi])
            pf, bpf = self.T("posf", [NCH, 128], F32)
            c.op("dve", "tensor_copy", reads=[bpi], writes=[bpf], out=pf[:], in_=pi_[:])
            pw, bpw = self.pw
            c.op("pe", "transpose", reads=[bpf, self.identf[1]], writes=[bpw], out=pw[:, 0:NCH], in_=pf[:],
                 identity=self.identf[0][0:NCH, 0:NCH])
            pT_, bpT_ = self.T("posT", [128, NCH], F32)
            c.op("act", "copy", reads=[bpw], writes=[bpT_], out=pT_[:], in_=pw[:, 0:NCH])
            iv, biv = self.load_bcast("invf", self.invf, 32)
            ang, bang = self.T("ang", [128, NCH, 32], F32)
            c.op("dve", "tensor_tensor", reads=[bpT_, biv], writes=[bang], out=ang[:],
                 in0=pT_[:].unsqueeze(2).to_broadcast([128, NCH, 32]), in1=iv[:].unsqueeze(1).to_broadcast([128, NCH, 32]), op=ALU.mult)
            t, bt = self.T("rr_t", [128, NCH, 32], F32)
            ti, bti = self.T("rr_ti", [128, NCH, 32], I32)
            r, br = self.T("rr_r", [128, NCH, 32], F32)
            TWO_PI = 2.0 * np.pi
            C1 = 6.28125
            C2 = TWO_PI - C1
            c.op("dve", "tensor_scalar", reads=[bang], writes=[bt], out=t[:], in0=ang[:], scalar1=float(1.0 / TWO_PI), scalar2=0.5,
                 op0=ALU.mult, op1=ALU.add)
            c.op("dve", "tensor_copy", reads=[bt], writes=[bti], out=ti[:], in_=t[:])
            c.op("dve", "tensor_copy", reads=[bti], writes=[bt], out=t[:], in_=ti[:])
            c.op("dve", "scalar_tensor_tensor", reads=[bt, bang], writes=[br], out=r[:], in0=t[:], scalar=float(-C1), in1=ang[:],
                 op0=ALU.mult, op1=ALU.add)
            c.op("dve", "scalar_tensor_tensor", reads=[bt, br], writes=[br], out=r[:], in0=t[:], scalar=float(-C2), in1=r[:],
                 op0=ALU.mult, op1=ALU.add)
            c.op("dve", "tensor_scalar", reads=[br], writes=[bt], out=t[:], in0=r[:], scalar1=float(-np.pi), scalar2=float(TWO_PI),
                 op0=ALU.is_lt, op1=ALU.mult)
            c.op("dve", "tensor_tensor", reads=[br, bt], writes=[br], out=r[:], in0=r[:], in1=t[:], op=ALU.add)
            c.op("dve", "tensor_scalar", reads=[br], writes=[bt], out=t[:], in0=r[:], scalar1=float(np.pi), scalar2=float(-TWO_PI),
                 op0=ALU.is_gt, op1=ALU.mult)
            c.op("dve", "tensor_tensor", reads=[br, bt], writes=[br], out=r[:], in0=r[:], in1=t[:], op=ALU.add)
            c.op("dve", "tensor_scalar", reads=[br], writes=[br], out=r[:], in0=r[:], scalar1=float(-3.1415925), scalar2=float(3.1415925),
                 op0=ALU.max, op1=ALU.min)
            c.op("act", "activation", reads=[br], writes=[bsin], out=sin[:], in_=r[:], func=AF.Sin)
            c.op("dve", "scalar_tensor_tensor", reads=[br], writes=[bt], out=t[:], in0=r[:], scalar=-1.0, in1=r[:], op0=ALU.mult,
                 op1=ALU.max)
            c.op("dve", "tensor_scalar", reads=[bt], writes=[bt], out=t[:], in0=t[:], scalar1=-1.0, scalar2=float(np.pi / 2),
                 op0=ALU.mult, op1=ALU.add)
            c.op("act", "activation", reads=[bt], writes=[bcos], out=cos[:], in_=t[:], func=AF.Sin)
            c.dma("sp", cosd, cos[:], reads=[bcos], writes=[self.dbuf("cosd", 0)])
            c.dma("sp", sind, sin[:], reads=[bsin], writes=[self.dbuf("sind", 0)])
        return cosd, sind

    def rope_apply(self, eng_a, eng_b, x1, x2, cosb, sinb, o1, o2, tmp, btmp, rd, wr, shape):
        c = self.c
        n = int(np.prod(shape[1:]))
        t = [tmp[:, i, 0:n].rearrange("p (a b) -> p a b", a=shape[1]) if len(shape) == 3 else tmp[:, i, 0:n] for i in range(4)]
        c.op(eng_a, "tensor_tensor", reads=rd, writes=[btmp], out=t[0], in0=x1, in1=cosb, op=ALU.mult)
        c.op(eng_a, "tensor_tensor", reads=rd, writes=[btmp], out=t[1], in0=x2, in1=sinb, op=ALU.mult)
        c.op(eng_b, "tensor_tensor", reads=rd, writes=[btmp], out=t[2], in0=x1, in1=sinb, op=ALU.mult)
        c.op(eng_b, "tensor_tensor", reads=rd, writes=[btmp], out=t[3], in0=x2, in1=cosb, op=ALU.mult)
        c.op(eng_a, "tensor_tensor", reads=[btmp], writes=wr, out=o1, in0=t[0], in1=t[1], op=ALU.subtract)
        c.op(eng_b, "tensor_tensor", reads=[btmp], writes=wr, out=o2, in0=t[2], in1=t[3], op=ALU.add)

    def p3_mla_proj(self):
        c = self.c
        W = self.W
        S = self.S
        win, bwin = self.T("b_win", [128, 8, 1344], BF16)
        wq, bwq = self.T("b_wq", [128, 4, 1536], BF16)
        wkv, bwkv = self.T("b_wkv", [128, 2, 2048], BF16)
        gqr, bgqr = self.load_bcast("gqr", W["b_qn_g"][0], 192)
        gkr, bgkr = self.load_bcast("gkr", W["b_kn_g"][0], 192)
        c.op("dve", "tensor_scalar", reads=[bgqr], writes=[bgqr], out=gqr[:], in0=gqr[:], scalar1=float(192 ** -0.5), scalar2=None,
             op0=ALU.mult)
        with c.scope():
            self.wstage = Rot([self.T(f"wstage{i}", [128, 2560], F32) for i in range(2)])
            n1, bn1 = self.load_small_cols("n1g1", W["norm1_g"][1], 8)
            self.load_w_gain(win, bwin, W["b_w_in"][0], 8, 1344, n1, bn1)
            gq_, bgq_ = self.load_small_cols("qng", W["b_q_norm_g"][0], 4)
            self.load_w_gain(wq, bwq, W["b_w_q_up"][0], 4, 1536, gq_, bgq_)
            gkv_, bgkv_ = self.load_small_cols("kvng", W["b_kv_norm_g"][0], 2)
            self.load_w_gain(wkv, bwkv, W["b_w_kv_up"][0], 2, 2048, gkv_, bgkv_)
        cosd, sind = self.rope_tables()
        c.barrier()
        c.warm_segs[c.segs[-1]] = True
        self.alloc_tile_bufs(full=False)
        csr = Rot([self.T(f"cs{i}", [128, 4, 64], F32) for i in range(2)])
        cqT, bcqT = self.T("cqT", [128, 4, 512], BF16)
        ckvT, bckvT = self.T("ckvT", [128, 2, 512], BF16)
        cqn = Rot([self.T(f"cqn{i}", [128, 512], BF16) for i in range(2)])
        ckvn = Rot([self.T(f"ckvn{i}", [128, 256], BF16) for i in range(2)])
        krr, bkrr = self.T("krr", [128, 4, 64], F32)
        zs, bzs = self.T("zs", [128, 4, 3], F32)
        qfr = Rot([self.T(f"qf{i}", [128, 8, 192], F32) for i in range(2)])
        sqfr = Rot([self.T(f"sqf{i}", [128, 8, 192], F32) for i in range(2)])
        qbr = Rot([self.T(f"qb{i}", [128, 8, 192], BF16) for i in range(2)])
        hsr = Rot([self.T(f"hs{i}", [128, 8], F32) for i in range(4)])
        kfr = Rot([self.T(f"kf{i}", [128, 8, 128], F32) for i in range(2)])
        kbr = Rot([self.T(f"kb{i}", [128, 8, 192], BF16) for i in range(2)])
        vb = Rot([self.T(f"vb{i}", [128, 8, 128], BF16) for i in range(1)])
        krgr = Rot([self.T(f"krg{i}", [128, 64], F32) for i in range(2)])
        krotr = Rot([self.T(f"krot{i}", [128, 64], F32) for i in range(2)])
        rtmpr = Rot([self.T(f"rtmp{i}", [128, 4, 256], F32) for i in range(2)])
        QTnr = Rot([self.T(f"QTn{i}", [128, 8, 128], BF16) for i in range(2)])
        QTrr = Rot([self.T(f"QTr{i}", [64, 8, 128], BF16) for i in range(2)])
        KTnr = Rot([self.T(f"KTn{i}", [128, 8, 128], BF16) for i in range(2)])
        KTrr = Rot([self.T(f"KTr{i}", [64, 8, 128], BF16) for i in range(2)])
        moT, bmoT = self.T("moT", [128, 4, 512], BF16)
        junk, bjunk = self.junk
        pTv = self.pT[0][:].bitcast(BF16)
        bpT = self.pT[1]
        pwv = self.pw[0][:].bitcast(BF16)
        bpw = self.pw[1]

        def to_T(src, bsrc, dn, bdn, dr, bdr, ch):
            for h in range(8):
                c.op("pe", "transpose", reads=[bsrc, self.identb[1]], writes=[bpT], out=pTv[:, h * 128:(h + 1) * 128],
                     in_=src[:, h, 0:128], identity=self.identb[0][:])
            c.op("act", "copy", reads=[bpT], writes=[bdn], out=dn[:], in_=pTv.rearrange("p (h t) -> p h t", h=8))
            for h in range(8):
                c.op("pe", "transpose", reads=[bsrc, self.identb[1]], writes=[bpw], out=pwv[0:64, h * 128:(h + 1) * 128],
                     in_=src[:, h, 128:192], identity=self.identb[0][:])
            c.op("dve", "tensor_copy", reads=[bpw], writes=[bdr], out=dr[:], in_=pwv[0:64, 0:1024].rearrange("p (h t) -> p h t", h=8))

        for j in range(self.NT):
            xt, bxt = self.xt.next()
            c.dma("sp", xt[:], self.x1[j * 512:(j + 1) * 512, :].rearrange("(c p) d -> p c d", p=128),
                  reads=[self.dbuf("x1", j)], writes=[bxt])
            self.norm_to_hT(xt, bxt)
            hT, bhT = self.hT
            cst, bcst = csr.next()
            c.dma("sp", cst[:, :, 0:32], cosd[:, j * 4:(j + 1) * 4, :], reads=[self.dbuf("cosd", 0)], writes=[bcst])
            c.dma("sp", cst[:, :, 32:64], sind[:, j * 4:(j + 1) * 4, :], reads=[self.dbuf("sind", 0)], writes=[bcst])
            for ch in range(4):
                p1, bp1 = self.mmrot.next()
                for k in range(8):
                    c.op("pe", "matmul", reads=[bhT, bwin], writes=[bp1], out=p1[:], lhsT=hT[:, k, ch * 128:(ch + 1) * 128],
                         rhs=win[:, k, 0:512], start=(k == 0), stop=(k == 7))
                p2, bp2 = self.mmrot.next()
                for k in range(8):
                    c.op("pe", "matmul", reads=[bhT, bwin], writes=[bp2], out=p2[:, 0:320], lhsT=hT[:, k, ch * 128:(ch + 1) * 128],
                         rhs=win[:, k, 512:832], start=(k == 0), stop=(k == 7))
                c.op("act", "activation", reads=[bp1], writes=[bjunk, bzs], out=junk[:, 0:512], in_=p1[:], func=AF.Square,
                     accum_out=zs[:, ch, 0:1])
                c.op("act", "activation", reads=[bp2], writes=[bjunk, bzs], out=junk[:, 0:256], in_=p2[:, 0:256], func=AF.Square,
                     accum_out=zs[:, ch, 1:2])
                c.op("act", "activation", reads=[bp2], writes=[bjunk, bzs], out=junk[:, 0:64], in_=p2[:, 256:320], func=AF.Square,
                     accum_out=zs[:, ch, 2:3])
                c.op("act", "copy", reads=[bp2], writes=[bkrr], out=krr[:, ch, :], in_=p2[:, 256:320])
                self.rsqrt_(zs[:, ch, 0:1], bzs, 1.0 / 512, 1)
                self.rsqrt_(zs[:, ch, 1:2], bzs, 1.0 / 256, 1)
                a, ba = cqn.next()
                c.op("dve", "tensor_scalar", reads=[bp1, bzs], writes=[ba], out=a[:], in0=p1[:], scalar1=zs[:, ch, 0:1], scalar2=None,
                     op0=ALU.mult)
                b_, bb_ = ckvn.next()
                c.op("dve", "tensor_scalar", reads=[bp2, bzs], writes=[bb_], out=b_[:], in0=p2[:, 0:256], scalar1=zs[:, ch, 1:2],
                     scalar2=None, op0=ALU.mult)
                for k in range(4):
                    c.op("pe", "transpose", reads=[ba, self.identb[1]], writes=[bpT], out=pTv[:, k * 128:(k + 1) * 128],
                         in_=a[:, k * 128:(k + 1) * 128], identity=self.identb[0][:])
                for k in range(2):
                    c.op("pe", "transpose", reads=[bb_, self.identb[1]], writes=[bpT], out=pTv[:, (4 + k) * 128:(5 + k) * 128],
                         in_=b_[:, k * 128:(k + 1) * 128], identity=self.identb[0][:])
                c.op("act", "copy", reads=[bpT], writes=[bcqT], out=cqT[:, :, ch * 128:(ch + 1) * 128],
                     in_=pTv[:, 0:512].rearrange("p (k t) -> p k t", k=4))
                c.op("act", "copy", reads=[bpT], writes=[bckvT], out=ckvT[:, :, ch * 128:(ch + 1) * 128],
                     in_=pTv[:, 512:768].rearrange("p (k t) -> p k t", k=2))
            for ch in range(4):
                gc = j * 4 + ch
                cosb = cst[:, ch, 0:32].unsqueeze(1).to_broadcast([128, 8, 32])
                sinb = cst[:, ch, 32:64].unsqueeze(1).to_broadcast([128, 8, 32])
                bcos = bsin = bcst
                qf, bqf = qfr.next()
                sqf, bsqf = sqfr.next()
                qb, bqb = qbr.next()
                hs, bhs = hsr.next()
                kf, bkf = kfr.next()
                kb, bkb = kbr.next()
                krg, bkrg = krgr.next()
                krot, bkrot = krotr.next()
                rtmp, brtmp = rtmpr.next()
                QTn, bQTn = QTnr.next()
                QTr, bQTr = QTrr.next()
                KTn, bKTn = KTnr.next()
                KTr, bKTr = KTrr.next()
                gs = slice(gc * 128, (gc + 1) * 128)
                for grp in range(4):
                    pq, bpq = self.mmrot.next()
                    for k in range(4):
                        c.op("pe", "matmul", reads=[bcqT, bwq], writes=[bpq], out=pq[:, 0:384], lhsT=cqT[:, k, ch * 128:(ch + 1) * 128],
                             rhs=wq[:, k, grp * 384:(grp + 1) * 384], start=(k == 0), stop=(k == 3))
                    c.op("act", "copy", reads=[bpq], writes=[bqf], out=qf[:, 2 * grp:2 * grp + 2, :],
                         in_=pq[:, 0:384].rearrange("p (h d) -> p h d", h=2))
                c.op("dve", "tensor_tensor", reads=[bqf], writes=[bsqf], out=sqf[:], in0=qf[:], in1=qf[:], op=ALU.mult)
                c.op("dve", "tensor_reduce", reads=[bsqf], writes=[bhs], out=hs[:], in_=sqf[:], axis=AX.X, op=ALU.add)
                self.rsqrt_(hs[:], bhs, 1.0 / 192, 8)
                c.op("dve", "tensor_tensor", reads=[bqf, bhs], writes=[bqf], out=qf[:], in0=qf[:],
                     in1=hs[:].unsqueeze(2).to_broadcast([128, 8, 192]), op=ALU.mult)
                c.op("pool", "tensor_tensor", reads=[bqf, bgqr], writes=[bqf], out=qf[:], in0=qf[:],
                     in1=gqr[:].unsqueeze(1).to_broadcast([128, 8, 192]), op=ALU.mult)
                c.op("act", "copy", reads=[bqf], writes=[bqb], out=qb[:, :, 0:128], in_=qf[:, :, 0:128])
                self.rope_apply("dve", "pool", qf[:, :, 128:160], qf[:, :, 160:192], cosb, sinb, qb[:, :, 128:160], qb[:, :, 160:192],
                                rtmp, brtmp, [bqf, bcos, bsin], [bqb], [128, 8, 32])
                to_T(qb, bqb, QTn, bQTn, QTr, bQTr, ch)
                c.dma("sp", self.QT[:, 0:128, gs].rearrange("h p t -> p h t"), QTn[:], reads=[bQTn], writes=[self.dbuf("QTn", gc)])
                c.dma("sp", self.QT[:, 128:192, gs].rearrange("h p t -> p h t"), QTr[:], reads=[bQTr], writes=[self.dbuf("QTr", gc)])
                sqf, bsqf = sqfr.next()
                hs, bhs = hsr.next()
                rtmp, brtmp = rtmpr.next()
                v_, bv_ = vb.next()
                for grp in range(4):
                    pk, bpk = self.mmrot.next()
                    for k in range(2):
                        c.op("pe", "matmul", reads=[bckvT, bwkv], writes=[bpk], out=pk[:], lhsT=ckvT[:, k, ch * 128:(ch + 1) * 128],
                             rhs=wkv[:, k, grp * 512:(grp + 1) * 512], start=(k == 0), stop=(k == 1))
                    pkv = pk[:].rearrange("p (h d) -> p h d", h=2)
                    c.op("act", "copy", reads=[bpk], writes=[bkf], out=kf[:, 2 * grp:2 * grp + 2, :], in_=pkv[:, :, 0:128])
                    c.op("dve", "tensor_copy", reads=[bpk], writes=[bv_], out=v_[:, 2 * grp:2 * grp + 2, :], in_=pkv[:, :, 128:256])
                c.dma("sp", self.Vd[gc * 128:(gc + 1) * 128, :], v_[:].rearrange("p h d -> p (h d)"), reads=[bv_],
                      writes=[self.dbuf("Vd", gc)])
                c.op("dve", "tensor_tensor", reads=[bkf], writes=[bsqf], out=sqf[:, :, 0:128], in0=kf[:], in1=kf[:], op=ALU.mult)
                c.op("dve", "tensor_reduce", reads=[bsqf], writes=[bhs], out=hs[:], in_=sqf[:, :, 0:128], axis=AX.X, op=ALU.add)
                c.op("dve", "tensor_scalar", reads=[bhs, bzs], writes=[bhs], out=hs[:], in0=hs[:], scalar1=zs[:, ch, 2:3], scalar2=None,
                     op0=ALU.add)
                self.rsqrt_(hs[:], bhs, 1.0 / 192, 8)
                c.op("dve", "tensor_tensor", reads=[bkf, bhs], writes=[bkf], out=kf[:], in0=kf[:],
                     in1=hs[:].unsqueeze(2).to_broadcast([128, 8, 128]), op=ALU.mult)
                c.op("pool", "tensor_tensor", reads=[bkf, bgkr], writes=[bkb], out=kb[:, :, 0:128], in0=kf[:],
                     in1=gkr[:, 0:128].unsqueeze(1).to_broadcast([128, 8, 128]), op=ALU.mult)
                c.op("pool", "tensor_tensor", reads=[bkrr, bgkr], writes=[bkrg], out=krg[:], in0=krr[:, ch, :], in1=gkr[:, 128:192],
                     op=ALU.mult)
                self.rope_apply("dve", "pool", krg[:, 0:32], krg[:, 32:64], cst[:, ch, 0:32], cst[:, ch, 32:64], krot[:, 0:32], krot[:, 32:64],
                                rtmp, brtmp, [bkrg, bcos, bsin], [bkrot], [128, 32])
                c.op("dve", "tensor_tensor", reads=[bkrot, bhs], writes=[bkb], out=kb[:, :, 128:192],
                     in0=krot[:].unsqueeze(1).to_broadcast([128, 8, 64]), in1=hs[:].unsqueeze(2).to_broadcast([128, 8, 64]), op=ALU.mult)
                to_T(kb, bkb, KTn, bKTn, KTr, bKTr, ch)
                c.dma("sp", self.KT[:, 0:128, gs].rearrange("h p t -> p h t"), KTn[:], reads=[bKTn], writes=[self.dbuf("KTn", gc)])
                c.dma("sp", self.KT[:, 128:192, gs].rearrange("h p t -> p h t"), KTr[:], reads=[bKTr], writes=[self.dbuf("KTr", gc)])
            cs = slice(j * 512, (j + 1) * 512)
            self.qmem_and_attn(1, win, bwin, 832, moT, bmoT, 0)
            c.dma("sp", self.MO[:, :, cs].rearrange("h p t -> p h t"), moT[:], reads=[bmoT], writes=[self.dbuf("MO", j)])

    def p4_attn(self):
        c = self.c
        S, NT, NCH = self.S, self.NT, self.NCH
        c.barrier()
        c.cp_segs[c.segs[-1]] = True
        kn = Rot([self.T(f"A_kn{i}", [128, S], BF16) for i in range(2)])
        kr = Rot([self.T(f"A_kr{i}", [128, S], BF16) for i in range(2)])
        va = Rot([self.T(f"A_v{i}", [128, NCH, 130], BF16) for i in range(2)])
        qn = Rot([self.T(f"A_qn{i}", [128, 512], BF16) for i in range(2)])
        qr = Rot([self.T(f"A_qr{i}", [128, 512], BF16) for i in range(2)])
        PT = Rot([self.T(f"A_P{i}", [128, 512], BF16) for i in range(8)])
        osb = Rot([self.T(f"A_osb{i}", [128, 4, 128], BF16) for i in range(2)])
        rden = Rot([self.T(f"A_rd{i}", [128, 4], F32) for i in range(2)])
        ot = Rot([self.T(f"A_o{i}", [128, 512], BF16) for i in range(2)])
        tri, btri = self.T("A_tri", [128, 128], BF16)
        c.op("pool", "memset", writes=[btri], ap=tri[:], constant=1.0)
        c.op("pool", "affine_select", reads=[btri], writes=[btri], out=tri[:], in_=tri[:], pattern=[[1, 128]], compare_op=ALU.is_ge,
             fill=0.0, base=0, channel_multiplier=-1)
        for t, b in kr.items + qr.items:
            c.op("pool", "memset", writes=[b], ap=t[:], constant=0.0)
        for t, b in va.items:
            c.op("pool", "memset", writes=[b], ap=t[:, :, 128:130], constant=1.0)
        srot = Rot(self.pmm)
        pwt = self.pw[0]
        osets = [[(pwt[:, 0:512], Buf("oA0", True)), (pwt[:, 512:1024], Buf("oA1", True))],
                 [(self.pT[0][:], Buf("oB0", True)), (self.pmisc[0][:], Buf("oB1", True))]]
        it = 0
        for h in range(8):
            Kn, bKn = kn.next()
            Kr, bKr = kr.next()
            V, bV = va.next()
            c.dma("sp", Kn[:], self.KT[h, 0:128, :], reads=[self.dbuf("KTn", g) for g in range(NCH)], writes=[bKn])
            c.dma("sp", Kr[0:64, :], self.KT[h, 128:192, :], reads=[self.dbuf("KTr", g) for g in range(NCH)], writes=[bKr])
            c.dma("sp", V[:, :, 0:128], self.Vd[:, h * 128:(h + 1) * 128].rearrange("(c p) d -> p c d", p=128),
                  reads=[self.dbuf("Vd", g) for g in range(NCH)], writes=[bV])
            for qt in range(NT):
                Qn, bQn = qn.next()
                Qr, bQr = qr.next()
                cs = slice(qt * 512, (qt + 1) * 512)
                c.dma("sp", Qn[:], self.QT[h, 0:128, cs], reads=[self.dbuf("QTn", g) for g in range(4 * qt, 4 * qt + 4)], writes=[bQn])
                c.dma("sp", Qr[0:64, :], self.QT[h, 128:192, cs], reads=[self.dbuf("QTr", g) for g in range(4 * qt, 4 * qt + 4)], writes=[bQr])
                nk = 4 * (qt + 1)
                oset = osets[it % 2]
                it += 1
                for kc in range(nk):
                    di = max(kc - 4 * qt, 0)
                    q0 = di * 128
                    ps_, bps = srot.next()
                    c.op("pe", "matmul", reads=[bKn, bQn], writes=[bps], out=ps_[:, q0:512], lhsT=Kn[:, kc * 128:(kc + 1) * 128],
                         rhs=Qn[:, q0:512], start=True, stop=False)
                    c.op("pe", "matmul", reads=[bKr, bQr], writes=[bps], out=ps_[:, q0:512], lhsT=Kr[:, kc * 128:(kc + 1) * 128],
                         rhs=Qr[:, q0:512], start=False, stop=True)
                    P, bP = PT.next()
                    c.op("act", "activation", reads=[bps], writes=[bP], out=P[:, q0:512], in_=ps_[:, q0:512], func=AF.Exp)
                    if kc >= 4 * qt:
                        c.op("pool", "tensor_tensor", reads=[bP, btri], writes=[bP], out=P[:, q0:q0 + 128], in0=P[:, q0:q0 + 128],
                             in1=tri[:], op=ALU.mult)
                    for qc in range(di, 4):
                        ob, bob = oset[qc // 2]
                        col = (qc % 2) * 130
                        c.op("pe", "matmul", reads=[bP, bV], writes=[bob], out=ob[:, col:col + 130], lhsT=P[:, qc * 128:(qc + 1) * 128],
                             rhs=V[:, kc, :], start=(kc == 0 and qc % 2 == 0), stop=(kc == 4 * qt + qc), skip_group_check=True)
                rd, brd = rden.next()
                ob_, bob_ = osb.next()
                pt_, bpt = srot.next()
                ptv = pt_[:].bitcast(BF16)
                for qc in range(4):
                    ob, bob = oset[qc // 2]
                    col = (qc % 2) * 130
                    c.op("dve", "reciprocal", reads=[bob], writes=[brd], out=rd[:, qc:qc + 1], in_=ob[:, col + 128:col + 129])
                    c.op("dve", "tensor_scalar", reads=[bob, brd], writes=[bob_], out=ob_[:, qc, :], in0=ob[:, col:col + 128],
                         scalar1=rd[:, qc:qc + 1], scalar2=None, op0=ALU.mult)
                    c.op("pe", "transpose", reads=[bob_, self.identb[1]], writes=[bpt], out=ptv[:, qc * 128:(qc + 1) * 128],
                         in_=ob_[:, qc, :], identity=self.identb[0][:])
                o, bo = ot.next()
                c.op("act", "copy", reads=[bpt], writes=[bo], out=o[:], in_=ptv[:, 0:512])
                c.dma("sp", self.OT[h, :, cs], o[:], reads=[bo], writes=[self.dbuf("OT", (h, qt))])

    def p5_out(self):
        c = self.c
        W = self.W
        wout, bwout = self.T("b_wout", [128, 12, 1024], BF16)
        self.load_w_cast(wout, bwout, W["b_w_out"][0], 12)
        c.barrier()
        c.warm_segs[c.segs[-1]] = True
        self.alloc_tile_bufs(deep=True)
        for j in range(self.NT):
            self.catT = self.catTr.next()
            catT, bcat = self.catT
            xt, bxt = self.xt.next()
            cs = slice(j * 512, (j + 1) * 512)
            c.dma("sp", xt[:], self.x1[j * 512:(j + 1) * 512, :].rearrange("(c p) d -> p c d", p=128),
                  reads=[self.dbuf("x1", j)], writes=[bxt])
            c.dma("sp", catT[:, 0:8, :], self.OT[:, :, cs].rearrange("h p t -> p h t"),
                  reads=[self.dbuf("OT", (h, j)) for h in range(8)], writes=[bcat])
            c.dma("sp", catT[:, 8:12, :], self.MO[:, :, cs].rearrange("h p t -> p h t"), reads=[self.dbuf("MO", j)], writes=[bcat])
            self.out_proj_norm2_router(1, j, xt, bxt, wout, bwout)


INV_FREQ = (10000.0 ** (-(np.arange(32, dtype=np.float32) * 2.0 / 64))).astype(np.float32)


def relayout_experts(inputs):
    g = np.asarray(inputs["moe_w_gate"]).reshape(2, 32, 8, 128, 256).transpose(0, 1, 3, 2, 4).reshape(2, 4096, 2048)
    u = np.asarray(inputs["moe_w_up"]).reshape(2, 32, 8, 128, 256).transpose(0, 1, 3, 2, 4).reshape(2, 4096, 2048)
    d = np.asarray(inputs["moe_w_down"]).reshape(2, 32, 2, 128, 1024).transpose(0, 1, 3, 2, 4).reshape(2, 4096, 2048)
    return {"moe_wgL": np.ascontiguousarray(g), "moe_wuL": np.ascontiguousarray(u), "moe_wdL": np.ascontiguousarray(d)}


def make_in_maps(inputs, S, ncores):
    maps = []
    inputs = dict(inputs)
    inputs.update(relayout_experts(inputs))
    for b in range(ncores):
        m = {n: np.ascontiguousarray(inputs[n]) for n, _ in WEIGHT_NAMES}
        m["x"] = np.ascontiguousarray(inputs["x"][b])
        m["mem"] = np.ascontiguousarray(inputs["mem"][b])
        m["positions"] = np.ascontiguousarray(inputs["positions"][b]).astype(np.int32)
        m["inv_freq"] = INV_FREQ
        maps.append(m)
    return maps


def kernel(**inputs):
    B, S, _ = inputs["x"].shape
    prog = Prog(S)
    maps = make_in_maps(inputs, S, B)
    res = run_bass_kernel_spmd(prog.nc, maps, core_ids=list(range(B)))
    return np.stack([r["out"] for r in res.results], axis=0).astype(np.float32)
```

```python
import contextlib
import numpy as np
import concourse.bass as bass
import concourse.mybir as mybir
from concourse.bass_utils import run_bass_kernel_spmd

F32 = mybir.dt.float32
BF16 = mybir.dt.bfloat16
I32 = mybir.dt.int32
AF = mybir.ActivationFunctionType
ALU = mybir.AluOpType
AX = mybir.AxisListType

D = 1024
EPS = 1e-6
NE = 32


class Buf:
    __slots__ = ("name", "w", "rl", "psum")

    def __init__(self, name="", psum=False):
        self.name = name
        self.w = None
        self.rl = []
        self.psum = psum


class Node:
    __slots__ = ("eng", "fn", "kw", "sync", "order", "dur", "occ", "isdma", "act_set")


def _fsize(ap):
    try:
        return int(ap.free_size())
    except Exception:
        return 512


class Ctx:
    NDSEM = 24
    import os as _os
    WINDOW = int(_os.environ.get('SCHEDW', '96'))

    def __init__(self, nc):
        self.nc = nc
        self.es = contextlib.ExitStack()
        self.engs = ("pe", "dve", "act", "pool", "sp")
        self.sems = {}
        for k in self.engs:
            self.sems[k] = self.es.enter_context(nc.semaphore("s_" + k))
        self.dsem = {}
        for q in ("sp", "pool", "act"):
            self.dsem[q] = [self.es.enter_context(nc.semaphore(f"d_{q}{i}")) for i in range(self.NDSEM)]
        self.nodes = []
        self.cp_segs = {}
        self.warm_segs = {}
        self.filler_kw = None
        self.segs = [0]
        self.scopes = [self.es]
        self.nalloc = 0
        self.ninst = 0
        self.nwaits = 0

    def sb(self, name, shape, dt):
        self.nalloc += 1
        return self.scopes[-1].enter_context(self.nc.sbuf_tensor(f"{name}_{self.nalloc}", list(shape), dt))

    def ps(self, name, shape, dt=F32):
        return self.es.enter_context(self.nc.psum_tensor(name, list(shape), dt))

    @contextlib.contextmanager
    def scope(self):
        st = contextlib.ExitStack()
        self.scopes.append(st)
        try:
            yield
        finally:
            self.barrier()
            self.scopes.pop()
            st.close()

    def barrier(self):
        if self.segs[-1] != len(self.nodes):
            self.segs.append(len(self.nodes))

    def close(self):
        self.es.close()

    def _record(self, eng, fn, kw, reads, writes, isdma, dur, occ, act_set=None):
        nid = len(self.nodes)
        seg0 = self.segs[-1]
        n = Node()
        n.eng, n.fn, n.kw, n.isdma, n.dur, n.occ, n.act_set = eng, fn, kw, isdma, dur, occ, act_set
        sync, order = set(), set()
        nodes = self.nodes

        def dep(m):
            if m is None or m < seg0:
                return
            mn = nodes[m]
            if eng == "pe" and not isdma and not mn.isdma and mn.eng == "pe":
                order.add(m)
            else:
                sync.add(m)
        for b in reads:
            dep(b.w)
            if b.psum:
                for r in b.rl:
                    if nodes[r].eng != eng:
                        dep(r)
        for b in writes:
            dep(b.w)
            for r in b.rl:
                dep(r)
        n.sync, n.order = sync, order
        nodes.append(n)
        for b in reads:
            b.rl.append(nid)
        for b in writes:
            b.w = nid
            b.rl = []
        self.ninst += 1
        return nid

    def op(self, e, name, reads=(), writes=(), **kw):
        act_set = None
        if e == "pe":
            if name == "matmul":
                nn = _fsize(kw["rhs"])
                f = 4.0 if kw["rhs"].dtype == F32 else 1.0
            else:
                nn = 128
                f = 1.0
            dur = f * max(64, nn) / 2.4 + 8
        elif e == "act":
            dur = (_fsize(kw["out"]) + 200) / 1.2
            fnc = kw.get("func")
            if fnc in (AF.Exp, AF.Gelu, AF.Silu, AF.Sin):
                act_set = fnc
        elif e == "dve":
            nn = _fsize(kw["out"] if "out" in kw else kw["ap"])
            f = 8.0 if name == "reciprocal" else (2.0 if name in ("tensor_tensor", "scalar_tensor_tensor") else 1.0)
            dur = (f * nn + 110) / 0.96
        else:
            nn = _fsize(kw["out"] if "out" in kw else kw["ap"])
            dur = (2.0 * nn + 200) / 0.96 + 400
        self._record(e, name, kw, reads, writes, False, dur, dur, act_set)

    def dma(self, q, out, in_, reads=(), writes=(), **kw):
        kw = dict(kw)
        kw["out"] = out
        kw["in_"] = in_
        try:
            nb = int(out.nbytes())
        except Exception:
            nb = 1 << 16
        occ = 1000.0 if q == "pool" else 70.0
        self._record(q, "dma_start", kw, reads, writes, True, 2200.0 + nb / 120.0, occ)

    def idma(self, out, out_off, in_, in_off, reads=(), writes=(), **extra):
        kw = dict(out=out, out_offset=out_off, in_=in_, in_offset=in_off)
        kw.update(extra)
        self._record("pool", "indirect_dma_start", kw, reads, writes, True, 4000.0, 1500.0)

    def wait_all(self, e, bufs):
        pass

    def _schedule_segment(self, a, b, order_out):
        nodes = self.nodes
        W = self.WINDOW
        pend = {e: [] for e in self.engs}
        succ_eng = {}
        for i in range(a, b):
            pend[nodes[i].eng].append(i)
        for i in range(a, b):
            for d in nodes[i].sync | nodes[i].order:
                succ_eng.setdefault(d, set()).add(nodes[i].eng)
        cp = {}
        for i in range(b - 1, a - 1, -1):
            cp[i] = cp.get(i, 0.0) + nodes[i].dur
            for d in nodes[i].sync | nodes[i].order:
                if d >= a and cp[i] > cp.get(d, 0.0):
                    cp[d] = cp[i]
        head = {e: 0 for e in self.engs}
        done = set()
        finish = {}
        etime = {e: 0.0 for e in self.engs}
        last_act = [None]
        cache = {e: None for e in self.engs}
        dirty = set(self.engs)
        remaining = b - a
        while remaining:
            for e in dirty:
                lst = pend[e]
                h = head[e]
                while h < len(lst) and lst[h] in done:
                    h += 1
                head[e] = h
                best = None
                cnt = 0
                k = h
                while k < len(lst) and cnt < W:
                    i = lst[k]
                    k += 1
                    if i in done:
                        continue
                    cnt += 1
                    nd = nodes[i]
                    ok = True
                    st = etime[e]
                    for d in nd.sync:
                        if d not in done:
                            ok = False
                            break
                        t = finish[d] + 80.0
                        if t > st:
                            st = t
                    if not ok:
                        continue
                    for d in nd.order:
                        if d not in done:
                            ok = False
                            break
                    if not ok:
                        continue
                    if e == "act" and nd.act_set is not None and nd.act_set != last_act[0]:
                        st += 1300.0
                    key = (st, -cp[i] if self.cp_segs.get(a, False) else 0.0)
                    if best is None or key < best[2]:
                        best = (st, i, key)
                cache[e] = best
            dirty = set()
            pick = None
            for e in self.engs:
                c_ = cache[e]
                if c_ is not None and (pick is None or c_[0] < pick[0]):
                    pick = (c_[0], c_[1], e)
            st, i, e = pick
            nd = nodes[i]
            if e == "pe" and self.filler_kw is not None and self.warm_segs.get(a, False):
                gap = st - etime["pe"]
                if 350.0 < gap:
                    nfill = min(int(gap * 0.9 / 230.0), 48)
                    for _ in range(nfill):
                        f = Node()
                        f.eng, f.fn, f.kw, f.isdma, f.dur, f.occ, f.act_set = "pe", "matmul", self.filler_kw, False, 230.0, 230.0, None
                        f.sync, f.order = set(), set()
                        nodes.append(f)
                        order_out["pe"].append(len(nodes) - 1)
                        self.nfill = getattr(self, "nfill", 0) + 1
            done.add(i)
            order_out[e].append(i)
            finish[i] = st + nd.dur
            etime[e] = st + nd.occ
            if e == "act" and nd.act_set is not None:
                last_act[0] = nd.act_set
            dirty.add(e)
            for se in succ_eng.get(i, ()):
                dirty.add(se)
            remaining -= 1

    def emit(self):
        nodes = self.nodes
        segs = self.segs + ([len(nodes)] if self.segs[-1] != len(nodes) else [])
        prog = {e: [] for e in self.engs}
        cnt = {e: 0 for e in self.engs}
        seen = {e: {} for e in self.engs}
        duse = {q: [0] * self.NDSEM for q in self.dsem}
        dn = {q: 0 for q in self.dsem}
        tok = {}

        def wait(e, key, val):
            if seen[e].get(key, 0) >= val:
                return
            semobj = self.dsem[key[0]][key[1]] if isinstance(key, tuple) else self.sems[key]
            prog[e].append((None, semobj, val))
            seen[e][key] = val
            self.nwaits += 1

        def full_barrier():
            for e in self.engs:
                for f in self.engs:
                    if f != e and cnt[f]:
                        wait(e, f, cnt[f])
                for q in self.dsem:
                    for slot in range(self.NDSEM):
                        if duse[q][slot]:
                            wait(e, (q, slot), 16 * duse[q][slot])

        for si in range(len(segs) - 1):
            a, b = segs[si], segs[si + 1]
            order = {e: [] for e in self.engs}
            self._schedule_segment(a, b, order)
            for e in self.engs:
                c0 = cnt[e]
                d0 = dn.get(e, 0)
                du = list(duse[e]) if e in duse else None
                for i in order[e]:
                    nd = nodes[i]
                    if nd.isdma:
                        slot = d0 % self.NDSEM
                        d0 += 1
                        du[slot] += 1
                        tok[i] = ((e, slot), 16 * du[slot])
                    else:
                        c0 += 1
                        tok[i] = (e, c0)
            for e in self.engs:
                for i in order[e]:
                    nd = nodes[i]
                    for d in nd.sync:
                        k_, v_ = tok[d]
                        wait(e, k_, v_)
                    if nd.isdma:
                        slot = dn[e] % self.NDSEM
                        dn[e] += 1
                        prev = duse[e][slot]
                        if prev:
                            wait(e, (e, slot), 16 * prev)
                        duse[e][slot] = prev + 1
                        prog[e].append(((nd.fn, nd.kw), self.dsem[e][slot], 16))
                    else:
                        cnt[e] += 1
                        prog[e].append(((nd.fn, nd.kw), self.sems[e], 1))
            full_barrier()

        def run(eng, items):
            regs = {}
            for fn, sem, v in items:
                if fn is None:
                    eng.wait_ge(sem, v)
                else:
                    kw = fn[1]
                    bc = kw.get("bounds_check")
                    if isinstance(bc, int):
                        if bc not in regs:
                            regs[bc] = eng.to_reg(bc)
                        kw = dict(kw)
                        kw["bounds_check"] = regs[bc]
                    getattr(eng, fn[0])(**kw).then_inc(sem, v)

        with self.nc.Block() as block:
            @block.tensor
            def _(e):
                run(e, prog["pe"])

            @block.vector
            def _(e):
                run(e, prog["dve"])

            @block.scalar
            def _(e):
                run(e, prog["act"])

            @block.gpsimd
            def _(e):
                run(e, prog["pool"])

            @block.sync
            def _(e):
                run(e, prog["sp"])


class Rot:
    def __init__(self, items):
        self.items = items
        self.i = 0

    def next(self):
        r = self.items[self.i % len(self.items)]
        self.i += 1
        return r


WEIGHT_NAMES = [
    ("mem_norm_g", [1024]), ("w_mem_kv", [1024, 1024]), ("mem_qn_g", [2, 128]), ("mem_kn_g", [2, 128]),
    ("norm1_g", [2, 1024]), ("norm2_g", [2, 1024]),
    ("a_w_in", [1, 1024, 2560]), ("a_ln_g", [1, 1024]), ("a_ln_b", [1, 1024]), ("a_w_s", [1, 8, 128, 128]),
    ("a_b_s", [1, 8, 128]), ("a_w_out", [1, 1536, 1024]),
    ("b_w_in", [1, 1024, 1344]), ("b_q_norm_g", [1, 512]), ("b_kv_norm_g", [1, 256]),
    ("b_w_q_up", [1, 512, 1536]), ("b_w_kv_up", [1, 256, 2048]), ("b_qn_g", [1, 192]), ("b_kn_g", [1, 192]),
    ("b_w_out", [1, 1536, 1024]),
    ("moe_w_group", [2, 1024, 4]), ("moe_b_group", [2, 4]), ("moe_w_expert", [2, 1024, 32]),
    ("moe_b_expert", [2, 32]),
    ("moe_wgL", [2, 4096, 2048]), ("moe_wuL", [2, 4096, 2048]), ("moe_wdL", [2, 4096, 2048]),
]


class Prog:
    def __init__(self, S, phases=("p0", "p1", "p2", "p3", "p4", "p5", "p6"), dbg=()):
        self.S = S
        self.NT = S // 512
        self.NCH = S // 128
        self.ST = min(2048, S)
        self.phases = phases
        nc = self.nc = bass.Bass("TRN2", target_bir_lowering=False)
        c = self.c = Ctx(nc)
        self.W = {}
        self.x = nc.dram_tensor("x", [S, D], F32, kind="ExternalInput").ap()
        self.mem = nc.dram_tensor("mem", [256, D], F32, kind="ExternalInput").ap()
        self.pos = nc.dram_tensor("positions", [S], I32, kind="ExternalInput").ap()
        self.invf = nc.dram_tensor("inv_freq", [32], F32, kind="ExternalInput").ap()
        for n, shp in WEIGHT_NAMES:
            self.W[n] = nc.dram_tensor(n, shp, F32, kind="ExternalInput").ap()
        self.out = nc.dram_tensor("out", [S, D], F32, kind="ExternalOutput").ap()
        self.db = {}
        self.dbg = dbg

        def scratch(name, shape, dt):
            kind = "ExternalOutput" if name in dbg else "Internal"
            return nc.dram_tensor(name, list(shape), dt, kind=kind).ap()

        self.xmid = [scratch("xmid0", [S, D], F32), scratch("xmid1", [S, D], F32)]
        self.x1 = scratch("x1", [S, D], F32)
        self.h2T = [scratch("h2T0", [8, 128, S], BF16), scratch("h2T1", [8, 128, S], BF16)]
        self.comb = [scratch("comb0", [S, NE], F32), scratch("comb1", [S, NE], F32)]
        self.NTILES = (2 * S) // 512 + 32
        self.NCAP = self.NTILES * 512
        self.rinfo = [scratch(f"rinfo{i}", [S, 66], F32) for i in range(2)]
        self.h2b = [scratch(f"h2b{i}", [S, D], BF16) for i in range(2)]
        xs_shared = scratch("Xs0", [self.NCAP, D], BF16)
        self.Xs = [xs_shared, xs_shared]
        self.Ys = [scratch(f"Ys{i}", [self.NCAP, D], BF16) for i in range(2)]
        self.QT = scratch("QT", [8, 192, S], BF16)
        self.KT = scratch("KT", [8, 192, S], BF16)
        self.Vd = scratch("Vd", [S, 1024], BF16)
        self.OT = scratch("OT", [8, 128, S], BF16)
        self.MO = scratch("MO", [4, 128, S], BF16)

        self.pmm = [(c.ps(f"pmm{i}", [128, 512], F32), Buf(f"pmm{i}", True)) for i in range(4)]
        self.pw = (c.ps("pw", [128, 1024], F32), Buf("pw", True))
        self.pT = (c.ps("pT", [128, 512], F32), Buf("pT", True))
        self.pmisc = (c.ps("pmisc", [128, 512], F32), Buf("pmisc", True))
        self.mmrot = Rot(self.pmm)

        self.setup_consts()
        self.zf = [[], []]
        for ph, fn in (("p0", self.p0_memkv), ("p1", self.p1_gmlp), ("p2", lambda: self.moe_sparse(0, self.xmid[0], self.x1)),
                       ("p3", self.p3_mla_proj), ("p4", self.p4_attn), ("p5", self.p5_out),
                       ("p6", lambda: self.moe_sparse(1, self.xmid[1], self.out))):
            if ph in phases:
                with c.scope():
                    fn()
        c.wait_all("sp", list(self.db.values()))
        c.emit()
        c.close()

    def dbuf(self, name, idx):
        k = (name, idx)
        if k not in self.db:
            self.db[k] = Buf(f"{name}{idx}")
        return self.db[k]

    def T(self, name, shape, dt):
        return (self.c.sb(name, shape, dt), Buf(name))

    def rsqrt_(self, t_ap, bt, scale, n):
        c = self.c
        c.op("pool", "tensor_scalar", reads=[bt], writes=[bt], out=t_ap, in0=t_ap, scalar1=scale, scalar2=EPS,
             op0=ALU.mult, op1=ALU.add)
        c.op("pool", "tensor_tensor", reads=[bt, self.mh[1]], writes=[bt], out=t_ap, in0=t_ap,
             in1=self.mh[0][:, 0:n], op=ALU.pow)

    def load_small_cols(self, name, src1d, k):
        t, b = self.T(name, [128, k], F32)
        self.c.dma("sp", t[:], src1d.rearrange("(k p) -> p k", p=128), writes=[b], allow_slow_non_contiguous=True)
        return t, b

    def load_bcast(self, name, src1d, n):
        t, b = self.T(name, [128, n], F32)
        self.c.dma("sp", t[:], src1d.partition_broadcast(128), writes=[b])
        return t, b

    def load_w_cast(self, dst, bdst, src2d, K):
        for k in range(K):
            self.c.dma("pool", dst[:, k, :], src2d[k * 128:(k + 1) * 128, :], writes=[bdst])

    def load_w_gain(self, dst, bdst, src2d, K, N, gain, bgain):
        c = self.c
        for k in range(K):
            st, bst = self.wstage.next()
            c.dma("sp", st[:, 0:N], src2d[k * 128:(k + 1) * 128, :], writes=[bst])
            c.op("dve", "tensor_scalar", reads=[bst, bgain], writes=[bdst], out=dst[:, k, :], in0=st[:, 0:N],
                 scalar1=gain[:, k:k + 1], scalar2=None, op0=ALU.mult)

    def setup_consts(self):
        c = self.c
        self.identb = self.T("identb", [128, 128], BF16)
        self.identf = self.T("identf", [128, 128], F32)
        self.onesb = self.T("onesb", [128, 128], BF16)
        self.mh = self.T("mh", [128, 16], F32)
        for t, b in (self.identb, self.identf):
            c.op("pool", "memset", writes=[b], ap=t[:], constant=0.0)
            c.op("pool", "affine_select", reads=[b], writes=[b], out=t[:], in_=t[:], pattern=[[-1, 128]],
                 compare_op=ALU.not_equal, fill=1.0, base=0, channel_multiplier=1)
        c.op("pool", "memset", writes=[self.onesb[1]], ap=self.onesb[0][:], constant=1.0)
        c.op("pool", "memset", writes=[self.mh[1]], ap=self.mh[0][:], constant=-0.5)
        self.kT = [self.T(f"kT{i}", [128, 4, 256], BF16) for i in range(2)]
        self.Vaug = self.T("Vaug", [128, 2, 4, 130], BF16)
        self.load_router_weights()
        self.fillt = self.T("fillt", [128, 512], BF16)
        c.op("pool", "memset", writes=[self.fillt[1]], ap=self.fillt[0][:], constant=0.0)
        c.barrier()
        c.filler_kw = dict(out=self.pmisc[0][:], lhsT=self.identb[0][:], rhs=self.fillt[0][:], start=True, stop=True)

    def p0_memkv(self):
        c = self.c
        W = self.W
        g, bg = self.load_small_cols("memg", W["mem_norm_g"], 8)
        self.wstage = Rot([self.T(f"wstage{i}", [128, 2560], F32) for i in range(2)])
        wkv, bwkv = self.T("wkv", [128, 8, 1024], BF16)
        self.load_w_gain(wkv, bwkv, W["w_mem_kv"], 8, 1024, g, bg)
        gq, bgq = self.T("gq", [128, 2], F32)
        gk, bgk = self.T("gk", [128, 2], F32)
        c.dma("sp", gq[:], W["mem_qn_g"].rearrange("l d -> d l"), writes=[bgq], allow_slow_non_contiguous=True)
        c.dma("sp", gk[:], W["mem_kn_g"].rearrange("l d -> d l"), writes=[bgk], allow_slow_non_contiguous=True)
        c.op("dve", "scalar_tensor_tensor", reads=[bgq, bgk], writes=[bgq], out=gq[:], in0=gq[:],
             scalar=float(128 ** -0.5), in1=gk[:], op0=ALU.mult, op1=ALU.mult)
        mt, bmt = self.T("memt", [128, 2, 1024], F32)
        c.dma("sp", mt[:], self.mem.rearrange("(c p) d -> p c d", p=128), writes=[bmt])
        ms, bms = self.T("mems", [128, 1024], BF16)
        junk, bjunk = self.T("junk0", [128, 1024], BF16)
        ss, bss = self.T("ss0", [128, 2], F32)
        mT, bmT = self.T("memT", [128, 8, 256], BF16)
        pTv = self.pT[0][:].bitcast(BF16)
        bpT = self.pT[1]
        for ch in range(2):
            c.op("act", "activation", reads=[bmt], writes=[bjunk, bss], out=junk[:], in_=mt[:, ch, :], func=AF.Square,
                 accum_out=ss[:, ch:ch + 1])
        self.rsqrt_(ss[:], bss, 1.0 / D, 2)
        for ch in range(2):
            c.op("dve", "tensor_scalar", reads=[bmt, bss], writes=[bms], out=ms[:], in0=mt[:, ch, :],
                 scalar1=ss[:, ch:ch + 1], scalar2=None, op0=ALU.mult)
            for k in range(8):
                c.op("pe", "transpose", reads=[bms, self.identb[1]], writes=[bpT], out=pTv[:, k * 128:(k + 1) * 128],
                     in_=ms[:, k * 128:(k + 1) * 128], identity=self.identb[0][:])
            c.op("act", "copy", reads=[bpT], writes=[bmT], out=mT[:, :, ch * 128:(ch + 1) * 128],
                 in_=pTv.rearrange("p (k t) -> p k t", k=8))
        c.op("pool", "memset", writes=[self.Vaug[1]], ap=self.Vaug[0][:, :, :, 128:130], constant=1.0)
        kss, bkss = self.T("kss", [128, 4], F32)
        kn, bkn = self.T("kn", [128, 4, 128], BF16)
        for ch in range(2):
            pk, bpk = self.mmrot.next()
            for k in range(8):
                c.op("pe", "matmul", reads=[bmT, bwkv], writes=[bpk], out=pk[:], lhsT=mT[:, k, ch * 128:(ch + 1) * 128],
                     rhs=wkv[:, k, 0:512], start=(k == 0), stop=(k == 7))
            for h in range(4):
                c.op("act", "activation", reads=[bpk], writes=[bjunk, bkss], out=junk[:, 0:128], in_=pk[:, h * 128:(h + 1) * 128],
                     func=AF.Square, accum_out=kss[:, h:h + 1])
            self.rsqrt_(kss[:], bkss, 1.0 / 128, 4)
            c.op("dve", "tensor_tensor", reads=[bpk, bkss], writes=[bkn], out=kn[:],
                 in0=pk[:].rearrange("p (h d) -> p h d", h=4), in1=kss[:].unsqueeze(2).to_broadcast([128, 4, 128]), op=ALU.mult)
            for h in range(4):
                c.op("pe", "transpose", reads=[bkn, self.identb[1]], writes=[bpT], out=pTv[:, h * 128:(h + 1) * 128],
                     in_=kn[:, h, :], identity=self.identb[0][:])
            for li in range(2):
                c.op("dve", "tensor_scalar", reads=[bpT, bgq], writes=[self.kT[li][1]],
                     out=self.kT[li][0][:, :, ch * 128:(ch + 1) * 128], in0=pTv[:, 0:512].rearrange("p (h m) -> p h m", h=4),
                     scalar1=gq[:, li:li + 1], scalar2=None, op0=ALU.mult)
            pv, bpv = self.mmrot.next()
            for k in range(8):
                c.op("pe", "matmul", reads=[bmT, bwkv], writes=[bpv], out=pv[:], lhsT=mT[:, k, ch * 128:(ch + 1) * 128],
                     rhs=wkv[:, k, 512:1024], start=(k == 0), stop=(k == 7))
            c.op("act", "copy", reads=[bpv], writes=[self.Vaug[1]], out=self.Vaug[0][:, ch, :, 0:128],
                 in_=pv[:].rearrange("p (h d) -> p h d", h=4))

    def alloc_tile_bufs(self, full=True, deep=False, nht=2):
        self.xt = Rot([self.T(f"xt{i}", [128, 4, 1024], F32) for i in range((3 if deep else 2) if full else 1)])
        self.hTrot = Rot([self.T(f"hT{i}", [128, 8, 512], BF16) for i in range(nht)])
        self.hT = self.hTrot.items[0]
        self.xs = Rot([self.T(f"xs{i}", [128, 1024], BF16) for i in range(2)])
        self.junk = self.T("junk", [128, 1024], BF16)
        self.ssn = self.T("ssn", [128, 4], F32)
        self.qT = self.T("qT", [128, 4, 512], BF16)
        self.qn = Rot([self.T(f"qn{i}", [128, 4, 128], BF16) for i in range(2)])
        self.qss = Rot([self.T(f"qss{i}", [128, 4], F32) for i in range(2)])
        self.PTm = Rot([self.T(f"PTm{i}", [128, 512], BF16) for i in range(3)])
        self.mrd = Rot([self.T(f"mrd{i}", [128, 4], F32) for i in range(2)])
        self.mon = Rot([self.T(f"mon{i}", [128, 4, 128], BF16) for i in range(2)])
        if not full:
            return
        self.catTr = Rot([self.T(f"catT{i}", [128, 12, 512], BF16) for i in range(2 if deep else 1)])
        self.catT = self.catTr.items[0]
        self.h2 = Rot([self.T(f"h2_{i}", [128, 1024], F32) for i in range(2 if deep else 1)])
        self.h2Tf = Rot([self.T(f"h2Tf{i}", [128, 8, 128], F32) for i in range(2 if deep else 1)])
        self.h2bt = Rot([self.T(f"h2bt{i}", [128, 1024], BF16) for i in range(2)])
        self.rt = Rot([dict((n, self.T(f"rt_{n}{i}", [128, w], F32)) for n, w in
                            (("lg", 36), ("gmax", 1), ("ngmax", 1), ("gexp", 4), ("gsum", 1), ("oh", 4), ("tmp", 32),
                             ("esel", 8), ("m1", 1), ("nm1", 1), ("sel1", 8), ("es2", 8), ("m2", 1), ("sel2", 8),
                             ("p2", 1), ("w1", 1), ("w2", 1), ("R", 66))) for i in range(4)])

    def norm_to_hT(self, xt, bxt):
        c = self.c
        ss, bss = self.ssn
        junk, bjunk = self.junk
        self.hT = self.hTrot.next()
        hT, bhT = self.hT
        pTv = self.pT[0][:].bitcast(BF16)
        bpT = self.pT[1]
        for ch in range(4):
            c.op("act", "activation", reads=[bxt], writes=[bjunk, bss], out=junk[:], in_=xt[:, ch, :], func=AF.Square,
                 accum_out=ss[:, ch:ch + 1])
        self.rsqrt_(ss[:], bss, 1.0 / D, 4)
        for ch in range(4):
            xs, bxs = self.xs.next()
            c.op("dve", "tensor_scalar", reads=[bxt, bss], writes=[bxs], out=xs[:], in0=xt[:, ch, :],
                 scalar1=ss[:, ch:ch + 1], scalar2=None, op0=ALU.mult)
            for k in range(8):
                c.op("pe", "transpose", reads=[bxs, self.identb[1]], writes=[bpT], out=pTv[:, k * 128:(k + 1) * 128],
                     in_=xs[:, k * 128:(k + 1) * 128], identity=self.identb[0][:])
            c.op("act", "copy", reads=[bpT], writes=[bhT], out=hT[:, :, ch * 128:(ch + 1) * 128],
                 in_=pTv.rearrange("p (k t) -> p k t", k=8))

    def qmem_and_attn(self, li, win, bwin, col0, dst, bdst, dst_k0):
        c = self.c
        hT, bhT = self.hT
        qT, bqT = self.qT
        junk, bjunk = self.junk
        pTv = self.pT[0][:].bitcast(BF16)
        bpT = self.pT[1]
        for ch in range(4):
            pq, bpq = self.mmrot.next()
            for k in range(8):
                c.op("pe", "matmul", reads=[bhT, bwin], writes=[bpq], out=pq[:], lhsT=hT[:, k, ch * 128:(ch + 1) * 128],
                     rhs=win[:, k, col0:col0 + 512], start=(k == 0), stop=(k == 7))
            qss, bqss = self.qss.next()
            for h in range(4):
                c.op("act", "activation", reads=[bpq], writes=[bjunk, bqss], out=junk[:, 0:128], in_=pq[:, h * 128:(h + 1) * 128],
                     func=AF.Square, accum_out=qss[:, h:h + 1])
            self.rsqrt_(qss[:], bqss, 1.0 / 128, 4)
            qn, bqn = self.qn.next()
            c.op("dve", "tensor_tensor", reads=[bpq, bqss], writes=[bqn], out=qn[:],
                 in0=pq[:].rearrange("p (h d) -> p h d", h=4), in1=qss[:].unsqueeze(2).to_broadcast([128, 4, 128]), op=ALU.mult)
            for h in range(4):
                c.op("pe", "transpose", reads=[bqn, self.identb[1]], writes=[bpT], out=pTv[:, h * 128:(h + 1) * 128],
                     in_=qn[:, h, :], identity=self.identb[0][:])
            c.op("act", "copy", reads=[bpT], writes=[bqT], out=qT[:, :, ch * 128:(ch + 1) * 128],
                 in_=pTv[:, 0:512].rearrange("p (h t) -> p h t", h=4))
        kT, bkT = self.kT[li]
        Va, bVa = self.Vaug
        for h in range(4):
            pts = []
            for mc in range(2):
                psc, bpsc = self.mmrot.next()
                c.op("pe", "matmul", reads=[bkT, bqT], writes=[bpsc], out=psc[:], lhsT=kT[:, h, mc * 128:(mc + 1) * 128],
                     rhs=qT[:, h, :], start=True, stop=True)
                PT, bPT = self.PTm.next()
                c.op("act", "activation", reads=[bpsc], writes=[bPT], out=PT[:], in_=psc[:], func=AF.Exp)
                pts.append((PT, bPT))
            banks = [self.mmrot.next(), self.mmrot.next()]
            for qc in range(4):
                ob, bob = banks[qc // 2]
                col = (qc % 2) * 130
                for mc in range(2):
                    c.op("pe", "matmul", reads=[bVa, pts[mc][1]], writes=[bob], out=ob[:, col:col + 130],
                         lhsT=pts[mc][0][:, qc * 128:(qc + 1) * 128], rhs=Va[:, mc, h, :], start=(mc == 0 and qc % 2 == 0),
                         stop=(mc == 1), skip_group_check=True)
            rd, brd = self.mrd.next()
            on, bon = self.mon.next()
            for qc in range(4):
                ob, bob = banks[qc // 2]
                col = (qc % 2) * 130
                c.op("dve", "reciprocal", reads=[bob], writes=[brd], out=rd[:, qc:qc + 1], in_=ob[:, col + 128:col + 129])
                c.op("dve", "tensor_scalar", reads=[bob, brd], writes=[bon], out=on[:, qc, :], in0=ob[:, col:col + 128],
                     scalar1=rd[:, qc:qc + 1], scalar2=None, op0=ALU.mult)
                c.op("pe", "transpose", reads=[bon, self.identb[1]], writes=[bpT], out=pTv[:, qc * 128:(qc + 1) * 128],
                     in_=on[:, qc, :], identity=self.identb[0][:])
            c.op("act", "copy", reads=[bpT], writes=[bdst], out=dst[:, dst_k0 + h, :], in_=pTv[:, 0:512])

    def out_proj_norm2_router(self, li, j, xt, bxt, wout, bwout):
        c = self.c
        catT, bcat = self.catT
        S = self.S
        junk, bjunk = self.junk
        for ch in range(4):
            for half in range(2):
                py, bpy = self.mmrot.next()
                for k in range(12):
                    c.op("pe", "matmul", reads=[bcat, bwout], writes=[bpy], out=py[:], lhsT=catT[:, k, ch * 128:(ch + 1) * 128],
                         rhs=wout[:, k, half * 512:(half + 1) * 512], start=(k == 0), stop=(k == 11))
                c.op("dve", "tensor_tensor", reads=[bpy, bxt], writes=[bxt], out=xt[:, ch, half * 512:(half + 1) * 512],
                     in0=py[:], in1=xt[:, ch, half * 512:(half + 1) * 512], op=ALU.add)
        c.dma("sp", self.xmid[li][j * 512:(j + 1) * 512, :].rearrange("(c p) d -> p c d", p=128), xt[:], reads=[bxt],
              writes=[self.dbuf(f"xmid{li}", j)])
        import os
        STG = float(os.environ.get("STG", "99"))
        if STG < 6:
            return
        ss, bss = self.ssn
        for ch in range(4):
            c.op("act", "activation", reads=[bxt], writes=[bjunk, bss], out=junk[:], in_=xt[:, ch, :], func=AF.Square,
                 accum_out=ss[:, ch:ch + 1])
        self.rsqrt_(ss[:], bss, 1.0 / D, 4)
        g2, bg2 = self.g2bc[li]
        wr, bwr = self.wr[li]
        rb, brb = self.rbias[li]
        pw, bpw = self.pw
        for ch in range(4):
            h2, bh2 = self.h2.next()
            c.op("dve", "scalar_tensor_tensor", reads=[bxt, bss, bg2], writes=[bh2], out=h2[:], in0=xt[:, ch, :],
                 scalar=ss[:, ch:ch + 1], in1=g2[:], op0=ALU.mult, op1=ALU.mult)
            if STG < 6.2:
                continue
            for k in range(8):
                c.op("pe", "transpose", reads=[bh2, self.identf[1]], writes=[bpw], out=pw[:, k * 128:(k + 1) * 128],
                     in_=h2[:, k * 128:(k + 1) * 128], identity=self.identf[0][:])
            if STG < 6.4:
                continue
            h2Tf, bh2Tf = self.h2Tf.next()
            c.op("act", "copy", reads=[bpw], writes=[bh2Tf], out=h2Tf[:], in_=pw[:].rearrange("p (k t) -> p k t", k=8))
            hb, bhb = self.h2bt.next()
            c.op("pool", "tensor_copy", reads=[bh2], writes=[bhb], out=hb[:], in_=h2[:])
            gch_ = j * 4 + ch
            c.dma("sp", self.h2b[li][gch_ * 128:(gch_ + 1) * 128, :], hb[:], reads=[bhb], writes=[self.dbuf(f"h2b{li}", gch_)])
            if STG < 7:
                continue
            pl, bpl = self.mmrot.next()
            for k in range(8):
                c.op("pe", "matmul", reads=[bh2Tf, bwr], writes=[bpl], out=pl[:, 0:36], lhsT=h2Tf[:, k, :], rhs=wr[:, k, :],
                     start=(k == 0), stop=(k == 7))
            if STG < 8:
                continue
            self.router(li, j * 4 + ch, pl, bpl, rb, brb)

    def router(self, li, gch, pl, bpl, rb, brb):
        c = self.c
        R = self.rt.next()

        def t(n):
            return R[n][0]

        def b(n):
            return R[n][1]
        c.op("dve", "tensor_tensor", reads=[bpl, brb], writes=[b("lg")], out=t("lg")[:], in0=pl[:, 0:36], in1=rb[:], op=ALU.add)
        gl = t("lg")[:, 0:4]
        el = t("lg")[:, 4:36]
        c.op("dve", "tensor_reduce", reads=[b("lg")], writes=[b("gmax")], out=t("gmax")[:], in_=gl, axis=AX.X, op=ALU.max)
        c.op("dve", "tensor_scalar", reads=[b("gmax")], writes=[b("ngmax")], out=t("ngmax")[:], in0=t("gmax")[:], scalar1=-1.0,
             scalar2=None, op0=ALU.mult)
        c.op("act", "activation", reads=[b("lg"), b("ngmax")], writes=[b("gexp"), b("gsum")], out=t("gexp")[:], in_=gl,
             func=AF.Exp, bias=t("ngmax")[:], accum_out=t("gsum")[:])
        c.op("dve", "reciprocal", reads=[b("gsum")], writes=[b("gsum")], out=t("gsum")[:], in_=t("gsum")[:])
        c.op("dve", "tensor_scalar", reads=[b("lg"), b("gmax")], writes=[b("oh")], out=t("oh")[:], in0=gl, scalar1=t("gmax")[:],
             scalar2=None, op0=ALU.is_equal)
        c.op("dve", "tensor_tensor", reads=[b("lg"), b("oh")], writes=[b("tmp")], out=t("tmp")[:].rearrange("p (g e) -> p g e", g=4),
             in0=el.rearrange("p (g e) -> p g e", g=4), in1=t("oh")[:].unsqueeze(2).to_broadcast([128, 4, 8]), op=ALU.mult)
        c.op("dve", "tensor_reduce", reads=[b("tmp")], writes=[b("esel")], out=t("esel")[:],
             in_=t("tmp")[:].rearrange("p (g e) -> p e g", g=4), axis=AX.X, op=ALU.add)
        c.op("dve", "tensor_reduce", reads=[b("esel")], writes=[b("m1")], out=t("m1")[:], in_=t("esel")[:], axis=AX.X, op=ALU.max)
        c.op("dve", "tensor_scalar", reads=[b("esel"), b("m1")], writes=[b("sel1")], out=t("sel1")[:], in0=t("esel")[:],
             scalar1=t("m1")[:], scalar2=None, op0=ALU.is_equal)
        c.op("dve", "scalar_tensor_tensor", reads=[b("sel1"), b("esel")], writes=[b("es2")], out=t("es2")[:], in0=t("sel1")[:],
             scalar=-1e30, in1=t("esel")[:], op0=ALU.mult, op1=ALU.add)
        c.op("dve", "tensor_reduce", reads=[b("es2")], writes=[b("m2")], out=t("m2")[:], in_=t("es2")[:], axis=AX.X, op=ALU.max)
        c.op("dve", "tensor_scalar", reads=[b("es2"), b("m2")], writes=[b("sel2")], out=t("sel2")[:], in0=t("es2")[:],
             scalar1=t("m2")[:], scalar2=None, op0=ALU.is_equal)
        c.op("dve", "tensor_scalar", reads=[b("m1")], writes=[b("nm1")], out=t("nm1")[:], in0=t("m1")[:], scalar1=-1.0,
             scalar2=None, op0=ALU.mult)
        c.op("act", "activation", reads=[b("m2"), b("nm1")], writes=[b("p2")], out=t("p2")[:], in_=t("m2")[:], func=AF.Exp,
             bias=t("nm1")[:])
        c.op("dve", "tensor_scalar", reads=[b("p2")], writes=[b("w1")], out=t("w1")[:], in0=t("p2")[:], scalar1=1.0, scalar2=None,
             op0=ALU.add)
        c.op("dve", "reciprocal", reads=[b("w1")], writes=[b("w1")], out=t("w1")[:], in_=t("w1")[:])
        c.op("dve", "tensor_tensor", reads=[b("w1"), b("gsum")], writes=[b("w1")], out=t("w1")[:], in0=t("w1")[:], in1=t("gsum")[:],
             op=ALU.mult)
        c.op("dve", "tensor_tensor", reads=[b("w1"), b("p2")], writes=[b("w2")], out=t("w2")[:], in0=t("w1")[:], in1=t("p2")[:],
             op=ALU.mult)
        ohb = t("oh")[:].unsqueeze(2).to_broadcast([128, 4, 8])
        c.op("dve", "tensor_tensor", reads=[b("oh"), b("sel1")], writes=[b("R")], out=t("R")[:, 0:32].rearrange("p (g e) -> p g e", g=4),
             in0=ohb, in1=t("sel1")[:].unsqueeze(1).to_broadcast([128, 4, 8]), op=ALU.mult)
        c.op("dve", "tensor_tensor", reads=[b("oh"), b("sel2")], writes=[b("R")], out=t("R")[:, 32:64].rearrange("p (g e) -> p g e", g=4),
             in0=ohb, in1=t("sel2")[:].unsqueeze(1).to_broadcast([128, 4, 8]), op=ALU.mult)
        c.op("dve", "tensor_copy", reads=[b("w1")], writes=[b("R")], out=t("R")[:, 64:65], in_=t("w1")[:])
        c.op("dve", "tensor_copy", reads=[b("w2")], writes=[b("R")], out=t("R")[:, 65:66], in_=t("w2")[:])
        c.dma("sp", self.rinfo[li][gch * 128:(gch + 1) * 128, :], t("R")[:], reads=[b("R")], writes=[self.dbuf(f"rinfo{li}", gch)])

    def load_router_weights(self):
        if hasattr(self, "g2bc"):
            return
        c = self.c
        W = self.W
        self.g2bc, self.wr, self.rbias = [], [], []
        for li in range(2):
            self.g2bc.append(self.load_bcast(f"g2bc{li}", W["norm2_g"][li], 1024))
            wr, bwr = self.T(f"wr{li}", [128, 8, 36], F32)
            c.dma("sp", wr[:, :, 0:4], W["moe_w_group"][li].rearrange("(k p) n -> p k n", p=128), writes=[bwr])
            c.dma("sp", wr[:, :, 4:36], W["moe_w_expert"][li].rearrange("(k p) n -> p k n", p=128), writes=[bwr])
            self.wr.append((wr, bwr))
            rb, brb = self.T(f"rbias{li}", [128, 36], F32)
            c.dma("sp", rb[:, 0:4], W["moe_b_group"][li].partition_broadcast(128), writes=[brb])
            c.dma("sp", rb[:, 4:36], W["moe_b_expert"][li].partition_broadcast(128), writes=[brb])
            self.rbias.append((rb, brb))

    def p1_gmlp(self):
        c = self.c
        W = self.W
        win, bwin = self.T("a_win", [128, 8, 2560], BF16)
        wout, bwout = self.T("a_wout", [128, 12, 1024], BF16)
        WsT, bWsT = self.T("WsT", [128, 8, 128], BF16)
        Cg, bCg = self.T("Cg", [128, 8, 128], F32)
        lng, blng = self.load_small_cols("lng", W["a_ln_g"][0], 8)
        self.load_w_cast(wout, bwout, W["a_w_out"][0], 12)
        with c.scope():
            self.p1_setup(win, bwin, WsT, bWsT, Cg, bCg)
        self.alloc_tile_bufs(nht=1)
        uT, buT = self.T("uT", [128, 8, 512], BF16)
        vrot = Rot([self.T(f"v{i}", [128, 1024], F32) for i in range(2)])
        vnrot = Rot([self.T(f"vn{i}", [128, 1024], BF16) for i in range(2)])
        strot = Rot([self.T(f"bnst{i}", [128, 2, 6], F32) for i in range(2)])
        mvrot = Rot([self.T(f"mv{i}", [128, 2], F32) for i in range(2)])
        gtrot = Rot([self.T(f"gt{i}", [128, 8, 128], F32) for i in range(2)])
        self.zero_fill_xs([bwin, bwout])
        c.barrier()
        c.warm_segs[c.segs[-1]] = True
        self.p1_main(win, bwin, wout, bwout, WsT, bWsT, Cg, bCg, lng, blng, uT, buT, vrot, vnrot, strot, mvrot, gtrot)

    def p1_setup(self, win, bwin, WsT, bWsT, Cg, bCg):
        c = self.c
        W = self.W
        self.wstage = Rot([self.T(f"wstage{i}", [128, 2560], F32) for i in range(2)])
        n1, bn1 = self.load_small_cols("n1g0", W["norm1_g"][0], 8)
        self.load_w_gain(win, bwin, W["a_w_in"][0], 8, 2560, n1, bn1)
        wsf, bwsf = self.T("wsf", [128, 8, 128], F32)
        c.dma("sp", wsf[:], W["a_w_s"][0].rearrange("g t s -> t g s"), writes=[bwsf])
        for g in range(8):
            c.op("pool", "affine_select", reads=[bwsf], writes=[bwsf], out=wsf[:, g, :], in_=wsf[:, g, :], pattern=[[-1, 128]],
                 compare_op=ALU.is_ge, fill=0.0, base=0, channel_multiplier=1)
        wsb, bwsb = self.T("wsb", [128, 8, 128], BF16)
        c.op("dve", "tensor_copy", reads=[bwsf], writes=[bwsb], out=wsb[:], in_=wsf[:])
        pTv = self.pT[0][:].bitcast(BF16)
        bpT = self.pT[1]
        for g in range(8):
            c.op("pe", "transpose", reads=[bwsb, self.identb[1]], writes=[bpT], out=pTv[:, g * 128:(g + 1) * 128],
                 in_=wsb[:, g, :], identity=self.identb[0][:])
        c.op("act", "copy", reads=[bpT], writes=[bWsT], out=WsT[:], in_=pTv.rearrange("p (g t) -> p g t", g=8))
        betab, bbetab = self.T("betab", [128, 1024], F32)
        c.dma("sp", betab[:], W["a_ln_b"][0].partition_broadcast(128), writes=[bbetab])
        betabb, bbetabb = self.T("betabb", [128, 1024], BF16)
        c.op("dve", "tensor_copy", reads=[bbetab], writes=[bbetabb], out=betabb[:], in_=betab[:])
        bsr, bbsr = self.T("bsr", [1, 1024], F32)
        c.dma("sp", bsr[:], W["a_b_s"][0].rearrange("g t -> (g t)").unsqueeze(0), writes=[bbsr])
        bsrb, bbsrb = self.T("bsrb", [1, 1024], BF16)
        c.op("dve", "tensor_copy", reads=[bbsr], writes=[bbsrb], out=bsrb[:], in_=bsr[:])
        pw, bpw = self.pw
        for g in range(8):
            c.op("pe", "matmul", reads=[bbetabb, bWsT], writes=[bpw], out=pw[:, g * 128:(g + 1) * 128],
                 lhsT=betabb[:, g * 128:(g + 1) * 128], rhs=WsT[:, g, :], start=True, stop=False)
            c.op("pe", "matmul", reads=[self.onesb[1], bbsrb], writes=[bpw], out=pw[:, g * 128:(g + 1) * 128],
                 lhsT=self.onesb[0][0:1, :], rhs=bsrb[0:1, g * 128:(g + 1) * 128], start=False, stop=True)
        c.op("act", "copy", reads=[bpw], writes=[bCg], out=Cg[:], in_=pw[:].rearrange("p (g t) -> p g t", g=8))

    def p1_main(self, win, bwin, wout, bwout, WsT, bWsT, Cg, bCg, lng, blng, uT, buT, vrot, vnrot, strot, mvrot, gtrot):
        c = self.c
        import os
        STG = float(os.environ.get("STG", "99"))
        if STG < 1:
            return
        pw, bpw = self.pw
        catT, bcat = self.catT
        for j in range(self.NT):
            xt, bxt = self.xt.next()
            c.dma("sp", xt[:], self.x[j * 512:(j + 1) * 512, :].rearrange("(c p) d -> p c d", p=128), writes=[bxt])
            self.norm_to_hT(xt, bxt)
            hT, bhT = self.hT
            if STG < 2:
                continue
            for n in range(8):
                pu, bpu = self.mmrot.next()
                for k in range(8):
                    c.op("pe", "matmul", reads=[bwin, bhT], writes=[bpu], out=pu[:], lhsT=win[:, k, n * 128:(n + 1) * 128],
                         rhs=hT[:, k, :], start=(k == 0), stop=(k == 7))
                c.op("act", "activation", reads=[bpu], writes=[buT], out=uT[:, n, :], in_=pu[:], func=AF.Gelu)
            if STG < 3:
                continue
            for ch in range(4):
                v, bv = vrot.next()
                for half in range(2):
                    pv, bpv = self.mmrot.next()
                    for k in range(8):
                        c.op("pe", "matmul", reads=[bhT, bwin], writes=[bpv], out=pv[:], lhsT=hT[:, k, ch * 128:(ch + 1) * 128],
                             rhs=win[:, k, 1024 + half * 512:1024 + (half + 1) * 512], start=(k == 0), stop=(k == 7))
                    c.op("act", "activation", reads=[bpv], writes=[bv], out=v[:, half * 512:(half + 1) * 512], in_=pv[:], func=AF.Gelu)
                st, bst = strot.next()
                mv, bmv = mvrot.next()
                for half in range(2):
                    c.op("dve", "bn_stats", reads=[bv], writes=[bst], out=st[:, half, :], in_=v[:, half * 512:(half + 1) * 512])
                c.op("dve", "bn_aggr", reads=[bst], writes=[bmv], out=mv[:], in_=st[:].rearrange("p a b -> p (a b)"))
                self.rsqrt_(mv[:, 1:2], bmv, 1.0, 1)
                vn, bvn = vnrot.next()
                c.op("dve", "tensor_scalar", reads=[bv, bmv], writes=[bvn], out=vn[:], in0=v[:], scalar1=mv[:, 0:1], scalar2=mv[:, 1:2],
                     op0=ALU.subtract, op1=ALU.mult)
                for g in range(8):
                    c.op("pe", "matmul", reads=[bvn, bWsT], writes=[bpw], out=pw[:, g * 128:(g + 1) * 128],
                         lhsT=vn[:, g * 128:(g + 1) * 128], rhs=WsT[:, g, :], start=True, stop=True)
                gt, bgt = gtrot.next()
                for g in range(8):
                    c.op("dve", "scalar_tensor_tensor", reads=[bpw, blng, bCg], writes=[bgt], out=gt[:, g, :],
                         in0=pw[:, g * 128:(g + 1) * 128], scalar=lng[:, g:g + 1], in1=Cg[:, g, :], op0=ALU.mult, op1=ALU.add)
                c.op("pool", "tensor_tensor", reads=[bgt, buT], writes=[bcat], out=catT[:, 0:8, ch * 128:(ch + 1) * 128],
                     in0=gt[:], in1=uT[:, :, ch * 128:(ch + 1) * 128], op=ALU.mult)
            if STG < 4:
                continue
            self.qmem_and_attn(0, win, bwin, 2048, catT, bcat, 8)
            if STG < 5:
                continue
            self.out_proj_norm2_router(0, j, xt, bxt, wout, bwout)

    def moe(self, li, xin, xout):
        c = self.c
        W = self.W
        S, ST = self.S, self.ST
        nst = S // ST
        ncs = ST // 128
        nts = ST // 512
        if True:
            self.moe_bufs = dict(
                xacc=self.T("xacc", [128, ncs, 1024], F32),
                h2s=self.T("h2s", [128, 8, ST], BF16),
                cmb=self.T("cmb", [128, ncs, NE], F32),
                wg=Rot([self.T(f"wg{i}", [128, 8, 256], BF16) for i in range(2)]),
                wu=Rot([self.T(f"wu{i}", [128, 8, 256], BF16) for i in range(2)]),
                wd=Rot([self.T(f"wd{i}", [128, 2, 1024], BF16) for i in range(2)]),
                sg=Rot([self.T(f"sg{i}", [128, 512], BF16) for i in range(2)]),
                a=Rot([self.T(f"a{i}", [128, 512], BF16) for i in range(4)]),
            )
        mb = self.moe_bufs
        xacc, bxacc = mb["xacc"]
        h2s, bh2s = mb["h2s"]
        cmb, bcmb = mb["cmb"]
        pg = [self.pmm[0], self.pmm[1]]
        pu = [self.pmm[2], self.pmm[3]]
        yrot = Rot([(self.pw[0][:, 0:512], self.pw[1]), (self.pT[0][:], self.pT[1]), (self.pmisc[0][:], self.pmisc[1])])
        for st in range(nst):
            r0 = st * ST
            tiles = range(st * nts, (st + 1) * nts)
            c.dma("sp", xacc[:], xin[r0:r0 + ST, :].rearrange("(c p) d -> p c d", p=128),
                  reads=[self.dbuf(f"xmid{li}", t) for t in tiles], writes=[bxacc])
            c.dma("sp", h2s[:], self.h2T[li][:, :, r0:r0 + ST].rearrange("k p t -> p k t"),
                  reads=[self.dbuf(f"h2T{li}", t) for t in tiles], writes=[bh2s])
            c.dma("sp", cmb[:], self.comb[li][r0:r0 + ST, :].rearrange("(c p) e -> p c e", p=128),
                  reads=[self.dbuf(f"comb{li}", ch) for ch in range(st * ncs, (st + 1) * ncs)], writes=[bcmb])
            for e in range(NE):
                wg, bwg = mb["wg"].next()
                wu, bwu = mb["wu"].next()
                wd, bwd = mb["wd"].next()
                c.dma("pool", wg[:], W["moe_w_gate"][li, e].rearrange("(k p) f -> p k f", p=128), writes=[bwg])
                c.dma("pool", wu[:], W["moe_w_up"][li, e].rearrange("(k p) f -> p k f", p=128), writes=[bwu])
                c.dma("pool", wd[:], W["moe_w_down"][li, e].rearrange("(k p) n -> p k n", p=128), writes=[bwd])
                for t in range(nts):
                    acts = []
                    for fc in range(2):
                        g_, bg_ = pg[fc]
                        u_, bu_ = pu[fc]
                        for k in range(8):
                            c.op("pe", "matmul", reads=[bwg, bh2s], writes=[bg_], out=g_[:], lhsT=wg[:, k, fc * 128:(fc + 1) * 128],
                                 rhs=h2s[:, k, t * 512:(t + 1) * 512], start=(k == 0), stop=(k == 7))
                        for k in range(8):
                            c.op("pe", "matmul", reads=[bwu, bh2s], writes=[bu_], out=u_[:], lhsT=wu[:, k, fc * 128:(fc + 1) * 128],
                                 rhs=h2s[:, k, t * 512:(t + 1) * 512], start=(k == 0), stop=(k == 7))
                        sg, bsg = mb["sg"].next()
                        c.op("act", "activation", reads=[bg_], writes=[bsg], out=sg[:], in_=g_[:], func=AF.Silu)
                        a, ba = mb["a"].next()
                        c.op("dve", "tensor_tensor", reads=[bsg, bu_], writes=[ba], out=a[:], in0=sg[:], in1=u_[:], op=ALU.mult)
                        acts.append((a, ba))
                    for ch in range(4):
                        gc = t * 4 + ch
                        for half in range(2):
                            py, bpy = yrot.next()
                            for fc in range(2):
                                c.op("pe", "matmul", reads=[acts[fc][1], bwd], writes=[bpy], out=py,
                                     lhsT=acts[fc][0][:, ch * 128:(ch + 1) * 128], rhs=wd[:, fc, half * 512:(half + 1) * 512],
                                     start=(fc == 0), stop=(fc == 1))
                            c.op("dve", "scalar_tensor_tensor", reads=[bpy, bcmb, bxacc], writes=[bxacc],
                                 out=xacc[:, gc, half * 512:(half + 1) * 512], in0=py, scalar=cmb[:, gc, e:e + 1],
                                 in1=xacc[:, gc, half * 512:(half + 1) * 512], op0=ALU.mult, op1=ALU.add)
            oname = "x1" if li == 0 else "out"
            c.dma("sp", xout[r0:r0 + ST, :].rearrange("(c p) d -> p c d", p=128), xacc[:], reads=[bxacc],
                  writes=[self.dbuf(oname, t) for t in tiles])

    def zero_fill_xs(self, after):
        c = self.c
        self.zt = self.T("zt", [128, 2048], BF16)
        c.op("pool", "memset", writes=[self.zt[1]], ap=self.zt[0][:], constant=0.0)
        for li in range(1):
            if "p2" not in self.phases and "p6" not in self.phases:
                continue
            rows_per = 128 * 2
            for i in range(self.NCAP // rows_per):
                bz = Buf()
                c.dma("act", self.Xs[li][i * rows_per:(i + 1) * rows_per, :].rearrange("(p a) d -> p (a d)", p=128), self.zt[0][:],
                      reads=[self.zt[1]] + list(after), writes=[bz])
                self.zf[0].append(bz)
                self.zf[1].append(bz)

    def moe_sparse(self, li, xin, xout):
        c = self.c
        W = self.W
        S, NCH, NT_, NCAP = self.S, self.NCH, self.NTILES, self.NCAP
        IOA = bass.IndirectOffsetOnAxis
        MT = max(1, (2 * S) // 512)
        FL = NCH * 32
        R, bR = self.T("mR", [128, NCH, 66], F32)
        c.dma("sp", R[:], self.rinfo[li].rearrange("(c p) e -> p c e", p=128),
              reads=[self.dbuf(f"rinfo{li}", g) for g in range(NCH)], writes=[bR])
        posi, bposi = self.T("mposi", [128, 2, NCH], I32)
        widx, bwidx = self.T("mwidx", [128, NT_], I32)
        with c.scope():
            Mb, bMb = self.T("mMb", [128, NCH, 32], BF16)
            c.op("dve", "tensor_tensor", reads=[bR], writes=[bMb], out=Mb[:], in0=R[:, :, 0:32], in1=R[:, :, 32:64], op=ALU.add)
            Ls, bLs = self.T("mLs", [128, 128], BF16)
            c.op("pool", "memset", writes=[bLs], ap=Ls[:], constant=1.0)
            c.op("pool", "affine_select", reads=[bLs], writes=[bLs], out=Ls[:], in_=Ls[:], pattern=[[1, 128]], compare_op=ALU.is_gt,
                 fill=0.0, base=0, channel_multiplier=-1)
            rank, brank = self.T("mrank", [128, NCH, 32], F32)
            cnt, bcnt = self.T("mcnt", [128, NCH, 32], F32)
            Mbf = Mb[:].rearrange("p c e -> p (c e)")
            rankf = rank[:].rearrange("p c e -> p (c e)")
            cntf = cnt[:].rearrange("p c e -> p (c e)")
            for g0 in range(0, FL, 512):
                n = min(512, FL - g0)
                p1, bp1 = self.mmrot.next()
                c.op("pe", "matmul", reads=[bLs, bMb], writes=[bp1], out=p1[:, 0:n], lhsT=Ls[:], rhs=Mbf[:, g0:g0 + n], start=True, stop=True)
                c.op("act", "copy", reads=[bp1], writes=[brank], out=rankf[:, g0:g0 + n], in_=p1[:, 0:n])
                p2, bp2 = self.mmrot.next()
                c.op("pe", "matmul", reads=[self.onesb[1], bMb], writes=[bp2], out=p2[:, 0:n], lhsT=self.onesb[0][:], rhs=Mbf[:, g0:g0 + n],
                     start=True, stop=True)
                c.op("dve", "tensor_copy", reads=[bp2], writes=[bcnt], out=cntf[:, g0:g0 + n], in_=p2[:, 0:n])
            pre, bpre = self.T("mpre", [128, NCH, 32], F32)
            c.op("dve", "memset", writes=[bpre], ap=pre[:, 0, :], constant=0.0)
            for ch in range(1, NCH):
                c.op("dve", "tensor_tensor", reads=[bpre, bcnt], writes=[bpre], out=pre[:, ch, :], in0=pre[:, ch - 1, :], in1=cnt[:, ch - 1, :],
                     op=ALU.add)
            ne, bne = self.T("mne", [128, 32], F32)
            c.op("dve", "tensor_tensor", reads=[bpre, bcnt], writes=[bne], out=ne[:], in0=pre[:, NCH - 1, :], in1=cnt[:, NCH - 1, :], op=ALU.add)
            thr, bthr = self.T("mthr", [128, 32, MT], F32)
            c.op("pool", "iota", writes=[bthr], out=thr[:], pattern=[[0, 32], [512, MT]], base=0, channel_multiplier=0,
                 allow_small_or_imprecise_dtypes=True)
            cmp_, bcmp = self.T("mcmp", [128, 32, MT], F32)
            c.op("dve", "tensor_tensor", reads=[bne, bthr], writes=[bcmp], out=cmp_[:], in0=ne[:].unsqueeze(2).to_broadcast([128, 32, MT]),
                 in1=thr[:], op=ALU.is_gt)
            tl, btl = self.T("mtl", [128, 32], F32)
            c.op("dve", "tensor_reduce", reads=[bcmp], writes=[btl], out=tl[:], in_=cmp_[:], axis=AX.X, op=ALU.add)
            Lt, bLt = self.T("mLt", [128, 32, 32], F32)
            c.op("pool", "memset", writes=[bLt], ap=Lt[:], constant=1.0)
            c.op("pool", "affine_select", reads=[bLt], writes=[bLt], out=Lt[:], in_=Lt[:], pattern=[[1, 32], [-1, 32]], compare_op=ALU.is_gt,
                 fill=0.0, base=0, channel_multiplier=0)
            t32, bt32 = self.T("mt32", [128, 32, 32], F32)
            c.op("dve", "tensor_tensor", reads=[btl, bLt], writes=[bt32], out=t32[:], in0=tl[:].unsqueeze(1).to_broadcast([128, 32, 32]),
                 in1=Lt[:], op=ALU.mult)
            ot, bot = self.T("mot", [128, 32], F32)
            c.op("dve", "tensor_reduce", reads=[bt32], writes=[bot], out=ot[:], in_=t32[:], axis=AX.X, op=ALU.add)
            up, bup = self.T("mup", [128, 32], F32)
            c.op("dve", "tensor_tensor", reads=[bot, btl], writes=[bup], out=up[:], in0=ot[:], in1=tl[:], op=ALU.add)
            off, boff = self.T("moff", [128, 32], F32)
            c.op("dve", "tensor_scalar", reads=[bot], writes=[boff], out=off[:], in0=ot[:], scalar1=512.0, scalar2=None, op0=ALU.mult)
            c.op("dve", "tensor_tensor", reads=[brank, bpre], writes=[brank], out=rank[:], in0=rank[:], in1=pre[:], op=ALU.add)
            c.op("dve", "tensor_tensor", reads=[brank, boff], writes=[brank], out=rank[:], in0=rank[:],
                 in1=off[:].unsqueeze(1).to_broadcast([128, NCH, 32]), op=ALU.add)
            posf, bposf = self.T("mposf", [128, 2, NCH], F32)
            for sl in range(2):
                c.op("dve", "tensor_tensor", reads=[bR, brank], writes=[bpre], out=pre[:], in0=R[:, :, sl * 32:(sl + 1) * 32], in1=rank[:],
                     op=ALU.mult)
                c.op("dve", "tensor_reduce", reads=[bpre], writes=[bposf], out=posf[:, sl, :], in_=pre[:], axis=AX.X, op=ALU.add)
            c.op("dve", "tensor_copy", reads=[bposf], writes=[bposi], out=posi[:], in_=posf[:])
            ti, bti = self.T("mti", [128, NT_], F32)
            c.op("pool", "iota", writes=[bti], out=ti[:], pattern=[[1, NT_]], base=0, channel_multiplier=0, allow_small_or_imprecise_dtypes=True)
            ei, bei = self.T("mei", [128, 32], F32)
            c.op("pool", "iota", writes=[bei], out=ei[:], pattern=[[1, 32]], base=0, channel_multiplier=0, allow_small_or_imprecise_dtypes=True)
            pidx, bpidx = self.T("mpidx", [128, 1], F32)
            c.op("pool", "iota", writes=[bpidx], out=pidx[:], pattern=[[0, 1]], base=0, channel_multiplier=1, allow_small_or_imprecise_dtypes=True)
            A1, bA1 = self.T("mA1", [128, NT_, 32], F32)
            A2, bA2 = self.T("mA2", [128, NT_, 32], F32)
            tib = ti[:].unsqueeze(2).to_broadcast([128, NT_, 32])
            c.op("dve", "tensor_tensor", reads=[bti, bot], writes=[bA1], out=A1[:], in0=tib, in1=ot[:].unsqueeze(1).to_broadcast([128, NT_, 32]),
                 op=ALU.is_ge)
            c.op("dve", "tensor_tensor", reads=[bti, bup], writes=[bA2], out=A2[:], in0=tib, in1=up[:].unsqueeze(1).to_broadcast([128, NT_, 32]),
                 op=ALU.is_lt)
            c.op("dve", "tensor_tensor", reads=[bA1, bA2], writes=[bA1], out=A1[:], in0=A1[:], in1=A2[:], op=ALU.mult)
            used, bused = self.T("mused", [128, NT_], F32)
            c.op("dve", "tensor_reduce", reads=[bA1], writes=[bused], out=used[:], in_=A1[:], axis=AX.X, op=ALU.add)
            c.op("dve", "tensor_tensor", reads=[bA1, bei], writes=[bA1], out=A1[:], in0=A1[:], in1=ei[:].unsqueeze(1).to_broadcast([128, NT_, 32]),
                 op=ALU.mult)
            eid, beid = self.T("meid", [128, NT_], F32)
            c.op("dve", "tensor_reduce", reads=[bA1], writes=[beid], out=eid[:], in_=A1[:], axis=AX.X, op=ALU.add)
            c.op("dve", "tensor_scalar", reads=[beid, bpidx], writes=[beid], out=eid[:], in0=eid[:], scalar1=128.0, scalar2=pidx[:, 0:1],
                 op0=ALU.mult, op1=ALU.add)
            if li:
                c.op("dve", "tensor_scalar", reads=[beid], writes=[beid], out=eid[:], in0=eid[:], scalar1=float(li * 4096), scalar2=None,
                     op0=ALU.add)
            c.op("dve", "tensor_scalar", reads=[bused], writes=[bused], out=used[:], in0=used[:], scalar1=-1.0e6, scalar2=1.0e6, op0=ALU.mult,
                 op1=ALU.add)
            c.op("dve", "tensor_tensor", reads=[beid, bused], writes=[beid], out=eid[:], in0=eid[:], in1=used[:], op=ALU.add)
            c.op("dve", "tensor_copy", reads=[beid], writes=[bwidx], out=widx[:], in_=eid[:])
        hrot = Rot([self.T(f"mh{i}", [128, 1024], BF16) for i in range(6)])
        scb = []
        for ch in range(NCH):
            hc, bhc = hrot.next()
            c.dma("sp", hc[:], self.h2b[li][ch * 128:(ch + 1) * 128, :], reads=[self.dbuf(f"h2b{li}", ch)], writes=[bhc])
            for sl in range(2):
                bs = Buf()
                c.idma(self.Xs[li], IOA(ap=posi[:, sl, ch:ch + 1], axis=0), hc[:], None, reads=[bhc, bposi] + self.zf[li], writes=[bs])
                scb.append(bs)
        wg = Rot([self.T(f"mwg{i}", [128, 8, 256], BF16) for i in range(2)])
        wu = Rot([self.T(f"mwu{i}", [128, 8, 256], BF16) for i in range(2)])
        wd = Rot([self.T(f"mwd{i}", [128, 2, 1024], BF16) for i in range(2)])
        xsr = Rot([self.T(f"mxs{i}", [128, 4, 1024], BF16) for i in range(2)])
        XTr = Rot([self.T(f"mXT{i}", [128, 8, 512], BF16) for i in range(2)])
        sgr = Rot([self.T(f"msg{i}", [128, 512], BF16) for i in range(2)])
        ar = Rot([self.T(f"ma{i}", [128, 512], BF16) for i in range(4)])
        ytr = Rot([self.T(f"myt{i}", [128, 4, 1024], BF16) for i in range(2)])
        pg = [self.pmm[0], self.pmm[1]]
        pu = [self.pmm[2], self.pmm[3]]
        pwt = self.pw[0]
        yrot = Rot([(pwt[:, 0:512], Buf("pwa", True)), (pwt[:, 512:1024], Buf("pwb", True))])
        trot = Rot([(self.pT[0][:].bitcast(BF16), self.pT[1]), (self.pmisc[0][:].bitcast(BF16), self.pmisc[1])])
        ysb = []
        loaded = {}

        def issue(i):
            g_, u_, d_, x_ = wg.next(), wu.next(), wd.next(), xsr.next()
            ioa = IOA(ap=widx[:, i:i + 1], axis=0)
            c.idma(g_[0][:].rearrange("p k f -> p (k f)"), None, W["moe_wgL"].rearrange("l r c -> (l r) c"), ioa, reads=[bwidx], writes=[g_[1]],
                   bounds_check=8191, oob_is_err=False)
            c.idma(u_[0][:].rearrange("p k f -> p (k f)"), None, W["moe_wuL"].rearrange("l r c -> (l r) c"), ioa, reads=[bwidx], writes=[u_[1]],
                   bounds_check=8191, oob_is_err=False)
            c.idma(d_[0][:].rearrange("p k f -> p (k f)"), None, W["moe_wdL"].rearrange("l r c -> (l r) c"), ioa, reads=[bwidx], writes=[d_[1]],
                   bounds_check=8191, oob_is_err=False)
            c.dma("sp", x_[0][:], self.Xs[li][i * 512:(i + 1) * 512, :].rearrange("(c p) d -> p c d", p=128),
                  reads=scb + self.zf[li], writes=[x_[1]])
            loaded[i] = (g_, u_, d_, x_)

        issue(0)
        for i in range(NT_):
            if i + 1 < NT_:
                issue(i + 1)
            (wg_, bwg), (wu_, bwu), (wd_, bwd), (x_, bx_) = loaded.pop(i)
            XT, bXT = XTr.next()
            for ch in range(4):
                pTv, bpT = trot.next()
                for k in range(8):
                    c.op("pe", "transpose", reads=[bx_, self.identb[1]], writes=[bpT], out=pTv[:, k * 128:(k + 1) * 128],
                         in_=x_[:, ch, k * 128:(k + 1) * 128], identity=self.identb[0][:])
                c.op("act", "copy", reads=[bpT], writes=[bXT], out=XT[:, 0:4, ch * 128:(ch + 1) * 128],
                     in_=pTv[:, 0:512].rearrange("p (k t) -> p k t", k=4))
                c.op("dve", "tensor_copy", reads=[bpT], writes=[bXT], out=XT[:, 4:8, ch * 128:(ch + 1) * 128],
                     in_=pTv[:, 512:1024].rearrange("p (k t) -> p k t", k=4))
            acts = []
            for fc in range(2):
                g_, bg_ = pg[fc]
                u_, bu_ = pu[fc]
                for k in range(8):
                    c.op("pe", "matmul", reads=[bwg, bXT], writes=[bg_], out=g_[:], lhsT=wg_[:, k, fc * 128:(fc + 1) * 128], rhs=XT[:, k, :],
                         start=(k == 0), stop=(k == 7))
                for k in range(8):
                    c.op("pe", "matmul", reads=[bwu, bXT], writes=[bu_], out=u_[:], lhsT=wu_[:, k, fc * 128:(fc + 1) * 128], rhs=XT[:, k, :],
                         start=(k == 0), stop=(k == 7))
                sg, bsg = sgr.next()
                c.op("act", "activation", reads=[bg_], writes=[bsg], out=sg[:], in_=g_[:], func=AF.Silu)
                a, ba = ar.next()
                c.op("dve", "tensor_tensor", reads=[bsg, bu_], writes=[ba], out=a[:], in0=sg[:], in1=u_[:], op=ALU.mult)
                acts.append((a, ba))
            yt, byt = ytr.next()
            n_ev = 0
            for ch in range(4):
                for half in range(2):
                    py, bpy = yrot.next()
                    for fc in range(2):
                        c.op("pe", "matmul", reads=[acts[fc][1], bwd], writes=[bpy], out=py, lhsT=acts[fc][0][:, ch * 128:(ch + 1) * 128],
                             rhs=wd_[:, fc, half * 512:(half + 1) * 512], start=(fc == 0), stop=(fc == 1))
                    if n_ev % 2 == 0:
                        c.op("act", "copy", reads=[bpy], writes=[byt], out=yt[:, ch, half * 512:(half + 1) * 512], in_=py)
                    else:
                        c.op("dve", "tensor_copy", reads=[bpy], writes=[byt], out=yt[:, ch, half * 512:(half + 1) * 512], in_=py)
                    n_ev += 1
            by_ = Buf()
            c.dma("sp", self.Ys[li][i * 512:(i + 1) * 512, :].rearrange("(c p) d -> p c d", p=128), yt[:], reads=[byt], writes=[by_])
            ysb.append(by_)
        y1r = Rot([self.T(f"my1{i}", [128, 1024], BF16) for i in range(4)])
        y2r = Rot([self.T(f"my2{i}", [128, 1024], BF16) for i in range(4)])
        xmr = Rot([self.T(f"mxm{i}", [128, 1024], F32) for i in range(4)])
        dgr = Rot([self.T(f"mdg{i}", [128, 2, 128], BF16) for i in range(4)])
        oname = "x1" if li == 0 else "out"
        for ch in range(NCH):
            y1, by1 = y1r.next()
            y2, by2 = y2r.next()
            xm, bxm = xmr.next()
            dg, bdg = dgr.next()
            c.idma(y1[:], None, self.Ys[li], IOA(ap=posi[:, 0, ch:ch + 1], axis=0), reads=ysb + [bposi], writes=[by1])
            c.idma(y2[:], None, self.Ys[li], IOA(ap=posi[:, 1, ch:ch + 1], axis=0), reads=ysb + [bposi], writes=[by2])
            c.dma("sp", xm[:], xin[ch * 128:(ch + 1) * 128, :], reads=[self.dbuf(f"xmid{li}", ch // 4)], writes=[bxm])
            for sl in range(2):
                c.op("act", "activation", reads=[self.identb[1], bR], writes=[bdg], out=dg[:, sl, :], in_=self.identb[0][:], func=AF.Copy,
                     scale=R[:, ch, 64 + sl:65 + sl])
            for half in range(2):
                pc, bpc = self.mmrot.next()
                hs_ = slice(half * 512, (half + 1) * 512)
                c.op("pe", "matmul", reads=[bdg, by1], writes=[bpc], out=pc[:], lhsT=dg[:, 0, :], rhs=y1[:, hs_], start=True, stop=False)
                c.op("pe", "matmul", reads=[bdg, by2], writes=[bpc], out=pc[:], lhsT=dg[:, 1, :], rhs=y2[:, hs_], start=False, stop=True)
                c.op("dve", "tensor_tensor", reads=[bpc, bxm], writes=[bxm], out=xm[:, hs_], in0=pc[:], in1=xm[:, hs_], op=ALU.add)
            if li == 0:
                wr_b = self.dbuf("x1c", ch)
            else:
                wr_b = self.dbuf("outc", ch)
            c.dma("sp", xout[ch * 128:(ch + 1) * 128, :], xm[:], reads=[bxm], writes=[wr_b])
        if li == 0:
            for t_ in range(self.NT):
                self.db[("x1", t_)] = self.db[("x1c", t_ * 4 + 3)]

    def rope_tables(self):
        c = self.c
        NCH = self.NCH
        cosd = self.nc.dram_tensor("cosd", [128, NCH, 32], F32).ap()
        sind = self.nc.dram_tensor("sind", [128, NCH, 32], F32).ap()
        with c.scope():
            cos, bcos = self.T("cos", [128, NCH, 32], F32)
            sin, bsin = self.T("sin", [128, NCH, 32], F32)
            pi_, bpi = self.T("posi", [NCH, 128], I32)
            c.dma("sp", pi_[:], self.pos.rearrange("(c p) -> c p", p=128), writes=[bpi])
            pf, bpf = self.T("posf", [NCH, 128], F32)
            c.op("dve", "tensor_copy", reads=[bpi], writes=[bpf], out=pf[:], in_=pi_[:])
            pw, bpw = self.pw
            c.op("pe", "transpose", reads=[bpf, self.identf[1]], writes=[bpw], out=pw[:, 0:NCH], in_=pf[:],
                 identity=self.identf[0][0:NCH, 0:NCH])
            pT_, bpT_ = self.T("posT", [128, NCH], F32)
            c.op("act", "copy", reads=[bpw], writes=[bpT_], out=pT_[:], in_=pw[:, 0:NCH])
            iv, biv = self.load_bcast("invf", self.invf, 32)
            ang, bang = self.T("ang", [128, NCH, 32], F32)
            c.op("dve", "tensor_tensor", reads=[bpT_, biv], writes=[bang], out=ang[:],
                 in0=pT_[:].unsqueeze(2).to_broadcast([128, NCH, 32]), in1=iv[:].unsqueeze(1).to_broadcast([128, NCH, 32]), op=ALU.mult)
            t, bt = self.T("rr_t", [128, NCH, 32], F32)
            ti, bti = self.T("rr_ti", [128, NCH, 32], I32)
            r, br = self.T("rr_r", [128, NCH, 32], F32)
            TWO_PI = 2.0 * np.pi
            C1 = 6.28125
            C2 = TWO_PI - C1
            c.op("dve", "tensor_scalar", reads=[bang], writes=[bt], out=t[:], in0=ang[:], scalar1=float(1.0 / TWO_PI), scalar2=0.5,
                 op0=ALU.mult, op1=ALU.add)
            c.op("dve", "tensor_copy", reads=[bt], writes=[bti], out=ti[:], in_=t[:])
            c.op("dve", "tensor_copy", reads=[bti], writes=[bt], out=t[:], in_=ti[:])
            c.op("dve", "scalar_tensor_tensor", reads=[bt, bang], writes=[br], out=r[:], in0=t[:], scalar=float(-C1), in1=ang[:],
                 op0=ALU.mult, op1=ALU.add)
            c.op("dve", "scalar_tensor_tensor", reads=[bt, br], writes=[br], out=r[:], in0=t[:], scalar=float(-C2), in1=r[:],
                 op0=ALU.mult, op1=ALU.add)
            c.op("dve", "tensor_scalar", reads=[br], writes=[bt], out=t[:], in0=r[:], scalar1=float(-np.pi), scalar2=float(TWO_PI),
                 op0=ALU.is_lt, op1=ALU.mult)
            c.op("dve", "tensor_tensor", reads=[br, bt], writes=[br], out=r[:], in0=r[:], in1=t[:], op=ALU.add)
            c.op("dve", "tensor_scalar", reads=[br], writes=[bt], out=t[:], in0=r[:], scalar1=float(np.pi), scalar2=float(-TWO_PI),
                 op0=ALU.is_gt, op1=ALU.mult)
            c.op("dve", "tensor_tensor", reads=[br, bt], writes=[br], out=r[:], in0=r[:], in1=t[:], op=ALU.add)
            c.op("dve", "tensor_scalar", reads=[br], writes=[br], out=r[:], in0=r[:], scalar1=float(-3.1415925), scalar2=float(3.1415925),
                 op0=ALU.max, op1=ALU.min)
            c.op("act", "activation", reads=[br], writes=[bsin], out=sin[:], in_=r[:], func=AF.Sin)
            c.op("dve", "scalar_tensor_tensor", reads=[br], writes=[bt], out=t[:], in0=r[:], scalar=-1.0, in1=r[:], op0=ALU.mult,
                 op1=ALU.max)
            c.op("dve", "tensor_scalar", reads=[bt], writes=[bt], out=t[:], in0=t[:], scalar1=-1.0, scalar2=float(np.pi / 2),
                 op0=ALU.mult, op1=ALU.add)
            c.op("act", "activation", reads=[bt], writes=[bcos], out=cos[:], in_=t[:], func=AF.Sin)
            c.dma("sp", cosd, cos[:], reads=[bcos], writes=[self.dbuf("cosd", 0)])
            c.dma("sp", sind, sin[:], reads=[bsin], writes=[self.dbuf("sind", 0)])
        return cosd, sind

    def rope_apply(self, eng_a, eng_b, x1, x2, cosb, sinb, o1, o2, tmp, btmp, rd, wr, shape):
        c = self.c
        n = int(np.prod(shape[1:]))
        t = [tmp[:, i, 0:n].rearrange("p (a b) -> p a b", a=shape[1]) if len(shape) == 3 else tmp[:, i, 0:n] for i in range(4)]
        c.op(eng_a, "tensor_tensor", reads=rd, writes=[btmp], out=t[0], in0=x1, in1=cosb, op=ALU.mult)
        c.op(eng_a, "tensor_tensor", reads=rd, writes=[btmp], out=t[1], in0=x2, in1=sinb, op=ALU.mult)
        c.op(eng_b, "tensor_tensor", reads=rd, writes=[btmp], out=t[2], in0=x1, in1=sinb, op=ALU.mult)
        c.op(eng_b, "tensor_tensor", reads=rd, writes=[btmp], out=t[3], in0=x2, in1=cosb, op=ALU.mult)
        c.op(eng_a, "tensor_tensor", reads=[btmp], writes=wr, out=o1, in0=t[0], in1=t[1], op=ALU.subtract)
        c.op(eng_b, "tensor_tensor", reads=[btmp], writes=wr, out=o2, in0=t[2], in1=t[3], op=ALU.add)

    def p3_mla_proj(self):
        c = self.c
        W = self.W
        S = self.S
        win, bwin = self.T("b_win", [128, 8, 1344], BF16)
        wq, bwq = self.T("b_wq", [128, 4, 1536], BF16)
        wkv, bwkv = self.T("b_wkv", [128, 2, 2048], BF16)
        gqr, bgqr = self.load_bcast("gqr", W["b_qn_g"][0], 192)
        gkr, bgkr = self.load_bcast("gkr", W["b_kn_g"][0], 192)
        c.op("dve", "tensor_scalar", reads=[bgqr], writes=[bgqr], out=gqr[:], in0=gqr[:], scalar1=float(192 ** -0.5), scalar2=None,
             op0=ALU.mult)
        with c.scope():
            self.wstage = Rot([self.T(f"wstage{i}", [128, 2560], F32) for i in range(2)])
            n1, bn1 = self.load_small_cols("n1g1", W["norm1_g"][1], 8)
            self.load_w_gain(win, bwin, W["b_w_in"][0], 8, 1344, n1, bn1)
            gq_, bgq_ = self.load_small_cols("qng", W["b_q_norm_g"][0], 4)
            self.load_w_gain(wq, bwq, W["b_w_q_up"][0], 4, 1536, gq_, bgq_)
            gkv_, bgkv_ = self.load_small_cols("kvng", W["b_kv_norm_g"][0], 2)
            self.load_w_gain(wkv, bwkv, W["b_w_kv_up"][0], 2, 2048, gkv_, bgkv_)
        cosd, sind = self.rope_tables()
        c.barrier()
        c.warm_segs[c.segs[-1]] = True
        self.alloc_tile_bufs(full=False)
        csr = Rot([self.T(f"cs{i}", [128, 4, 64], F32) for i in range(2)])
        cqT, bcqT = self.T("cqT", [128, 4, 512], BF16)
        ckvT, bckvT = self.T("ckvT", [128, 2, 512], BF16)
        cqn = Rot([self.T(f"cqn{i}", [128, 512], BF16) for i in range(2)])
        ckvn = Rot([self.T(f"ckvn{i}", [128, 256], BF16) for i in range(2)])
        krr, bkrr = self.T("krr", [128, 4, 64], F32)
        zs, bzs = self.T("zs", [128, 4, 3], F32)
        qfr = Rot([self.T(f"qf{i}", [128, 8, 192], F32) for i in range(2)])
        sqfr = Rot([self.T(f"sqf{i}", [128, 8, 192], F32) for i in range(2)])
        qbr = Rot([self.T(f"qb{i}", [128, 8, 192], BF16) for i in range(2)])
        hsr = Rot([self.T(f"hs{i}", [128, 8], F32) for i in range(4)])
        kfr = Rot([self.T(f"kf{i}", [128, 8, 128], F32) for i in range(2)])
        kbr = Rot([self.T(f"kb{i}", [128, 8, 192], BF16) for i in range(2)])
        vb = Rot([self.T(f"vb{i}", [128, 8, 128], BF16) for i in range(1)])
        krgr = Rot([self.T(f"krg{i}", [128, 64], F32) for i in range(2)])
        krotr = Rot([self.T(f"krot{i}", [128, 64], F32) for i in range(2)])
        rtmpr = Rot([self.T(f"rtmp{i}", [128, 4, 256], F32) for i in range(2)])
        QTnr = Rot([self.T(f"QTn{i}", [128, 8, 128], BF16) for i in range(2)])
        QTrr = Rot([self.T(f"QTr{i}", [64, 8, 128], BF16) for i in range(2)])
        KTnr = Rot([self.T(f"KTn{i}", [128, 8, 128], BF16) for i in range(2)])
        KTrr = Rot([self.T(f"KTr{i}", [64, 8, 128], BF16) for i in range(2)])
        moT, bmoT = self.T("moT", [128, 4, 512], BF16)
        junk, bjunk = self.junk
        pTv = self.pT[0][:].bitcast(BF16)
        bpT = self.pT[1]
        pwv = self.pw[0][:].bitcast(BF16)
        bpw = self.pw[1]

        def to_T(src, bsrc, dn, bdn, dr, bdr, ch):
            for h in range(8):
                c.op("pe", "transpose", reads=[bsrc, self.identb[1]], writes=[bpT], out=pTv[:, h * 128:(h + 1) * 128],
                     in_=src[:, h, 0:128], identity=self.identb[0][:])
            c.op("act", "copy", reads=[bpT], writes=[bdn], out=dn[:], in_=pTv.rearrange("p (h t) -> p h t", h=8))
            for h in range(8):
                c.op("pe", "transpose", reads=[bsrc, self.identb[1]], writes=[bpw], out=pwv[0:64, h * 128:(h + 1) * 128],
                     in_=src[:, h, 128:192], identity=self.identb[0][:])
            c.op("dve", "tensor_copy", reads=[bpw], writes=[bdr], out=dr[:], in_=pwv[0:64, 0:1024].rearrange("p (h t) -> p h t", h=8))

        for j in range(self.NT):
            xt, bxt = self.xt.next()
            c.dma("sp", xt[:], self.x1[j * 512:(j + 1) * 512, :].rearrange("(c p) d -> p c d", p=128),
                  reads=[self.dbuf("x1", j)], writes=[bxt])
            self.norm_to_hT(xt, bxt)
            hT, bhT = self.hT
            cst, bcst = csr.next()
            c.dma("sp", cst[:, :, 0:32], cosd[:, j * 4:(j + 1) * 4, :], reads=[self.dbuf("cosd", 0)], writes=[bcst])
            c.dma("sp", cst[:, :, 32:64], sind[:, j * 4:(j + 1) * 4, :], reads=[self.dbuf("sind", 0)], writes=[bcst])
            for ch in range(4):
                p1, bp1 = self.mmrot.next()
                for k in range(8):
                    c.op("pe", "matmul", reads=[bhT, bwin], writes=[bp1], out=p1[:], lhsT=hT[:, k, ch * 128:(ch + 1) * 128],
                         rhs=win[:, k, 0:512], start=(k == 0), stop=(k == 7))
                p2, bp2 = self.mmrot.next()
                for k in range(8):
                    c.op("pe", "matmul", reads=[bhT, bwin], writes=[bp2], out=p2[:, 0:320], lhsT=hT[:, k, ch * 128:(ch + 1) * 128],
                         rhs=win[:, k, 512:832], start=(k == 0), stop=(k == 7))
                c.op("act", "activation", reads=[bp1], writes=[bjunk, bzs], out=junk[:, 0:512], in_=p1[:], func=AF.Square,
                     accum_out=zs[:, ch, 0:1])
                c.op("act", "activation", reads=[bp2], writes=[bjunk, bzs], out=junk[:, 0:256], in_=p2[:, 0:256], func=AF.Square,
                     accum_out=zs[:, ch, 1:2])
                c.op("act", "activation", reads=[bp2], writes=[bjunk, bzs], out=junk[:, 0:64], in_=p2[:, 256:320], func=AF.Square,
                     accum_out=zs[:, ch, 2:3])
                c.op("act", "copy", reads=[bp2], writes=[bkrr], out=krr[:, ch, :], in_=p2[:, 256:320])
                self.rsqrt_(zs[:, ch, 0:1], bzs, 1.0 / 512, 1)
                self.rsqrt_(zs[:, ch, 1:2], bzs, 1.0 / 256, 1)
                a, ba = cqn.next()
                c.op("dve", "tensor_scalar", reads=[bp1, bzs], writes=[ba], out=a[:], in0=p1[:], scalar1=zs[:, ch, 0:1], scalar2=None,
                     op0=ALU.mult)
                b_, bb_ = ckvn.next()
                c.op("dve", "tensor_scalar", reads=[bp2, bzs], writes=[bb_], out=b_[:], in0=p2[:, 0:256], scalar1=zs[:, ch, 1:2],
                     scalar2=None, op0=ALU.mult)
                for k in range(4):
                    c.op("pe", "transpose", reads=[ba, self.identb[1]], writes=[bpT], out=pTv[:, k * 128:(k + 1) * 128],
                         in_=a[:, k * 128:(k + 1) * 128], identity=self.identb[0][:])
                for k in range(2):
                    c.op("pe", "transpose", reads=[bb_, self.identb[1]], writes=[bpT], out=pTv[:, (4 + k) * 128:(5 + k) * 128],
                         in_=b_[:, k * 128:(k + 1) * 128], identity=self.identb[0][:])
                c.op("act", "copy", reads=[bpT], writes=[bcqT], out=cqT[:, :, ch * 128:(ch + 1) * 128],
                     in_=pTv[:, 0:512].rearrange("p (k t) -> p k t", k=4))
                c.op("act", "copy", reads=[bpT], writes=[bckvT], out=ckvT[:, :, ch * 128:(ch + 1) * 128],
                     in_=pTv[:, 512:768].rearrange("p (k t) -> p k t", k=2))
            for ch in range(4):
                gc = j * 4 + ch
                cosb = cst[:, ch, 0:32].unsqueeze(1).to_broadcast([128, 8, 32])
                sinb = cst[:, ch, 32:64].unsqueeze(1).to_broadcast([128, 8, 32])
                bcos = bsin = bcst
                qf, bqf = qfr.next()
                sqf, bsqf = sqfr.next()
                qb, bqb = qbr.next()
                hs, bhs = hsr.next()
                kf, bkf = kfr.next()
                kb, bkb = kbr.next()
                krg, bkrg = krgr.next()
                krot, bkrot = krotr.next()
                rtmp, brtmp = rtmpr.next()
                QTn, bQTn = QTnr.next()
                QTr, bQTr = QTrr.next()
                KTn, bKTn = KTnr.next()
                KTr, bKTr = KTrr.next()
                gs = slice(gc * 128, (gc + 1) * 128)
                for grp in range(4):
                    pq, bpq = self.mmrot.next()
                    for k in range(4):
                        c.op("pe", "matmul", reads=[bcqT, bwq], writes=[bpq], out=pq[:, 0:384], lhsT=cqT[:, k, ch * 128:(ch + 1) * 128],
                             rhs=wq[:, k, grp * 384:(grp + 1) * 384], start=(k == 0), stop=(k == 3))
                    c.op("act", "copy", reads=[bpq], writes=[bqf], out=qf[:, 2 * grp:2 * grp + 2, :],
                         in_=pq[:, 0:384].rearrange("p (h d) -> p h d", h=2))
                c.op("dve", "tensor_tensor", reads=[bqf], writes=[bsqf], out=sqf[:], in0=qf[:], in1=qf[:], op=ALU.mult)
                c.op("dve", "tensor_reduce", reads=[bsqf], writes=[bhs], out=hs[:], in_=sqf[:], axis=AX.X, op=ALU.add)
                self.rsqrt_(hs[:], bhs, 1.0 / 192, 8)
                c.op("dve", "tensor_tensor", reads=[bqf, bhs], writes=[bqf], out=qf[:], in0=qf[:],
                     in1=hs[:].unsqueeze(2).to_broadcast([128, 8, 192]), op=ALU.mult)
                c.op("pool", "tensor_tensor", reads=[bqf, bgqr], writes=[bqf], out=qf[:], in0=qf[:],
                     in1=gqr[:].unsqueeze(1).to_broadcast([128, 8, 192]), op=ALU.mult)
                c.op("act", "copy", reads=[bqf], writes=[bqb], out=qb[:, :, 0:128], in_=qf[:, :, 0:128])
                self.rope_apply("dve", "pool", qf[:, :, 128:160], qf[:, :, 160:192], cosb, sinb, qb[:, :, 128:160], qb[:, :, 160:192],
                                rtmp, brtmp, [bqf, bcos, bsin], [bqb], [128, 8, 32])
                to_T(qb, bqb, QTn, bQTn, QTr, bQTr, ch)
                c.dma("sp", self.QT[:, 0:128, gs].rearrange("h p t -> p h t"), QTn[:], reads=[bQTn], writes=[self.dbuf("QTn", gc)])
                c.dma("sp", self.QT[:, 128:192, gs].rearrange("h p t -> p h t"), QTr[:], reads=[bQTr], writes=[self.dbuf("QTr", gc)])
                sqf, bsqf = sqfr.next()
                hs, bhs = hsr.next()
                rtmp, brtmp = rtmpr.next()
                v_, bv_ = vb.next()
                for grp in range(4):
                    pk, bpk = self.mmrot.next()
                    for k in range(2):
                        c.op("pe", "matmul", reads=[bckvT, bwkv], writes=[bpk], out=pk[:], lhsT=ckvT[:, k, ch * 128:(ch + 1) * 128],
                             rhs=wkv[:, k, grp * 512:(grp + 1) * 512], start=(k == 0), stop=(k == 1))
                    pkv = pk[:].rearrange("p (h d) -> p h d", h=2)
                    c.op("act", "copy", reads=[bpk], writes=[bkf], out=kf[:, 2 * grp:2 * grp + 2, :], in_=pkv[:, :, 0:128])
                    c.op("dve", "tensor_copy", reads=[bpk], writes=[bv_], out=v_[:, 2 * grp:2 * grp + 2, :], in_=pkv[:, :, 128:256])
                c.dma("sp", self.Vd[gc * 128:(gc + 1) * 128, :], v_[:].rearrange("p h d -> p (h d)"), reads=[bv_],
                      writes=[self.dbuf("Vd", gc)])
                c.op("dve", "tensor_tensor", reads=[bkf], writes=[bsqf], out=sqf[:, :, 0:128], in0=kf[:], in1=kf[:], op=ALU.mult)
                c.op("dve", "tensor_reduce", reads=[bsqf], writes=[bhs], out=hs[:], in_=sqf[:, :, 0:128], axis=AX.X, op=ALU.add)
                c.op("dve", "tensor_scalar", reads=[bhs, bzs], writes=[bhs], out=hs[:], in0=hs[:], scalar1=zs[:, ch, 2:3], scalar2=None,
                     op0=ALU.add)
                self.rsqrt_(hs[:], bhs, 1.0 / 192, 8)
                c.op("dve", "tensor_tensor", reads=[bkf, bhs], writes=[bkf], out=kf[:], in0=kf[:],
                     in1=hs[:].unsqueeze(2).to_broadcast([128, 8, 128]), op=ALU.mult)
                c.op("pool", "tensor_tensor", reads=[bkf, bgkr], writes=[bkb], out=kb[:, :, 0:128], in0=kf[:],
                     in1=gkr[:, 0:128].unsqueeze(1).to_broadcast([128, 8, 128]), op=ALU.mult)
                c.op("pool", "tensor_tensor", reads=[bkrr, bgkr], writes=[bkrg], out=krg[:], in0=krr[:, ch, :], in1=gkr[:, 128:192],
                     op=ALU.mult)
                self.rope_apply("dve", "pool", krg[:, 0:32], krg[:, 32:64], cst[:, ch, 0:32], cst[:, ch, 32:64], krot[:, 0:32], krot[:, 32:64],
                                rtmp, brtmp, [bkrg, bcos, bsin], [bkrot], [128, 32])
                c.op("dve", "tensor_tensor", reads=[bkrot, bhs], writes=[bkb], out=kb[:, :, 128:192],
                     in0=krot[:].unsqueeze(1).to_broadcast([128, 8, 64]), in1=hs[:].unsqueeze(2).to_broadcast([128, 8, 64]), op=ALU.mult)
                to_T(kb, bkb, KTn, bKTn, KTr, bKTr, ch)
                c.dma("sp", self.KT[:, 0:128, gs].rearrange("h p t -> p h t"), KTn[:], reads=[bKTn], writes=[self.dbuf("KTn", gc)])
                c.dma("sp", self.KT[:, 128:192, gs].rearrange("h p t -> p h t"), KTr[:], reads=[bKTr], writes=[self.dbuf("KTr", gc)])
            cs = slice(j * 512, (j + 1) * 512)
            self.qmem_and_attn(1, win, bwin, 832, moT, bmoT, 0)
            c.dma("sp", self.MO[:, :, cs].rearrange("h p t -> p h t"), moT[:], reads=[bmoT], writes=[self.dbuf("MO", j)])

    def p4_attn(self):
        c = self.c
        S, NT, NCH = self.S, self.NT, self.NCH
        c.barrier()
        c.cp_segs[c.segs[-1]] = True
        kn = Rot([self.T(f"A_kn{i}", [128, S], BF16) for i in range(2)])
        kr = Rot([self.T(f"A_kr{i}", [128, S], BF16) for i in range(2)])
        va = Rot([self.T(f"A_v{i}", [128, NCH, 130], BF16) for i in range(2)])
        qn = Rot([self.T(f"A_qn{i}", [128, 512], BF16) for i in range(2)])
        qr = Rot([self.T(f"A_qr{i}", [128, 512], BF16) for i in range(2)])
        PT = Rot([self.T(f"A_P{i}", [128, 512], BF16) for i in range(8)])
        osb = Rot([self.T(f"A_osb{i}", [128, 4, 128], BF16) for i in range(2)])
        rden = Rot([self.T(f"A_rd{i}", [128, 4], F32) for i in range(2)])
        ot = Rot([self.T(f"A_o{i}", [128, 512], BF16) for i in range(2)])
        tri, btri = self.T("A_tri", [128, 128], BF16)
        c.op("pool", "memset", writes=[btri], ap=tri[:], constant=1.0)
        c.op("pool", "affine_select", reads=[btri], writes=[btri], out=tri[:], in_=tri[:], pattern=[[1, 128]], compare_op=ALU.is_ge,
             fill=0.0, base=0, channel_multiplier=-1)
        for t, b in kr.items + qr.items:
            c.op("pool", "memset", writes=[b], ap=t[:], constant=0.0)
        for t, b in va.items:
            c.op("pool", "memset", writes=[b], ap=t[:, :, 128:130], constant=1.0)
        srot = Rot(self.pmm)
        pwt = self.pw[0]
        osets = [[(pwt[:, 0:512], Buf("oA0", True)), (pwt[:, 512:1024], Buf("oA1", True))],
                 [(self.pT[0][:], Buf("oB0", True)), (self.pmisc[0][:], Buf("oB1", True))]]
        it = 0
        for h in range(8):
            Kn, bKn = kn.next()
            Kr, bKr = kr.next()
            V, bV = va.next()
            c.dma("sp", Kn[:], self.KT[h, 0:128, :], reads=[self.dbuf("KTn", g) for g in range(NCH)], writes=[bKn])
            c.dma("sp", Kr[0:64, :], self.KT[h, 128:192, :], reads=[self.dbuf("KTr", g) for g in range(NCH)], writes=[bKr])
            c.dma("sp", V[:, :, 0:128], self.Vd[:, h * 128:(h + 1) * 128].rearrange("(c p) d -> p c d", p=128),
                  reads=[self.dbuf("Vd", g) for g in range(NCH)], writes=[bV])
            for qt in range(NT):
                Qn, bQn = qn.next()
                Qr, bQr = qr.next()
                cs = slice(qt * 512, (qt + 1) * 512)
                c.dma("sp", Qn[:], self.QT[h, 0:128, cs], reads=[self.dbuf("QTn", g) for g in range(4 * qt, 4 * qt + 4)], writes=[bQn])
                c.dma("sp", Qr[0:64, :], self.QT[h, 128:192, cs], reads=[self.dbuf("QTr", g) for g in range(4 * qt, 4 * qt + 4)], writes=[bQr])
                nk = 4 * (qt + 1)
                oset = osets[it % 2]
                it += 1
                for kc in range(nk):
                    di = max(kc - 4 * qt, 0)
                    q0 = di * 128
                    ps_, bps = srot.next()
                    c.op("pe", "matmul", reads=[bKn, bQn], writes=[bps], out=ps_[:, q0:512], lhsT=Kn[:, kc * 128:(kc + 1) * 128],
                         rhs=Qn[:, q0:512], start=True, stop=False)
                    c.op("pe", "matmul", reads=[bKr, bQr], writes=[bps], out=ps_[:, q0:512], lhsT=Kr[:, kc * 128:(kc + 1) * 128],
                         rhs=Qr[:, q0:512], start=False, stop=True)
                    P, bP = PT.next()
                    c.op("act", "activation", reads=[bps], writes=[bP], out=P[:, q0:512], in_=ps_[:, q0:512], func=AF.Exp)
                    if kc >= 4 * qt:
                        c.op("pool", "tensor_tensor", reads=[bP, btri], writes=[bP], out=P[:, q0:q0 + 128], in0=P[:, q0:q0 + 128],
                             in1=tri[:], op=ALU.mult)
                    for qc in range(di, 4):
                        ob, bob = oset[qc // 2]
                        col = (qc % 2) * 130
                        c.op("pe", "matmul", reads=[bP, bV], writes=[bob], out=ob[:, col:col + 130], lhsT=P[:, qc * 128:(qc + 1) * 128],
                             rhs=V[:, kc, :], start=(kc == 0 and qc % 2 == 0), stop=(kc == 4 * qt + qc), skip_group_check=True)
                rd, brd = rden.next()
                ob_, bob_ = osb.next()
                pt_, bpt = srot.next()
                ptv = pt_[:].bitcast(BF16)
                for qc in range(4):
                    ob, bob = oset[qc // 2]
                    col = (qc % 2) * 130
                    c.op("dve", "reciprocal", reads=[bob], writes=[brd], out=rd[:, qc:qc + 1], in_=ob[:, col + 128:col + 129])
                    c.op("dve", "tensor_scalar", reads=[bob, brd], writes=[bob_], out=ob_[:, qc, :], in0=ob[:, col:col + 128],
                         scalar1=rd[:, qc:qc + 1], scalar2=None, op0=ALU.mult)
                    c.op("pe", "transpose", reads=[bob_, self.identb[1]], writes=[bpt], out=ptv[:, qc * 128:(qc + 1) * 128],
                         in_=ob_[:, qc, :], identity=self.identb[0][:])
                o, bo = ot.next()
                c.op("act", "copy", reads=[bpt], writes=[bo], out=o[:], in_=ptv[:, 0:512])
                c.dma("sp", self.OT[h, :, cs], o[:], reads=[bo], writes=[self.dbuf("OT", (h, qt))])

    def p5_out(self):
        c = self.c
        W = self.W
        wout, bwout = self.T("b_wout", [128, 12, 1024], BF16)
        self.load_w_cast(wout, bwout, W["b_w_out"][0], 12)
        c.barrier()
        c.warm_segs[c.segs[-1]] = True
        self.alloc_tile_bufs(deep=True)
        for j in range(self.NT):
            self.catT = self.catTr.next()
            catT, bcat = self.catT
            xt, bxt = self.xt.next()
            cs = slice(j * 512, (j + 1) * 512)
            c.dma("sp", xt[:], self.x1[j * 512:(j + 1) * 512, :].rearrange("(c p) d -> p c d", p=128),
                  reads=[self.dbuf("x1", j)], writes=[bxt])
            c.dma("sp", catT[:, 0:8, :], self.OT[:, :, cs].rearrange("h p t -> p h t"),
                  reads=[self.dbuf("OT", (h, j)) for h in range(8)], writes=[bcat])
            c.dma("sp", catT[:, 8:12, :], self.MO[:, :, cs].rearrange("h p t -> p h t"), reads=[self.dbuf("MO", j)], writes=[bcat])
            self.out_proj_norm2_router(1, j, xt, bxt, wout, bwout)


INV_FREQ = (10000.0 ** (-(np.arange(32, dtype=np.float32) * 2.0 / 64))).astype(np.float32)


def relayout_experts(inputs):
    g = np.asarray(inputs["moe_w_gate"]).reshape(2, 32, 8, 128, 256).transpose(0, 1, 3, 2, 4).reshape(2, 4096, 2048)
    u = np.asarray(inputs["moe_w_up"]).reshape(2, 32, 8, 128, 256).transpose(0, 1, 3, 2, 4).reshape(2, 4096, 2048)
    d = np.asarray(inputs["moe_w_down"]).reshape(2, 32, 2, 128, 1024).transpose(0, 1, 3, 2, 4).reshape(2, 4096, 2048)
    return {"moe_wgL": np.ascontiguousarray(g), "moe_wuL": np.ascontiguousarray(u), "moe_wdL": np.ascontiguousarray(d)}


def make_in_maps(inputs, S, ncores):
    maps = []
    inputs = dict(inputs)
    inputs.update(relayout_experts(inputs))
    for b in range(ncores):
        m = {n: np.ascontiguousarray(inputs[n]) for n, _ in WEIGHT_NAMES}
        m["x"] = np.ascontiguousarray(inputs["x"][b])
        m["mem"] = np.ascontiguousarray(inputs["mem"][b])
        m["positions"] = np.ascontiguousarray(inputs["positions"][b]).astype(np.int32)
        m["inv_freq"] = INV_FREQ
        maps.append(m)
    return maps


def kernel(**inputs):
    B, S, _ = inputs["x"].shape
    prog = Prog(S)
    maps = make_in_maps(inputs, S, B)
    res = run_bass_kernel_spmd(prog.nc, maps, core_ids=list(range(B)))
    return np.stack([r["out"] for r in res.results], axis=0).astype(np.float32)
```
